# Optimizing a Trainium2 kernel written in Bass

```python
import jax, jax.numpy as jnp
from jax import lax
import numpy as np

D_MODEL = 1024
BATCH = 16
SEQ = 2048
DEPTH = 2

N_MIXERS = 2
PLE_DIM = 256
NORM_EPS = 1e-6
A_HEADS = 8
A_QK_DIM = 128
A_V_DIM = 128
A_Q_RANK = 384
A_KV_RANK = 256
IDX_HEADS = 8
IDX_DIM = 64
TOPK_MAX = 256
Q_BLOCK = 128
A_WIDTH = A_HEADS * A_V_DIM
A_SPLITS = [A_Q_RANK, A_Q_RANK + A_KV_RANK, A_Q_RANK + A_KV_RANK + IDX_DIM, A_Q_RANK + A_KV_RANK + IDX_DIM + IDX_HEADS]
A_IN = A_SPLITS[-1] + A_WIDTH
R_HEADS = 4
R_QK_DIM = D_MODEL // R_HEADS
R_V_DIM = 2 * D_MODEL // R_HEADS
R_WIDTH = R_HEADS * R_V_DIM
R_CHUNK = 128
R_SPLITS = [D_MODEL, 2 * D_MODEL, 2 * D_MODEL + R_WIDTH]
R_IN = R_SPLITS[-1] + R_WIDTH
N_A_LAYERS = (DEPTH + 1) // 2
N_B_LAYERS = DEPTH // 2

kernel_name = "dsa_retention_interleaved_hybrid"


def rmsnorm(x, g):
    xf = x.astype(jnp.float32)
    y = xf * lax.rsqrt(jnp.mean(xf * xf, axis=-1, keepdims=True) + NORM_EPS)
    return (y * g.astype(jnp.float32)).astype(x.dtype)


def layernorm(x, g, b):
    xf = x.astype(jnp.float32)
    mu = jnp.mean(xf, axis=-1, keepdims=True)
    var = jnp.mean(jnp.square(xf - mu), axis=-1, keepdims=True)
    y = (xf - mu) * lax.rsqrt(var + NORM_EPS)
    return (y * g.astype(jnp.float32) + b.astype(jnp.float32)).astype(x.dtype)


def dsa_mixer(h, w_in, g_q, g_kv, w_q_up, w_idx_q, g_ik, b_ik, w_uk, w_uv, w_out):
    B, L, _ = h.shape
    z = h @ w_in
    c_q, c_kv, k_idx, w_idx, gate = jnp.split(z, A_SPLITS, axis=-1)
    c_q = rmsnorm(c_q, g_q)
    c_kv = rmsnorm(c_kv, g_kv)
    q = (c_q @ w_q_up).reshape(B, L, A_HEADS, A_QK_DIM)
    q_abs = jnp.einsum('blhd,hcd->blhc', q, w_uk) * (A_QK_DIM ** -0.5)
    q_idx = (c_q @ w_idx_q).reshape(B, L, IDX_HEADS, IDX_DIM)
    k_idx = layernorm(k_idx, g_ik, b_ik)
    w_idx = w_idx * (IDX_HEADS ** -0.5 * IDX_DIM ** -0.5)
    topk = min(TOPK_MAX, L // 4)
    nblk = L // Q_BLOCK
    key_pos = jnp.arange(L)

    def to_blocks(a):
        return a.reshape(B, nblk, Q_BLOCK, *a.shape[2:]).swapaxes(0, 1)

    def block(args):
        blk, qa, qi, wi = args
        t = blk * Q_BLOCK + jnp.arange(Q_BLOCK)
        sc = jnp.einsum('bqhd,bsd->bqhs', qi, k_idx)
        idx_score = jnp.einsum('bqhs,bqh->bqs', jax.nn.relu(sc), wi).astype(jnp.float32)
        causal = key_pos[None, :] <= t[:, None]
        idx_score = jnp.where(causal[None], idx_score, -jnp.inf)
        _, sel = lax.top_k(idx_score, topk)
        valid = sel <= t[None, :, None]
        kv_sel = jax.vmap(lambda c, i: c[i])(c_kv, sel)
        s = jnp.einsum('bqhc,bqkc->bqhk', qa, kv_sel).astype(jnp.float32)
        s = jnp.where(valid[:, :, None, :], s, -jnp.inf)
        pr = jax.nn.softmax(s, axis=-1).astype(kv_sel.dtype)
        return jnp.einsum('bqhk,bqkc->bqhc', pr, kv_sel)

    o = lax.map(block, (jnp.arange(nblk), to_blocks(q_abs), to_blocks(q_idx), to_blocks(w_idx)))
    o = o.swapaxes(0, 1).reshape(B, L, A_HEADS, A_KV_RANK)
    o = jnp.einsum('blhc,hcv->blhv', o, w_uv).reshape(B, L, A_WIDTH)
    return (o * jax.nn.silu(gate)) @ w_out


def rotate_every_two(x):
    x1 = x[..., ::2]
    x2 = x[..., 1::2]
    return jnp.stack((-x2, x1), axis=-1).reshape(x.shape)


def retention_mixer(h, w_in, w_out):
    B, L, _ = h.shape
    z = h @ w_in
    q, k, v, gate = jnp.split(z, R_SPLITS, axis=-1)
    q = q.reshape(B, L, R_HEADS, R_QK_DIM).astype(jnp.float32)
    k = k.reshape(B, L, R_HEADS, R_QK_DIM).astype(jnp.float32) * (R_QK_DIM ** -0.5)
    v = v.reshape(B, L, R_HEADS, R_V_DIM).astype(jnp.float32)
    pos = jnp.arange(L, dtype=jnp.float32)
    angle = 1.0 / (10000.0 ** jnp.linspace(0.0, 1.0, R_QK_DIM // 2, dtype=jnp.float32))
    angle = jnp.repeat(angle, 2)
    theta = pos[:, None] * angle[None, :]
    sin = jnp.sin(theta)[None, :, None, :]
    cos = jnp.cos(theta)[None, :, None, :]
    q = q * cos + rotate_every_two(q) * sin
    k = k * cos + rotate_every_two(k) * sin
    log_gamma = jnp.log(1.0 - 2.0 ** (-5.0 - jnp.arange(R_HEADS, dtype=jnp.float32)))
    ci = jnp.arange(R_CHUNK, dtype=jnp.float32)
    diff = ci[:, None] - ci[None, :]
    dmask = jnp.where(diff[None] >= 0, jnp.exp(diff[None] * log_gamma[:, None, None]), 0.0)
    xi = jnp.exp((ci[None, :] + 1.0) * log_gamma[:, None]).T
    zeta = jnp.exp((R_CHUNK - 1.0 - ci[None, :]) * log_gamma[:, None])
    chunk_decay = jnp.exp(R_CHUNK * log_gamma)
    n = L // R_CHUNK

    def to_chunks(a):
        return a.reshape(B, n, R_CHUNK, *a.shape[2:]).swapaxes(0, 1)

    def step(state, inp):
        qc, kc, vc = inp
        s = jnp.einsum('bihd,bjhd->bhij', qc, kc) * dmask[None]
        inner = jnp.einsum('bhij,bjhv->bihv', s, vc)
        cross = jnp.einsum('bihd,bhdv->bihv', qc, state) * xi[None, :, :, None]
        state = state * chunk_decay[None, :, None, None] + jnp.einsum('bjhd,bjhv,hj->bhdv', kc, vc, zeta)
        return state, inner + cross

    state0 = jnp.zeros((B, R_HEADS, R_QK_DIM, R_V_DIM), jnp.float32)
    _, o = lax.scan(step, state0, (to_chunks(q), to_chunks(k), to_chunks(v)))
    o = o.swapaxes(0, 1).reshape(B, L, R_HEADS, R_V_DIM)
    mu = jnp.mean(o, axis=-1, keepdims=True)
    var = jnp.mean(jnp.square(o - mu), axis=-1, keepdims=True)
    o = ((o - mu) * lax.rsqrt(var + NORM_EPS)).reshape(B, L, R_WIDTH).astype(h.dtype)
    return (o * jax.nn.silu(gate)) @ w_out


def setup_inputs(seed: int = 0) -> dict:
    key = jax.random.key(seed)
    ks = jax.random.split(key, 24)
    f32 = jnp.float32

    def nrm(k, shape, fan_in):
        return jax.random.normal(k, shape, f32) * (fan_in ** -0.5)

    def gain(k, shape):
        return 1.0 + 0.02 * jax.random.normal(k, shape, f32)

    return {
        "x": jax.random.normal(ks[0], (BATCH, SEQ, D_MODEL), f32),
        "p": jax.random.normal(ks[1], (DEPTH, BATCH, SEQ, PLE_DIM), f32),
        "g_pre": gain(ks[2], (DEPTH, D_MODEL)),
        "a_w_in": nrm(ks[3], (N_A_LAYERS, D_MODEL, A_IN), D_MODEL),
        "a_g_q": gain(ks[4], (N_A_LAYERS, A_Q_RANK)),
        "a_g_kv": gain(ks[5], (N_A_LAYERS, A_KV_RANK)),
        "a_w_q_up": nrm(ks[6], (N_A_LAYERS, A_Q_RANK, A_HEADS * A_QK_DIM), A_Q_RANK),
        "a_w_idx_q": nrm(ks[7], (N_A_LAYERS, A_Q_RANK, IDX_HEADS * IDX_DIM), A_Q_RANK),
        "a_g_ik": gain(ks[8], (N_A_LAYERS, IDX_DIM)),
        "a_b_ik": 0.02 * jax.random.normal(ks[9], (N_A_LAYERS, IDX_DIM), f32),
        "a_w_uk": nrm(ks[10], (N_A_LAYERS, A_HEADS, A_KV_RANK, A_QK_DIM), A_KV_RANK),
        "a_w_uv": nrm(ks[11], (N_A_LAYERS, A_HEADS, A_KV_RANK, A_V_DIM), A_KV_RANK),
        "a_w_out": nrm(ks[12], (N_A_LAYERS, A_WIDTH, D_MODEL), A_WIDTH),
        "r_w_in": nrm(ks[13], (N_B_LAYERS, D_MODEL, R_IN), D_MODEL),
        "r_w_out": nrm(ks[14], (N_B_LAYERS, R_WIDTH, D_MODEL), R_WIDTH),
        "w_ple_gate": nrm(ks[15], (DEPTH, D_MODEL, D_MODEL), D_MODEL),
        "g_ple": gain(ks[16], (DEPTH, D_MODEL)),
        "w_ple": nrm(ks[17], (DEPTH, PLE_DIM, D_MODEL), PLE_DIM),
        "g_final": gain(ks[18], (D_MODEL,)),
    }


def reference(x, p, g_pre, a_w_in, a_g_q, a_g_kv, a_w_q_up, a_w_idx_q, a_g_ik, a_b_ik,
              a_w_uk, a_w_uv, a_w_out, r_w_in, r_w_out, w_ple_gate, g_ple, w_ple, g_final):
    h = x
    for i in range(DEPTH):
        hn = rmsnorm(h, g_pre[i])
        j = i // N_MIXERS
        if i % N_MIXERS == 0:
            y = dsa_mixer(hn, a_w_in[j], a_g_q[j], a_g_kv[j], a_w_q_up[j], a_w_idx_q[j],
                          a_g_ik[j], a_b_ik[j], a_w_uk[j], a_w_uv[j], a_w_out[j])
        else:
            y = retention_mixer(hn, r_w_in[j], r_w_out[j])
        h = h + y
        ple_gate = jax.nn.sigmoid(rmsnorm(h, g_ple[i]) @ w_ple_gate[i])
        h = h + (p[i] @ w_ple[i]) * ple_gate
    return rmsnorm(h, g_final)
```

```python
import numpy as np
import concourse.bass as bass
import concourse.mybir as mybir
from concourse.bass_utils import run_bass_kernel_spmd
from contextlib import ExitStack

F32 = mybir.dt.float32
BF16 = mybir.dt.bfloat16
AF = mybir.ActivationFunctionType
ALU = mybir.AluOpType
AX = mybir.AxisListType

D = 1024
EPS = 1e-6
COMPUTE = ("pe", "act", "dve", "pool")


class Buf:
    __slots__ = ("name", "writer", "readers")

    def __init__(self, name):
        self.name = name
        self.writer = None
        self.readers = []


class DmaSem:
    __slots__ = ("sem", "count")

    def __init__(self, sem):
        self.sem = sem
        self.count = 0


class Op:
    __slots__ = ("eng", "fn", "waits", "sig", "count", "dsem", "dval", "is_dma")

    def __init__(self, eng, fn):
        self.eng = eng
        self.fn = fn
        self.waits = []
        self.sig = False
        self.count = None
        self.dsem = None
        self.dval = None
        self.is_dma = False


class Prog:
    def __init__(self, nc, es, strict=False):
        self.nc = nc
        self.es = es
        self.strict = strict
        self.ops = {e: [] for e in ("pe", "act", "dve", "pool", "sp")}
        self.esem = {e: es.enter_context(nc.semaphore("s_" + e)) for e in COMPUTE}
        self.dsems = []
        self.pending = {e: [] for e in self.ops}

    def sb(self, name, shape, dt):
        return self.es.enter_context(self.nc.sbuf_tensor("sb_" + name, list(shape), dt))

    def dsem(self, name):
        d = DmaSem(self.es.enter_context(self.nc.semaphore("d_" + name)))
        self.dsems.append(d)
        return d

    def _dep(self, op, prod):
        if prod is None or prod is op:
            return
        if prod.is_dma:
            op.waits.append((prod.dsem, prod.dsem.count))
        else:
            prod.sig = True
            op.waits.append(prod)

    def barrier(self):
        lasts = []
        for e in COMPUTE:
            comp = [o for o in self.ops[e] if not o.is_dma]
            if comp:
                comp[-1].sig = True
                lasts.append(comp[-1])
        dm = [(d, d.count) for d in self.dsems if d.count > 0]
        for e in self.ops:
            self.pending[e] = list(lasts) + list(dm)

    def _track(self, op, reads, writes):
        for b in reads:
            w = b.writer
            if w is not None:
                if not (w.eng == op.eng and op.eng == "pe" and not w.is_dma and not op.is_dma):
                    self._dep(op, w)
        for b in writes:
            w = b.writer
            if w is not None and (w.is_dma or op.is_dma or w.eng != op.eng or (self.strict and op.eng != "pe")):
                self._dep(op, w)
            for r in b.readers:
                if r.is_dma or op.is_dma or r.eng != op.eng or (self.strict and op.eng != "pe"):
                    self._dep(op, r)
        for b in reads:
            b.readers.append(op)
        for b in writes:
            b.writer = op
            b.readers = []

    def op(self, eng, fn, reads=(), writes=()):
        o = Op(eng, fn)
        if self.pending[eng]:
            o.waits.extend(self.pending[eng])
            self.pending[eng] = []
        self._track(o, reads, writes)
        self.ops[eng].append(o)
        return o

    def dma(self, eng, out, in_, dsem, reads=(), writes=()):
        def fn(e):
            return e.dma_start(out=out, in_=in_)
        o = Op(eng, fn)
        o.is_dma = True
        o.dsem = dsem
        if self.pending[eng]:
            o.waits.extend(self.pending[eng])
            self.pending[eng] = []
        self._track(o, reads, writes)
        dsem.count += 16
        o.dval = dsem.count
        self.ops[eng].append(o)
        return o

    def emit(self, final_waits=()):
        nc = self.nc
        for e in COMPUTE:
            c = 0
            for o in self.ops[e]:
                if o.is_dma:
                    continue
                if o.sig:
                    c += 1
                o.count = c

        def run(engname, engobj):
            waited = {}
            for o in self.ops[engname]:
                for p in o.waits:
                    if isinstance(p, tuple):
                        sem, val = p[0].sem, p[1]
                    else:
                        if p.eng == engname and p.count is None:
                            continue
                        sem, val = self.esem[p.eng], p.count
                    key = id(sem)
                    if waited.get(key, 0) < val:
                        engobj.wait_ge(sem, val)
                        waited[key] = val
                ins = o.fn(engobj)
                if o.is_dma:
                    ins.then_inc(o.dsem.sem, 16)
                elif o.sig:
                    ins.then_inc(self.esem[engname], 1)
            if engname == "sp":
                for ds in final_waits:
                    engobj.wait_ge(ds.sem, ds.count)

        with nc.Block() as block:
            @block.tensor
            def _(e):
                run("pe", e)

            @block.scalar
            def _(e):
                run("act", e)

            @block.vector
            def _(e):
                run("dve", e)

            @block.gpsimd
            def _(e):
                run("pool", e)

            @block.sync
            def _(e):
                run("sp", e)


I32 = mybir.dt.int32


def newton_rsqrt(P, v, tmp, bufs, n):
    hv = tmp[:, 0:n]
    y = tmp[:, n:2 * n]
    t = tmp[:, 2 * n:3 * n]
    P.op("dve", lambda e: e.tensor_scalar(out=hv, in0=v, scalar1=-0.5, scalar2=None, op0=ALU.mult), reads=bufs, writes=bufs)
    P.op("dve", lambda e: e.tensor_copy(out=t, in_=v.bitcast(I32)), reads=bufs, writes=bufs)
    P.op("dve", lambda e: e.tensor_scalar(out=y.bitcast(I32), in0=t, scalar1=-0.5, scalar2=1597463007.0,
                                          op0=ALU.mult, op1=ALU.add), reads=bufs, writes=bufs)
    for it in range(3):
        dst = v if it == 2 else y
        P.op("dve", lambda e: e.tensor_tensor(out=t, in0=y, in1=y, op=ALU.mult), reads=bufs, writes=bufs)
        P.op("dve", lambda e: e.tensor_tensor(out=t, in0=t, in1=hv, op=ALU.mult), reads=bufs, writes=bufs)
        P.op("dve", lambda e, dst=dst: e.scalar_tensor_tensor(out=dst, in0=t, scalar=1.5, in1=y, op0=ALU.add, op1=ALU.mult),
             reads=bufs, writes=bufs)


def build(L=2048, NS=2, KBIS=24, mode="full", strict=False):
    NT = L // 128
    TOPK = min(256, L // 4)
    nc = bass.Bass("TRN2", target_bir_lowering=False)

    def din(name, shape):
        return nc.dram_tensor(name, list(shape), F32, kind="ExternalInput").ap()

    x_d = din("x", [NS, L, D])
    p_d = din("p", [2, NS, L, 256])
    vec_d = din("vecs", [5, 128, D])
    vsm_d = din("vsm", [128, 768])
    w0in_d = din("w0in", [128, 8, 1792])
    wq_d = din("wq", [128, 3, 1024])
    wiq_d = din("wiq", [128, 3, 512])
    wukT_d = din("wukT", [128, 8, 256])
    wuv_d = din("wuv", [128, 2, 1024])
    wout0_d = din("wout0", [128, 8, 1024])
    wpg_d = din("wpg", [2, 128, 8, 1024])
    wp_d = din("wp", [2, 128, 2, 1024])
    w1in_d = din("w1in", [128, 8, 6144])
    wout1_d = din("wout1", [128, 16, 1024])
    ident_d = din("ident", [128, 128])
    causT_d = din("causT", [128, 128])
    cbias_d = din("cbias", [128, 128])
    pow2_d = din("pow2", [128, KBIS])
    rot_d = din("rot", [NT, 128, 4, 4, 128])
    out_d = nc.dram_tensor("out", [NS, L, D], F32, kind="ExternalOutput").ap()
    h1_d = nc.dram_tensor("h1s", [NS, L, D], F32, kind="Internal").ap()
    og_d = nc.dram_tensor("ogs", [NS, L, 2048], BF16, kind="Internal").ap()

    gam = [1.0 - 2.0 ** (-5.0 - h) for h in range(4)]
    gch = [g ** 128 for g in gam]

    es = ExitStack()
    with es:
        P = Prog(nc, es, strict)

        def B(name):
            return Buf(name)

        PS = es.enter_context(nc.psum_tensor("PS", [128, 4096], F32))
        PB = [B("bank%d" % k) for k in range(8)]

        def pbk(lo, hi):
            return PB[lo // 512:(hi - 1) // 512 + 1]

        sb = P.sb
        ident = sb("ident", [128, 128], BF16); b_ident = B("ident")
        causT = sb("causT", [128, 128], BF16); b_causT = B("causT")
        cbias = sb("cbias", [128, 128], F32); b_cbias = B("cbias")
        pow2 = sb("pow2", [128, KBIS], F32); b_pow2 = B("pow2")
        ds_c = P.dsem("const")
        P.dma("pool", ident[:], ident_d, ds_c, writes=[b_ident])
        P.dma("pool", causT[:], causT_d, ds_c, writes=[b_causT])
        ds_c2 = P.dsem("const2")
        P.dma("sp", cbias[:], cbias_d, ds_c2, writes=[b_cbias])
        P.dma("sp", pow2[:], pow2_d, ds_c2, writes=[b_pow2])

        gpre = sb("gpre", [128, D], F32); b_gpre = B("gpre")
        gple = sb("gple", [128, D], F32); b_gple = B("gple")
        ds_w = P.dsem("w")
        ds_v = P.dsem("v")
        xt = [sb("xt%d" % k, [128, D], F32) for k in range(2)]
        b_xt = [B("xt%d" % k) for k in range(2)]
        ds_x = [P.dsem("x%d" % k) for k in range(2)]
        pt = [sb("pt%d" % k, [128, 256], F32) for k in range(2)]
        b_pt = [B("pt%d" % k) for k in range(2)]
        ds_p = [P.dsem("p%d" % k) for k in range(2)]
        st = sb("st", [128, 32], F32); b_st = B("st")
        hn = sb("hn", [128, D], BF16); b_hn = B("hn")
        hnT = sb("hnT", [128, 8, 128], BF16); b_hnT = B("hnT")
        ptb = sb("ptb", [128, 256], BF16); b_ptb = B("ptb")
        pT = sb("pT", [128, 2, 128], BF16); b_pT = B("pT")
        b_bq = [B("bq%d" % k) for k in range(4)]
        b_tg = b_bq[0:2]
        b_t2 = b_bq[2:4]
        b_og = B("og")
        th = sb("th", [128, 2048], BF16); b_th = B("th")
        b_h1 = [[B("h1_%d_%d" % (s, i)) for i in range(NT)] for s in range(NS)]
        b_ogd = [[B("ogd_%d_%d" % (s, i)) for i in range(NT)] for s in range(NS)]

        def transposes(src, nchunk, pcol0, dst_view_fn, rb, wb):
            for g0 in range(0, nchunk, 4):
                g1 = min(nchunk, g0 + 4)
                c0 = pcol0 + g0 * 128
                c1 = pcol0 + g1 * 128
                banks = pbk(c0, c1)

                def mm(e, g0=g0, g1=g1, c0=c0):
                    ins = None
                    for c in range(g0, g1):
                        ins = e.matmul(PS[:, c0 + (c - g0) * 128: c0 + (c - g0 + 1) * 128],
                                       lhsT=src[:, c * 128:(c + 1) * 128], rhs=ident[:], start=True, stop=True)
                    return ins
                P.op("pe", mm, reads=rb + [b_ident], writes=banks)
                P.op("act", lambda e, g0=g0, g1=g1, c0=c0, c1=c1: e.activation(
                    out=dst_view_fn(g0, g1), in_=PS[:, c0:c1].rearrange("p (c t) -> p c t", t=128), func=AF.Copy),
                    reads=banks, writes=wb)

        def rstd_from_ss(col, n, scale):
            P.op("dve", lambda e: e.tensor_scalar(out=st[:, col:col + n], in0=st[:, col:col + n],
                                                  scalar1=scale, scalar2=EPS, op0=ALU.mult, op1=ALU.add),
                 reads=[b_st], writes=[b_st])
            newton_rsqrt(P, st[:, col:col + n], st[:, 28:28 + 3 * n], [b_st], n)

        def rmsnorm_T(xs, bx, gvec, bg):
            P.op("act", lambda e: e.activation(out=hn[:], in_=xs[:], func=AF.Square, accum_out=st[:, 0:1]),
                 reads=[bx], writes=[b_hn, b_st])
            rstd_from_ss(0, 1, 1.0 / D)
            P.op("dve", lambda e: e.scalar_tensor_tensor(out=hn[:], in0=xs[:], scalar=st[:, 0:1], in1=gvec[:],
                                                         op0=ALU.mult, op1=ALU.mult),
                 reads=[bx, b_st, bg], writes=[b_hn])
            transposes(hn, 8, 0, lambda g0, g1: hnT[:, g0:g1, :], [b_hn], [b_hnT])

        def load_tile(s, i, k, layer, src=None, rd=()):
            if src is None:
                src = x_d
            P.dma("sp", xt[k][:], src[s, i * 128:(i + 1) * 128, :], ds_x[k], reads=list(rd), writes=[b_xt[k]])
            P.dma("sp", pt[k][:], p_d[layer, s, i * 128:(i + 1) * 128, :], ds_p[k], writes=[b_pt[k]])

        def ple_stage(k, wpg, b_wpg, wp, b_wp, tg, t2):
            X = xt[k]; bX = b_xt[k]
            rmsnorm_T(X, bX, gple, b_gple)

            def mm_u(e):
                ins = None
                for nb in range(2):
                    for c in range(8):
                        ins = e.matmul(PS[:, 1024 + nb * 512:1024 + (nb + 1) * 512], lhsT=hnT[:, c, :],
                                       rhs=wpg[:, c, nb * 512:(nb + 1) * 512], start=(c == 0), stop=(c == 7))
                return ins
            P.op("pe", mm_u, reads=[b_hnT, b_wpg], writes=PB[2:4])
            P.op("act", lambda e: e.activation(out=ptb[:], in_=pt[k][:], func=AF.Copy),
                 reads=[b_pt[k]], writes=[b_ptb])
            transposes(ptb, 2, 3072, lambda g0, g1: pT[:, g0:g1, :], [b_ptb], [b_pT])

            def mm_pw(e):
                ins = None
                for nb in range(2):
                    for c in range(2):
                        ins = e.matmul(PS[:, 2048 + nb * 512:2048 + (nb + 1) * 512], lhsT=pT[:, c, :],
                                       rhs=wp[:, c, nb * 512:(nb + 1) * 512], start=(c == 0), stop=(c == 1))
                return ins
            P.op("pe", mm_pw, reads=[b_pT, b_wp], writes=PB[4:6])
            P.op("act", lambda e: e.activation(out=tg, in_=PS[:, 1024:2048], func=AF.Tanh, scale=0.5),
                 reads=PB[2:4], writes=b_tg)
            P.op("dve", lambda e: e.scalar_tensor_tensor(out=t2, in0=tg, scalar=1.0, in1=PS[:, 2048:3072],
                                                         op0=ALU.add, op1=ALU.mult),
                 reads=b_tg + PB[4:6], writes=b_t2)
            P.op("dve", lambda e: e.scalar_tensor_tensor(out=X[:], in0=t2, scalar=0.5, in1=X[:],
                                                         op0=ALU.mult, op1=ALU.add),
                 reads=b_t2 + [bX], writes=[bX])

        def layer0(es0):
            sb = lambda name, shape, dt: es0.enter_context(nc.sbuf_tensor("sb_" + name, list(shape), dt))
            w0in = sb("w0in", [128, 8, 1792], BF16); b_w0in = B("w0in")
            wq = sb("wq", [128, 3, 1024], BF16); b_wq = B("wq")
            wiq = sb("wiq", [128, 3, 512], BF16); b_wiq = B("wiq")
            wukT = sb("wukT", [128, 8, 256], BF16); b_wukT = B("wukT")
            wuv = sb("wuv", [128, 2, 1024], BF16); b_wuv = B("wuv")
            wout0 = sb("wout0", [128, 8, 1024], BF16); b_wout0 = B("wout0")
            wpg = sb("wpg0", [128, 8, 1024], BF16); b_wpg = B("wpg0")
            wp = sb("wp0", [128, 2, 1024], BF16); b_wp = B("wp0")
            vsm = sb("vsm", [128, 768], F32); b_vsm = B("vsm")
            P.dma("pool", w0in[:], w0in_d, ds_w, writes=[b_w0in])
            P.dma("sp", gpre[:], vec_d[0], ds_v, writes=[b_gpre])
            P.dma("sp", gple[:], vec_d[1], ds_v, writes=[b_gple])
            P.dma("sp", vsm[:], vsm_d, ds_v, writes=[b_vsm])
            P.dma("pool", wq[:], wq_d, ds_w, writes=[b_wq])
            P.dma("pool", wiq[:], wiq_d, ds_w, writes=[b_wiq])
            P.dma("pool", wukT[:], wukT_d, ds_w, writes=[b_wukT])
            P.dma("pool", wuv[:], wuv_d, ds_w, writes=[b_wuv])
            P.dma("pool", wout0[:], wout0_d, ds_w, writes=[b_wout0])
            P.dma("pool", wpg[:], wpg_d[0], ds_w, writes=[b_wpg])
            P.dma("pool", wp[:], wp_d[0], ds_w, writes=[b_wp])

            ckvT = sb("ckvT", [128, 2, L], BF16); b_ckvT = [B("ckvT%d" % j) for j in range(NT)]
            Vaug = sb("Vaug", [128, NT, 8, 129], BF16); b_Vaug = [B("Vaug%d" % j) for j in range(NT)]
            b_Vones = B("Vones")
            kxT = sb("kxT", [128, L], BF16); b_kxT = [B("kxT%d" % j) for j in range(NT)]
            hnF = sb("hnF", [128, D], BF16); b_hnF = B("hnF")
            hnTF_1 = sb("hnTF", [128, 8, 128], BF16); b_hnTF_1 = B("hnTF")
            hnTF2 = [hnTF_1, hnTF_1]; b_hnTF2 = [b_hnTF_1, b_hnTF_1]
            stF = sb("stF", [128, 32], F32); b_stF = B("stF")
            cqn = sb("cqn", [128, 384], BF16); b_cqn = B("cqn")
            cqT = sb("cqT", [128, 3, 128], BF16); b_cqT = B("cqT")
            ckvn = sb("ckvn", [128, 256], BF16); b_ckvn = B("ckvn")
            kxt = sb("kxt", [128, 64], F32); b_kxt = B("kxt")
            kx2 = sb("kx2", [128, 128], BF16); b_kx2 = B("kx2")
            wi2 = [sb("wi%d" % k, [128, 24], F32) for k in range(2)]; b_wi2 = [B("wi%d" % k) for k in range(2)]
            xt3 = xt + [sb("xt2a", [128, D], F32)]; b_xt3 = b_xt + [B("xt2a")]; ds_x3 = ds_x + [P.dsem("x2a")]
            qT_1 = sb("qT", [128, 8, 128], BF16); b_qT_1 = B("qT")
            qT2 = [qT_1, qT_1]; b_qT2 = [b_qT_1, b_qT_1]
            qiT2 = [sb("qiT%d" % k, [128, 4, 128], BF16) for k in range(2)]; b_qiT2 = [B("qiT%d" % k) for k in range(2)]
            acc = sb("acc", [128, L], F32); b_acc = B("acc")
            rl = [sb("rl%d" % k, [128, 512], F32) for k in range(2)]; b_rl = [B("rl%d" % k) for k in range(2)]
            bs = sb("bs", [128, 8 + KBIS], F32); b_bs = B("bs")
            qaT = [sb("qaT%d" % k, [128, 2, 8, 128], BF16) for k in range(2)]; b_qaT = [B("qaT%d" % k) for k in range(2)]
            mq1 = sb("maskq", [128, L], BF16); bmq1 = B("maskq")
            maskq = [mq1, mq1]; b_maskq = [bmq1, bmq1]
            thg = [th[:, 0:1024], th[:, 1024:2048]]; b_thg = [B("thg0"), B("thg1")]
            maskT = sb("maskT", [128, NT, 128], BF16); b_maskT = B("maskT")
            NE = 4
            Eb = [sb("Eb%d" % k, [128, 4, 128], BF16) for k in range(NE)]; b_Eb = [B("Eb%d" % k) for k in range(NE)]
            rden = sb("rden", [128, 4, 1], F32); b_rden = B("rden")
            big0 = sb("big0", [128, 1024], F32)
            on = big0[:, :]; b_on = b_bq[0:2]
            tg = big0[:, :]
            ogB = hn[:, :]
            P.op("dve", lambda e: e.memset(Vaug[:, :, :, 128:129], 1.0), writes=[b_Vones])

            def rstdF(col, n, scale):
                P.op("dve", lambda e: e.tensor_scalar(out=stF[:, col:col + n], in0=stF[:, col:col + n],
                                                      scalar1=scale, scalar2=EPS, op0=ALU.mult, op1=ALU.add),
                     reads=[b_stF], writes=[b_stF])
                newton_rsqrt(P, stF[:, col:col + n], stF[:, 16:16 + 3 * n], [b_stF], n)

            def proj(s, i, par, sl):
                X = xt3[sl]; bX = b_xt3[sl]
                qT = qT2[par]; b_qT = b_qT2[par]
                hnTF = hnTF2[par]; b_hnTF = b_hnTF2[par]
                WI = wi2[par]; bWI = b_wi2[par]
                QI = qiT2[par]; bQI = b_qiT2[par]
                P.op("act", lambda e: e.activation(out=hnF[:], in_=X[:], func=AF.Square, accum_out=stF[:, 0:1]),
                     reads=[bX], writes=[b_hnF, b_stF])
                rstdF(0, 1, 1.0 / D)
                P.op("dve", lambda e: e.scalar_tensor_tensor(out=hnF[:], in0=X[:], scalar=stF[:, 0:1], in1=gpre[:],
                                                             op0=ALU.mult, op1=ALU.mult),
                     reads=[bX, b_stF, b_gpre], writes=[b_hnF])
                yield 2.5
                transposes(hnF, 8, 0, lambda g0, g1: hnTF[:, g0:g1, :], [b_hnF], [b_hnTF])
                yield 2.5
                def mm_za(e):
                    ins = None
                    for (pc, wc, n) in ((0, 0, 512), (512, 512, 256)):
                        for c in range(8):
                            ins = e.matmul(PS[:, pc:pc + n], lhsT=hnTF[:, c, :], rhs=w0in[:, c, wc:wc + n],
                                           start=(c == 0), stop=(c == 7))
                    return ins
                P.op("pe", mm_za, reads=[b_hnTF, b_w0in], writes=PB[0:2])
                ZQ, ZK, ZW, ZKV = 0, 384, 448, 512
                P.op("act", lambda e: e.activation(out=cqn[:], in_=PS[:, ZQ:ZQ + 384], func=AF.Square,
                                                   accum_out=stF[:, 4:5]),
                     reads=[PB[0]], writes=[b_cqn, b_stF])
                P.op("act", lambda e: e.activation(out=ckvn[:], in_=PS[:, ZKV:ZKV + 256], func=AF.Square,
                                                   accum_out=stF[:, 5:6]),
                     reads=[PB[1]], writes=[b_ckvn, b_stF])
                P.op("act", lambda e: e.activation(out=kx2[:, 0:64], in_=PS[:, ZK:ZK + 64], func=AF.Square,
                                                   accum_out=stF[:, 6:7]),
                     reads=[PB[0]], writes=[b_kx2, b_stF])
                P.op("act", lambda e: e.activation(out=kx2[:, 64:128], in_=PS[:, ZK:ZK + 64], func=AF.Copy,
                                                   accum_out=stF[:, 7:8]),
                     reads=[PB[0]], writes=[b_kx2, b_stF])
                P.op("dve", lambda e: e.tensor_scalar(out=stF[:, 4:5], in0=stF[:, 4:5], scalar1=1.0 / 384,
                                                      scalar2=None, op0=ALU.mult), reads=[b_stF], writes=[b_stF])
                P.op("dve", lambda e: e.tensor_scalar(out=stF[:, 5:6], in0=stF[:, 5:6], scalar1=1.0 / 256,
                                                      scalar2=None, op0=ALU.mult), reads=[b_stF], writes=[b_stF])
                P.op("dve", lambda e: e.tensor_scalar(out=stF[:, 7:8], in0=stF[:, 7:8], scalar1=1.0 / 64,
                                                      scalar2=None, op0=ALU.mult), reads=[b_stF], writes=[b_stF])
                P.op("dve", lambda e: e.tensor_tensor(out=stF[:, 8:9], in0=stF[:, 7:8], in1=stF[:, 7:8],
                                                      op=ALU.mult), reads=[b_stF], writes=[b_stF])
                P.op("dve", lambda e: e.scalar_tensor_tensor(out=stF[:, 6:7], in0=stF[:, 6:7], scalar=1.0 / 64,
                                                             in1=stF[:, 8:9], op0=ALU.mult, op1=ALU.subtract),
                     reads=[b_stF], writes=[b_stF])
                rstdF(4, 3, 1.0)
                P.op("dve", lambda e: e.scalar_tensor_tensor(out=cqn[:], in0=PS[:, ZQ:ZQ + 384], scalar=stF[:, 4:5],
                                                             in1=vsm[:, 0:384], op0=ALU.mult, op1=ALU.mult),
                     reads=[PB[0], b_stF, b_vsm], writes=[b_cqn])
                P.op("dve", lambda e: e.scalar_tensor_tensor(out=ckvn[:], in0=PS[:, ZKV:ZKV + 256], scalar=stF[:, 5:6],
                                                             in1=vsm[:, 384:640], op0=ALU.mult, op1=ALU.mult),
                     reads=[PB[1], b_stF, b_vsm], writes=[b_ckvn])
                P.op("dve", lambda e: e.tensor_scalar(out=kxt[:], in0=PS[:, ZK:ZK + 64], scalar1=stF[:, 7:8],
                                                      scalar2=stF[:, 6:7], op0=ALU.subtract, op1=ALU.mult),
                     reads=[PB[0], b_stF], writes=[b_kxt])
                P.op("dve", lambda e: e.tensor_tensor(out=kxt[:], in0=kxt[:], in1=vsm[:, 640:704], op=ALU.mult),
                     reads=[b_kxt, b_vsm], writes=[b_kxt])
                P.op("dve", lambda e: e.tensor_tensor(out=kx2[:, 0:64], in0=kxt[:], in1=vsm[:, 704:768], op=ALU.add),
                     reads=[b_kxt, b_vsm], writes=[b_kx2])
                P.op("dve", lambda e: e.tensor_tensor(out=kx2[:, 64:128], in0=kxt[:], in1=vsm[:, 704:768], op=ALU.add),
                     reads=[b_kxt, b_vsm], writes=[b_kx2])
                P.op("dve", lambda e: e.tensor_scalar(out=WI[:, 0:8], in0=PS[:, ZW:ZW + 8],
                                                      scalar1=float(8 ** -0.5 * 64 ** -0.5), scalar2=None, op0=ALU.mult),
                     reads=[PB[0]], writes=[bWI])
                P.op("act", lambda e: e.activation(out=WI[:, 8:16], in_=WI[:, 0:8], func=AF.Abs),
                     reads=[bWI], writes=[bWI])
                P.op("act", lambda e: e.activation(out=WI[:, 16:24], in_=WI[:, 0:8], func=AF.Sign),
                     reads=[bWI], writes=[bWI])
                yield 7.5
                transposes(cqn, 3, 0, lambda g0, g1: cqT[:, g0:g1, :], [b_cqn], [b_cqT])
                transposes(ckvn, 2, 512, lambda g0, g1: ckvT[:, g0:g1, i * 128:(i + 1) * 128], [b_ckvn], [b_ckvT[i]])
                transposes(kx2, 1, 768, lambda g0, g1: kxT[:, i * 128:(i + 1) * 128].unsqueeze(1), [b_kx2], [b_kxT[i]])
                yield 2.5
                def mm_v(e):
                    ins = None
                    for nb in range(2):
                        for c in range(2):
                            ins = e.matmul(PS[:, nb * 512:(nb + 1) * 512], lhsT=ckvT[:, c, i * 128:(i + 1) * 128],
                                           rhs=wuv[:, c, nb * 512:(nb + 1) * 512], start=(c == 0), stop=(c == 1))
                    return ins
                P.op("pe", mm_v, reads=[b_ckvT[i], b_wuv], writes=PB[0:2])
                P.op("dve", lambda e: e.tensor_copy(out=Vaug[:, i, :, 0:128],
                                                    in_=PS[:, 0:1024].rearrange("p (h v) -> p h v", h=8)),
                     reads=PB[0:2], writes=[b_Vaug[i]])
                yield 2.5
                def mm_q(e):
                    ins = None
                    for h in range(8):
                        for c in range(3):
                            ins = e.matmul(PS[:, h * 128:(h + 1) * 128], lhsT=wq[:, c, h * 128:(h + 1) * 128],
                                           rhs=cqT[:, c, :], start=(c == 0), stop=(c == 2))
                    return ins
                P.op("pe", mm_q, reads=[b_wq, b_cqT], writes=PB[0:2])
                P.op("dve", lambda e: e.tensor_copy(out=qT[:], in_=PS[:, 0:1024].rearrange("p (h t) -> p h t", h=8)),
                     reads=PB[0:2], writes=[b_qT])
                yield 2.5
                def mm_qi(e):
                    ins = None
                    for hp in range(4):
                        for c in range(3):
                            ins = e.matmul(PS[:, hp * 128:(hp + 1) * 128], lhsT=wiq[:, c, hp * 128:(hp + 1) * 128],
                                           rhs=cqT[:, c, :], start=(c == 0), stop=(c == 2))
                    return ins
                P.op("pe", mm_qi, reads=[b_wiq, b_cqT], writes=[PB[0]])
                P.op("act", lambda e: e.activation(out=QI[:], in_=PS[:, 0:512].rearrange("p (h t) -> p h t", h=4),
                                                   func=AF.Copy),
                     reads=[PB[0]], writes=[bQI])
                yield 2.5

            def projB(s, i, par):
                QA = qaT[par]; bQA = b_qaT[par]
                TH = thg[par]; bTH = b_thg[par]
                qT = qT2[par]; b_qT = b_qT2[par]
                hnTF = hnTF2[par]; b_hnTF = b_hnTF2[par]
                for cc in range(2):
                    bk = 1 - cc

                    def mm_qa(e, cc=cc, bk=bk):
                        ins = None
                        for hh in range(8):
                            hq, hr = divmod(hh, 4)
                            ins = e.matmul(PS[:, hq * 512 + hr * 128: hq * 512 + (hr + 1) * 128],
                                           lhsT=wukT[:, hh, cc * 128:(cc + 1) * 128], rhs=qT[:, hh, :], start=True, stop=True)
                        return ins
                    P.op("pe", mm_qa, reads=[b_wukT, b_qT], writes=PB[0:2])
                    P.op("dve", lambda e, cc=cc: e.tensor_scalar(out=QA[:, cc, :, :],
                                                                 in0=PS[:, 0:1024].rearrange("p (h t) -> p h t", h=8),
                                                                 scalar1=float(128 ** -0.5), scalar2=None, op0=ALU.mult),
                         reads=PB[0:2], writes=[bQA])
                    yield 2.5
                def mm_zb(e):
                    ins = None
                    for nb in range(2):
                        for c in range(8):
                            ins = e.matmul(PS[:, nb * 512:(nb + 1) * 512], lhsT=hnTF[:, c, :],
                                           rhs=w0in[:, c, 768 + nb * 512:768 + (nb + 1) * 512],
                                           start=(c == 0), stop=(c == 7))
                    return ins
                P.op("pe", mm_zb, reads=[b_hnTF, b_w0in], writes=PB[0:2])
                P.op("act", lambda e: e.activation(out=TH, in_=PS[:, 0:1024], func=AF.Tanh, scale=0.5),
                     reads=PB[0:2], writes=[bTH])
                P.op("dve", lambda e: e.scalar_tensor_tensor(out=TH, in0=TH, scalar=1.0, in1=PS[:, 0:1024],
                                                             op0=ALU.add, op1=ALU.mult),
                     reads=[bTH] + PB[0:2], writes=[bTH])
                yield 2.5

            def idxbis(s, i, par):
                nk = (i + 1) * 128
                MQ = maskq[par]; bMQ = b_maskq[par]
                WI = wi2[par]; bWI = b_wi2[par]
                QI = qiT2[par]; bQI = b_qiT2[par]
                nkb = (nk + 511) // 512
                for kb in range(nkb):
                    k0 = kb * 512
                    wd = min(512, nk - k0)
                    kbufs = b_kxT[k0 // 128:(k0 + wd) // 128]
                    for h in range(8):
                        bank = h % 2
                        r = h % 2
                        pr = (h % 2) * 64
                        P.op("pe", lambda e, h=h, bank=bank, pr=pr, k0=k0, wd=wd: e.matmul(
                            PS[:, bank * 512:bank * 512 + wd], lhsT=QI[pr:pr + 64, h // 2, :],
                            rhs=kxT[pr:pr + 64, k0:k0 + wd], start=True, stop=True),
                            reads=[bQI] + kbufs, writes=[PB[bank]])
                        P.op("act", lambda e, h=h, bank=bank, r=r, wd=wd: e.activation(
                            out=rl[r][:, 0:wd], in_=PS[:, bank * 512:bank * 512 + wd], func=AF.Relu,
                            scale=WI[:, 8 + h:9 + h]),
                            reads=[PB[bank], bWI], writes=[b_rl[r]])
                        if h == 0:
                            P.op("dve", lambda e, r=r, k0=k0, wd=wd: e.tensor_scalar(
                                out=acc[:, k0:k0 + wd], in0=rl[r][:, 0:wd], scalar1=WI[:, 16:17], scalar2=None,
                                op0=ALU.mult), reads=[b_rl[r], bWI], writes=[b_acc])
                        else:
                            P.op("dve", lambda e, h=h, r=r, k0=k0, wd=wd: e.scalar_tensor_tensor(
                                out=acc[:, k0:k0 + wd], in0=rl[r][:, 0:wd], scalar=WI[:, 16 + h:17 + h],
                                in1=acc[:, k0:k0 + wd], op0=ALU.mult, op1=ALU.add),
                                reads=[b_rl[r], bWI, b_acc], writes=[b_acc])
                        yield 0.7
                LO, HI, WW, THR, CAND, CNT, MM = 0, 1, 2, 3, 4, 5, 6
                P.op("dve", lambda e: e.tensor_reduce(out=bs[:, LO:LO + 1], in_=acc[:, 0:nk], axis=AX.X, op=ALU.min),
                     reads=[b_acc], writes=[b_bs])
                P.op("dve", lambda e: e.tensor_reduce(out=bs[:, HI:HI + 1], in_=acc[:, 0:nk], axis=AX.X, op=ALU.max),
                     reads=[b_acc], writes=[b_bs])
                P.op("dve", lambda e: e.tensor_tensor(out=acc[:, i * 128:(i + 1) * 128], in0=acc[:, i * 128:(i + 1) * 128],
                                                      in1=cbias[:], op=ALU.add),
                     reads=[b_acc, b_cbias], writes=[b_acc])
                P.op("dve", lambda e: e.tensor_copy(out=bs[:, THR:THR + 1], in_=bs[:, LO:LO + 1]),
                     reads=[b_bs], writes=[b_bs])
                yield 2.5
                if nk > TOPK:
                    NTHR, NCAND = WW, CAND
                    P.op("dve", lambda e: e.tensor_tensor(out=bs[:, 7:8], in0=bs[:, HI:HI + 1], in1=bs[:, LO:LO + 1],
                                                          op=ALU.subtract), reads=[b_bs], writes=[b_bs])
                    P.op("dve", lambda e: e.tensor_scalar(out=bs[:, 8:8 + KBIS], in0=pow2[:], scalar1=bs[:, 7:8],
                                                          scalar2=None, op0=ALU.mult),
                         reads=[b_bs, b_pow2], writes=[b_bs])
                    P.op("dve", lambda e: e.tensor_scalar(out=bs[:, NTHR:NTHR + 1], in0=bs[:, LO:LO + 1], scalar1=-1.0,
                                                          scalar2=None, op0=ALU.mult), reads=[b_bs], writes=[b_bs])
                    for kk in range(KBIS):
                        P.op("dve", lambda e, kk=kk: e.tensor_scalar(out=bs[:, NCAND:NCAND + 1], in0=bs[:, NTHR:NTHR + 1],
                                                                     scalar1=bs[:, 8 + kk:9 + kk], scalar2=None,
                                                                     op0=ALU.subtract),
                             reads=[b_bs], writes=[b_bs])
                        P.op("act", lambda e: e.activation(out=MQ[:, 0:nk], in_=acc[:, 0:nk], func=AF.Sign,
                                                           bias=bs[:, NCAND:NCAND + 1], accum_out=bs[:, CNT:CNT + 1]),
                             reads=[b_acc, b_bs], writes=[bMQ, b_bs])
                        P.op("dve", lambda e: e.tensor_scalar(out=bs[:, MM:MM + 1], in0=bs[:, CNT:CNT + 1],
                                                              scalar1=float(2 * TOPK - 1 - nk), scalar2=1e30,
                                                              op0=ALU.is_lt, op1=ALU.mult),
                             reads=[b_bs], writes=[b_bs])
                        P.op("dve", lambda e: e.scalar_tensor_tensor(out=bs[:, NTHR:NTHR + 1], in0=bs[:, NCAND:NCAND + 1],
                                                                     scalar=bs[:, MM:MM + 1], in1=bs[:, NTHR:NTHR + 1],
                                                                     op0=ALU.add, op1=ALU.min),
                             reads=[b_bs], writes=[b_bs])
                        yield 1.5 + (nk + 224) / 1200.0
                    P.op("dve", lambda e: e.tensor_scalar(out=bs[:, THR:THR + 1], in0=bs[:, NTHR:NTHR + 1], scalar1=-1.0,
                                                          scalar2=None, op0=ALU.mult), reads=[b_bs], writes=[b_bs])
                P.op("dve", lambda e: e.tensor_scalar(out=MQ[:, 0:nk], in0=acc[:, 0:nk], scalar1=bs[:, THR:THR + 1],
                                                      scalar2=None, op0=ALU.is_ge),
                     reads=[b_acc, b_bs], writes=[bMQ])
                yield 2.5

            SBK = [4, 5, 6, 7]

            def back(s, i, par, sl):
                X = xt3[sl]; bX = b_xt3[sl]
                QA = qaT[par]; bQA = b_qaT[par]
                MQ = maskq[par]; bMQ = b_maskq[par]
                TH = thg[par]; bTH = b_thg[par]
                for gi, g0 in enumerate(range(0, i + 1, 4)):
                    g1 = min(i + 1, g0 + 4)
                    bank = SBK[gi % 4]

                    def mm_mt(e, g0=g0, g1=g1, bank=bank):
                        ins = None
                        for j in range(g0, g1):
                            ins = e.matmul(PS[:, bank * 512 + (j - g0) * 128: bank * 512 + (j - g0 + 1) * 128],
                                           lhsT=MQ[:, j * 128:(j + 1) * 128], rhs=ident[:], start=True, stop=True)
                        return ins
                    P.op("pe", mm_mt, reads=[bMQ, b_ident], writes=[PB[bank]])
                    P.op("act", lambda e, g0=g0, g1=g1, bank=bank: e.activation(
                        out=maskT[:, g0:g1, :],
                        in_=PS[:, bank * 512: bank * 512 + (g1 - g0) * 128].rearrange("p (j t) -> p j t", t=128),
                        func=AF.Copy), reads=[PB[bank]], writes=[b_maskT])
                    yield 1.0
                OV = PS[:, 1024:2048].rearrange("p (h w) -> p h w", h=4)
                for hf in range(2):
                    N = i + 1

                    def score_step(n, hf=hf):
                        u = n % NE
                        bank = SBK[u]

                        def mm_s(e, j=n, hf=hf, bank=bank):
                            ins = None
                            for cc in range(2):
                                ins = e.matmul(PS[:, bank * 512:(bank + 1) * 512], lhsT=ckvT[:, cc, j * 128:(j + 1) * 128],
                                               rhs=QA[:, cc, 4 * hf:4 * hf + 4, :], start=(cc == 0), stop=(cc == 1))
                            return ins
                        P.op("pe", mm_s, reads=[b_ckvT[n], bQA], writes=[PB[bank]])
                        P.op("act", lambda e, u=u, bank=bank: e.activation(
                            out=Eb[u][:], in_=PS[:, bank * 512:(bank + 1) * 512].rearrange("p (h t) -> p h t", h=4),
                            func=AF.Exp), reads=[PB[bank]], writes=[b_Eb[u]])
                        P.op("dve", lambda e, u=u, j=n: e.tensor_tensor(
                            out=Eb[u][:], in0=Eb[u][:], in1=maskT[:, j:j + 1, :].to_broadcast([128, 4, 128]), op=ALU.mult),
                            reads=[b_Eb[u], b_maskT], writes=[b_Eb[u]])

                    def pv_step(n, hf=hf):
                        u = n % NE

                        def mm_pv(e, j=n, hf=hf, u=u):
                            ins = None
                            for hl in range(4):
                                h = 4 * hf + hl
                                ins = e.matmul(PS[:, 1024 + hl * 256:1024 + hl * 256 + 129], lhsT=Eb[u][:, hl, :],
                                               rhs=Vaug[:, j, h, :], start=(j == 0 and hl % 2 == 0), stop=(j == i),
                                               skip_group_check=True)
                            return ins
                        P.op("pe", mm_pv, reads=[b_Eb[u], b_Vaug[n], b_Vones], writes=PB[2:4])

                    for n in range(min(NE - 1, N)):
                        score_step(n)
                    yield 2.5
                    for n in range(N):
                        if n + NE - 1 < N:
                            score_step(n + NE - 1)
                        pv_step(n)
                        yield 0.85
                    P.op("dve", lambda e: e.reciprocal(out=rden[:], in_=OV[:, :, 128:129]), reads=PB[2:4], writes=[b_rden])
                    P.op("dve", lambda e, hf=hf: e.tensor_tensor(
                        out=on[:, hf * 512:(hf + 1) * 512].rearrange("p (h v) -> p h v", h=4), in0=OV[:, :, 0:128],
                        in1=rden[:].to_broadcast([128, 4, 128]), op=ALU.mult),
                        reads=PB[2:4] + [b_rden], writes=[b_on[hf]])
                    yield 0.85
                P.op("dve", lambda e: e.scalar_tensor_tensor(out=ogB, in0=on, scalar=0.5, in1=TH,
                                                             op0=ALU.mult, op1=ALU.mult),
                     reads=b_on + [bTH], writes=[b_hn])
                transposes(hn, 8, 2048, lambda g0, g1: hnT[:, g0:g1, :], [b_hn], [b_hnT])
                yield 2.5

                def mm_y(e):
                    ins = None
                    for nb in range(2):
                        for c in range(8):
                            ins = e.matmul(PS[:, 3072 + nb * 512:3072 + (nb + 1) * 512], lhsT=hnT[:, c, :],
                                           rhs=wout0[:, c, nb * 512:(nb + 1) * 512], start=(c == 0), stop=(c == 7))
                    return ins
                P.op("pe", mm_y, reads=[b_hnT, b_wout0], writes=PB[6:8])
                P.op("dve", lambda e: e.tensor_tensor(out=X[:], in0=X[:], in1=PS[:, 3072:4096], op=ALU.add),
                     reads=[bX] + PB[6:8], writes=[bX])
                yield 2.5
                P.op("act", lambda e: e.activation(out=hn[:], in_=X[:], func=AF.Square, accum_out=st[:, 0:1]),
                     reads=[bX], writes=[b_hn, b_st])
                P.op("dve", lambda e: e.tensor_scalar(out=st[:, 0:1], in0=st[:, 0:1], scalar1=1.0 / D, scalar2=EPS,
                                                      op0=ALU.mult, op1=ALU.add), reads=[b_st], writes=[b_st])
                newton_rsqrt(P, st[:, 0:1], st[:, 28:31], [b_st], 1)
                P.op("dve", lambda e: e.scalar_tensor_tensor(out=hn[:], in0=X[:], scalar=st[:, 0:1], in1=gple[:],
                                                             op0=ALU.mult, op1=ALU.mult),
                     reads=[bX, b_st, b_gple], writes=[b_hn])
                transposes(hn, 8, 2048, lambda g0, g1: hnT[:, g0:g1, :], [b_hn], [b_hnT])
                yield 2.5

                def mm_u(e):
                    ins = None
                    for nb in range(2):
                        for c in range(8):
                            ins = e.matmul(PS[:, 1024 + nb * 512:1024 + (nb + 1) * 512], lhsT=hnT[:, c, :],
                                           rhs=wpg[:, c, nb * 512:(nb + 1) * 512], start=(c == 0), stop=(c == 7))
                    return ins
                P.op("pe", mm_u, reads=[b_hnT, b_wpg], writes=PB[2:4])
                P.op("act", lambda e: e.activation(out=ptb[:], in_=pt[par][:], func=AF.Copy),
                     reads=[b_pt[par]], writes=[b_ptb])
                transposes(ptb, 2, 3072, lambda g0, g1: pT[:, g0:g1, :], [b_ptb], [b_pT])
                yield 2.5

                def mm_pw(e):
                    ins = None
                    for nb in range(2):
                        for c in range(2):
                            ins = e.matmul(PS[:, 3072 + nb * 512:3072 + (nb + 1) * 512], lhsT=pT[:, c, :],
                                           rhs=wp[:, c, nb * 512:(nb + 1) * 512], start=(c == 0), stop=(c == 1))
                    return ins
                P.op("pe", mm_pw, reads=[b_pT, b_wp], writes=PB[6:8])
                P.op("act", lambda e: e.activation(out=tg, in_=PS[:, 1024:2048], func=AF.Tanh, scale=0.5),
                     reads=PB[2:4], writes=b_tg)
                P.op("dve", lambda e: e.scalar_tensor_tensor(out=tg, in0=tg, scalar=1.0, in1=PS[:, 3072:4096],
                                                             op0=ALU.add, op1=ALU.mult),
                     reads=b_tg + PB[6:8], writes=b_tg)
                P.op("dve", lambda e: e.scalar_tensor_tensor(out=X[:], in0=tg, scalar=0.5, in1=X[:],
                                                             op0=ALU.mult, op1=ALU.add),
                     reads=b_tg + [bX], writes=[bX])
                if mode == "full":
                    P.dma("sp", h1_d[s, i * 128:(i + 1) * 128, :], X[:], ds_x3[sl], reads=[bX], writes=[b_h1[s][i]])
                else:
                    P.dma("sp", out_d[s, i * 128:(i + 1) * 128, :], X[:], ds_x3[sl], reads=[bX])
                yield 2.5

            items = [(s, i) for s in range(NS) for i in range(NT)]

            def run_all(g):
                for _ in g:
                    pass

            def interleave3(gl):
                gens = [[g, 0.0, t] for (g, t) in gl]
                while gens:
                    g = min(gens, key=lambda x: x[1] / x[2])
                    try:
                        c = next(g[0])
                        g[1] += (c if c else 1.0)
                    except StopIteration:
                        gens.remove(g)

            def idxbis_total(i):
                nk = (i + 1) * 128
                t = 5.0 + 0.7 * 8 * ((nk + 511) // 512)
                if nk > TOPK:
                    t += KBIS * (1.5 + (nk + 224) / 1200.0)
                return t

            def back_total(i):
                return 1.0 * ((i + 4) // 4) + 2 * (0.85 * (i + 2) + 2.5) + 2.5 * 5

            PROJ_T = 32.0

            def loadx(g):
                s_, i_ = items[g]
                P.dma("sp", xt3[g % 3][:], x_d[s_, i_ * 128:(i_ + 1) * 128, :], ds_x3[g % 3], writes=[b_xt3[g % 3]])

            def loadp(g):
                s_, i_ = items[g]
                P.dma("sp", pt[g % 2][:], p_d[0, s_, i_ * 128:(i_ + 1) * 128, :], ds_p[g % 2], writes=[b_pt[g % 2]])

            NI = len(items)
            for g in range(min(3, NI)):
                loadx(g)
            for g in range(min(2, NI)):
                loadp(g)
            for s_ in range(NS):
                g0 = s_ * NT
                run_all(proj(s_, 0, g0 % 2, g0 % 3))
                run_all(projB(s_, 0, g0 % 2))
                gl = [(idxbis(s_, 0, g0 % 2), idxbis_total(0))]
                if NT > 1:
                    gl.append((proj(s_, 1, (g0 + 1) % 2, (g0 + 1) % 3), PROJ_T))
                interleave3(gl)
                for m in range(NT):
                    g = g0 + m
                    if m + 1 < NT:
                        run_all(projB(s_, m + 1, (g + 1) % 2))
                    gl = [(back(s_, m, g % 2, g % 3), back_total(m))]
                    if m + 1 < NT:
                        gl.append((idxbis(s_, m + 1, (g + 1) % 2), idxbis_total(m + 1)))
                    if m + 2 < NT:
                        gl.append((proj(s_, m + 2, (g + 2) % 2, (g + 2) % 3), PROJ_T))
                    interleave3(gl)
                    if g + 3 < NI:
                        loadx(g + 3)
                    if g + 2 < NI:
                        loadp(g + 2)

        def layer1a(es1):
            sb = lambda name, shape, dt: es1.enter_context(nc.sbuf_tensor("sb_" + name, list(shape), dt))
            w1in = sb("w1in", [128, 8, 6144], BF16); b_w1in = [B("w1in%d" % c) for c in range(3)]
            for c in range(3):
                P.dma("pool", w1in[:, :, c * 2048:(c + 1) * 2048], w1in_d[:, :, c * 2048:(c + 1) * 2048], ds_w,
                      writes=[b_w1in[c]])
            P.dma("sp", gpre[:], vec_d[2], ds_v, writes=[b_gpre])
            rot = [sb("rot%d" % k, [128, 4, 4, 128], F32) for k in range(2)]
            b_rot = [B("rot%d" % k) for k in range(2)]
            ds_r = [P.dsem("r%d" % k) for k in range(2)]
            ds_g = [P.dsem("og%d" % k) for k in range(2)]
            Uh = sb("Uh", [128, 2, 4, 512], F32); b_Uh = B("Uh")
            Ubf = sb("Ubf", [128, 2, 4, 512], BF16); b_Ubf = B("Ubf")
            qr = [sb("qr%d" % k, [128, 1024], BF16) for k in range(2)]; b_qr = [B("qr%d" % k) for k in range(2)]
            kr = [sb("kr%d" % k, [128, 1024], BF16) for k in range(2)]; b_kr = [B("kr%d" % k) for k in range(2)]
            vb = [sb("vb%d" % k, [128, 2048], BF16) for k in range(2)]; b_vb = [B("vb%d" % k) for k in range(2)]
            thb = [th, sb("thb1", [128, 2048], BF16)]; b_thb = [b_th, B("thb1")]
            ogb = [sb("ogb%d" % k, [128, 2048], BF16) for k in range(2)]; b_ogb = [B("ogb%d" % k) for k in range(2)]
            qkT = sb("qkT", [128, 16, 128], BF16); b_qkT = B("qkT")
            rtm = sb("rtm", [128, 2048], F32); b_rt = [B("rtm%d" % k) for k in range(4)]
            rt_ = [rtm[:, q * 512:(q + 1) * 512] for q in range(4)]
            stB = sb("stB", [128, 48], F32); b_stB = B("stB")
            sTm = sb("sTm", [128, 4, 128], BF16); b_sTm = B("sTm")

            def front(s, i, par):
                X = xt[par]; bX = b_xt[par]
                R = rot[par]; bR = b_rot[par]
                P.op("act", lambda e: e.activation(out=hn[:], in_=X[:], func=AF.Square, accum_out=st[:, 0:1]),
                     reads=[bX], writes=[b_hn, b_st])
                P.op("dve", lambda e: e.tensor_scalar(out=st[:, 0:1], in0=st[:, 0:1], scalar1=1.0 / D, scalar2=EPS,
                                                      op0=ALU.mult, op1=ALU.add), reads=[b_st], writes=[b_st])
                newton_rsqrt(P, st[:, 0:1], st[:, 28:31], [b_st], 1)
                P.op("dve", lambda e: e.scalar_tensor_tensor(out=hn[:], in0=X[:], scalar=st[:, 0:1], in1=gpre[:],
                                                             op0=ALU.mult, op1=ALU.mult),
                     reads=[bX, b_st, b_gpre], writes=[b_hn])
                yield 4.0
                transposes(hn, 8, 0, lambda g0, g1: hnT[:, g0:g1, :], [b_hn], [b_hnT])
                yield 2.0

                def mm_in(e, col0, nb):
                    ins = None
                    for c in range(8):
                        ins = e.matmul(PS[:, nb * 512:(nb + 1) * 512], lhsT=hnT[:, c, :],
                                       rhs=w1in[:, c, col0 + nb * 512:col0 + (nb + 1) * 512],
                                       start=(c == 0), stop=(c == 7))
                    return ins
                for nb in range(4):
                    P.op("pe", lambda e, nb=nb: mm_in(e, 0, nb), reads=[b_hnT, b_w1in[0]], writes=[PB[nb]])

                def v4(t):
                    return t.rearrange("p (h m) -> p h m", h=4)
                for (zc0, ti, dstt, bd) in ((0, 0, qr[par], b_qr[par]), (1024, 2, kr[par], b_kr[par])):
                    Z = PS[:, zc0:zc0 + 1024].rearrange("p (h m two) -> p h m two", h=4, two=2)
                    Dv = dstt[:].rearrange("p (h m two) -> p h m two", h=4, two=2)
                    C = R[:, ti, :, :]
                    S = R[:, ti + 1, :, :]
                    zb = pbk(zc0, zc0 + 1024)
                    P.op("dve", lambda e, Z=Z, C=C: e.tensor_tensor(out=v4(rt_[0]), in0=Z[:, :, :, 0], in1=C, op=ALU.mult),
                         reads=zb + [bR], writes=[b_rt[0]])
                    P.op("dve", lambda e, Z=Z, S=S: e.tensor_tensor(out=v4(rt_[1]), in0=Z[:, :, :, 1], in1=S, op=ALU.mult),
                         reads=zb + [bR], writes=[b_rt[1]])
                    P.op("dve", lambda e, Dv=Dv: e.tensor_tensor(out=Dv[:, :, :, 0], in0=v4(rt_[0]), in1=v4(rt_[1]),
                                                                 op=ALU.subtract),
                         reads=[b_rt[0], b_rt[1]], writes=[bd])
                    P.op("dve", lambda e, Z=Z, C=C: e.tensor_tensor(out=v4(rt_[2]), in0=Z[:, :, :, 1], in1=C, op=ALU.mult),
                         reads=zb + [bR], writes=[b_rt[2]])
                    P.op("dve", lambda e, Z=Z, S=S: e.tensor_tensor(out=v4(rt_[3]), in0=Z[:, :, :, 0], in1=S, op=ALU.mult),
                         reads=zb + [bR], writes=[b_rt[3]])
                    P.op("dve", lambda e, Dv=Dv: e.tensor_tensor(out=Dv[:, :, :, 1], in0=v4(rt_[2]), in1=v4(rt_[3]),
                                                                 op=ALU.add),
                         reads=[b_rt[2], b_rt[3]], writes=[bd])
                yield 14.0
                for nb in range(4):
                    P.op("pe", lambda e, nb=nb: mm_in(e, 2048, nb), reads=[b_hnT, b_w1in[1]], writes=[PB[nb]])
                    P.op("act", lambda e, nb=nb: e.activation(out=vb[par][:, nb * 512:(nb + 1) * 512],
                                                              in_=PS[:, nb * 512:(nb + 1) * 512], func=AF.Copy),
                         reads=[PB[nb]], writes=[b_vb[par]])
                    yield 1.7
                for nb in range(4):
                    P.op("pe", lambda e, nb=nb: mm_in(e, 4096, nb), reads=[b_hnT, b_w1in[2]], writes=[PB[nb]])
                    P.op("act", lambda e, nb=nb: e.activation(out=thb[par][:, nb * 512:(nb + 1) * 512],
                                                              in_=PS[:, nb * 512:(nb + 1) * 512], func=AF.Tanh, scale=0.5),
                         reads=[PB[nb]], writes=[b_thb[par]])
                    P.op("dve", lambda e, nb=nb: e.scalar_tensor_tensor(
                        out=thb[par][:, nb * 512:(nb + 1) * 512], in0=thb[par][:, nb * 512:(nb + 1) * 512], scalar=1.0,
                        in1=PS[:, nb * 512:(nb + 1) * 512], op0=ALU.add, op1=ALU.mult),
                        reads=[b_thb[par], PB[nb]], writes=[b_thb[par]])
                    yield 1.7

            def back(s, i, par):
                QR = qr[par]; KR = kr[par]; VB = vb[par]; TH = thb[par]; OG = ogb[par]
                bQR = b_qr[par]; bKR = b_kr[par]; bVB = b_vb[par]; bTH = b_thb[par]; bOG = b_ogb[par]
                transposes(QR, 8, 2048, lambda g0, g1: qkT[:, g0:g1, :], [bQR], [b_qkT])
                yield 2.0
                transposes(KR, 8, 3072, lambda g0, g1: qkT[:, 8 + g0:8 + g1, :], [bKR], [b_qkT])
                yield 2.0
                def mm_st(e):
                    ins = None
                    for h in range(4):
                        for dc in range(2):
                            ins = e.matmul(PS[:, 2048 + h * 128:2048 + (h + 1) * 128], lhsT=qkT[:, 8 + 2 * h + dc, :],
                                           rhs=qkT[:, 2 * h + dc, :], start=(dc == 0), stop=(dc == 1))
                    return ins
                P.op("pe", mm_st, reads=[b_qkT], writes=[PB[4]])
                P.op("dve", lambda e: e.tensor_tensor(out=sTm[:], in0=PS[:, 2048:2560].rearrange("p (h t) -> p h t", h=4),
                                                      in1=causT[:].unsqueeze(1).to_broadcast([128, 4, 128]), op=ALU.mult),
                     reads=[PB[4], b_causT], writes=[b_sTm])
                yield 1.5
                for h in range(4):
                    def mm_o(e, h=h):
                        ins = e.matmul(PS[:, 2048 + h * 512:2048 + (h + 1) * 512], lhsT=sTm[:, h, :],
                                       rhs=VB[:, h * 512:(h + 1) * 512], start=True, stop=(i == 0))
                        if i > 0:
                            for dc in range(2):
                                ins = e.matmul(PS[:, 2048 + h * 512:2048 + (h + 1) * 512], lhsT=qkT[:, 2 * h + dc, :],
                                               rhs=Ubf[:, dc, h, :], start=False, stop=(dc == 1))
                        return ins
                    P.op("pe", mm_o, reads=[b_sTm, bVB, b_qkT] + ([b_Ubf] if i > 0 else []), writes=[PB[4 + h]])
                    oc = 2048 + h * 512
                    P.op("act", lambda e, h=h, oc=oc: e.activation(out=OG[:, h * 512:(h + 1) * 512], in_=PS[:, oc:oc + 512],
                                                                   func=AF.Copy, accum_out=stB[:, 12 + h:13 + h]),
                         reads=[PB[4 + h]], writes=[bOG, b_stB])
                    P.op("act", lambda e, h=h, oc=oc: e.activation(out=OG[:, h * 512:(h + 1) * 512], in_=PS[:, oc:oc + 512],
                                                                   func=AF.Square, accum_out=stB[:, 16 + h:17 + h]),
                         reads=[PB[4 + h]], writes=[bOG, b_stB])
                    yield 1.5
                if i + 1 < NT:
                    for h in range(4):
                        pc = (h % 2) * 1024

                        def mm_d(e, h=h, pc=pc):
                            ins = None
                            for dc in range(2):
                                ins = e.matmul(PS[:, pc + dc * 512:pc + (dc + 1) * 512],
                                               lhsT=KR[:, h * 256 + dc * 128:h * 256 + (dc + 1) * 128],
                                               rhs=VB[:, h * 512:(h + 1) * 512], start=True, stop=True)
                            return ins
                        P.op("pe", mm_d, reads=[bKR, bVB], writes=pbk(pc, pc + 1024))
                        Dl = PS[:, pc:pc + 1024].rearrange("p (c v) -> p c v", c=2)
                        if i == 0:
                            P.op("dve", lambda e, h=h, Dl=Dl: e.tensor_copy(out=Uh[:, :, h, :], in_=Dl),
                                 reads=pbk(pc, pc + 1024), writes=[b_Uh])
                        else:
                            P.op("dve", lambda e, h=h, Dl=Dl: e.scalar_tensor_tensor(
                                out=Uh[:, :, h, :], in0=Uh[:, :, h, :], scalar=float(gch[h]), in1=Dl,
                                op0=ALU.mult, op1=ALU.add), reads=pbk(pc, pc + 1024) + [b_Uh], writes=[b_Uh])
                        P.op("act", lambda e, h=h: e.activation(out=Ubf[:, :, h, :], in_=Uh[:, :, h, :], func=AF.Copy,
                                                                scale=float(gch[h])),
                             reads=[b_Uh], writes=[b_Ubf])
                        yield 2.5

                P.op("dve", lambda e: e.tensor_scalar(out=stB[:, 12:16], in0=stB[:, 12:16], scalar1=1.0 / 512, scalar2=None,
                                                      op0=ALU.mult), reads=[b_stB], writes=[b_stB])
                P.op("dve", lambda e: e.tensor_tensor(out=stB[:, 20:24], in0=stB[:, 12:16], in1=stB[:, 12:16], op=ALU.mult),
                     reads=[b_stB], writes=[b_stB])
                P.op("dve", lambda e: e.scalar_tensor_tensor(out=stB[:, 16:20], in0=stB[:, 16:20], scalar=1.0 / 512,
                                                             in1=stB[:, 20:24], op0=ALU.mult, op1=ALU.subtract),
                     reads=[b_stB], writes=[b_stB])
                P.op("dve", lambda e: e.tensor_scalar(out=stB[:, 16:20], in0=stB[:, 16:20], scalar1=1.0, scalar2=EPS,
                                                      op0=ALU.mult, op1=ALU.add), reads=[b_stB], writes=[b_stB])
                newton_rsqrt(P, stB[:, 16:20], stB[:, 32:44], [b_stB], 4)
                P.op("dve", lambda e: e.scalar_tensor_tensor(out=stB[:, 24:28], in0=stB[:, 12:16], scalar=-1.0, in1=stB[:, 16:20],
                                                             op0=ALU.mult, op1=ALU.mult), reads=[b_stB], writes=[b_stB])
                yield 5.0
                for h in range(4):
                    oc = 2048 + h * 512
                    P.op("act", lambda e, h=h, oc=oc: e.activation(out=PS[:, oc:oc + 512], in_=PS[:, oc:oc + 512],
                                                                   func=AF.Identity, scale=stB[:, 16 + h:17 + h],
                                                                   bias=stB[:, 24 + h:25 + h]),
                         reads=[PB[4 + h], b_stB], writes=[PB[4 + h]])
                    P.op("dve", lambda e, h=h, oc=oc: e.scalar_tensor_tensor(
                        out=OG[:, h * 512:(h + 1) * 512], in0=PS[:, oc:oc + 512], scalar=0.5,
                        in1=TH[:, h * 512:(h + 1) * 512], op0=ALU.mult, op1=ALU.mult),
                        reads=[PB[4 + h], bTH], writes=[bOG])
                    yield 1.2
                if mode == "l1a":
                    P.op("act", lambda e: e.activation(out=xt[par][:], in_=OG[:, 0:1024], func=AF.Copy), reads=[bOG], writes=[b_xt[par]])
                    P.dma("sp", out_d[s, i * 128:(i + 1) * 128, :], xt[par][:], ds_x[par], reads=[b_xt[par]])
                else:
                    P.dma("sp", og_d[s, i * 128:(i + 1) * 128, :], OG[:], ds_g[par], reads=[bOG], writes=[b_ogd[s][i]])
            src = h1_d if mode == "full" else x_d
            items = [(s, i) for s in range(NS) for i in range(NT)]

            def load1(n, k):
                s, i = items[n]
                load_tile(s, i, k, 1, src, [b_h1[s][i]] if mode == "full" else [])
                P.dma("sp", rot[k][:], rot_d[i], ds_r[k], writes=[b_rot[k]])

            def run_all(g):
                for _ in g:
                    pass

            def interleave(ga, gb):
                gens = [[ga, 0.0, 34.0], [gb, 0.0, 37.0]]
                while gens:
                    g = min(gens, key=lambda x: x[1] / x[2])
                    try:
                        c = next(g[0])
                        g[1] += (c if c else 1.0)
                    except StopIteration:
                        gens.remove(g)

            load1(0, 0)
            if len(items) > 1:
                load1(1, 1)
            run_all(front(items[0][0], items[0][1], 0))
            for n in range(len(items)):
                s, i = items[n]
                if n + 2 < len(items):
                    load1(n + 2, n % 2)
                gb = back(s, i, n % 2)
                if n + 1 < len(items):
                    interleave(gb, front(items[n + 1][0], items[n + 1][1], (n + 1) % 2))
                else:
                    run_all(gb)

        def layer1b(es2):
            sb = lambda name, shape, dt: es2.enter_context(nc.sbuf_tensor("sb_" + name, list(shape), dt))
            wout1 = sb("wout1", [128, 16, 1024], BF16); b_wout1 = B("wout1")
            wpg = sb("wpg1", [128, 8, 1024], BF16); b_wpg = B("wpg1")
            wp = sb("wp1", [128, 2, 1024], BF16); b_wp = B("wp1")
            gfin = sb("gfin", [128, D], F32); b_gfin = B("gfin")
            ogT16 = sb("ogT16", [128, 16, 128], BF16); b_ogT16 = B("ogT16")
            hn2 = sb("hn2", [128, D], BF16); b_hn2 = B("hn2")
            st2 = sb("st2", [128, 8], F32); b_st2 = B("st2")
            big = sb("big2", [128, 2048], F32)
            tg = big[:, 0:1024]
            t2 = big[:, 1024:2048]
            P.dma("pool", wout1[:], wout1_d, ds_w, writes=[b_wout1])
            P.dma("pool", wpg[:], wpg_d[1], ds_w, writes=[b_wpg])
            P.dma("pool", wp[:], wp_d[1], ds_w, writes=[b_wp])
            P.dma("sp", gple[:], vec_d[3], ds_v, writes=[b_gple])
            P.dma("sp", gfin[:], vec_d[4], ds_v, writes=[b_gfin])
            ogl = [sb("ogl%d" % k, [128, 2048], BF16) for k in range(2)]; b_ogl = [B("ogl%d" % k) for k in range(2)]
            ds_gl = [P.dsem("gl%d" % k) for k in range(2)]
            src = h1_d if mode == "full" else x_d
            xtb = xt + [sb("xt%d" % k, [128, D], F32) for k in (2, 3)]
            b_xtb = b_xt + [B("xt2"), B("xt3")]; ds_xb = ds_x + [P.dsem("x2"), P.dsem("x3")]
            ptb3 = pt + [sb("pt%d" % k, [128, 256], F32) for k in (2, 3)]
            b_ptb3 = b_pt + [B("pt2"), B("pt3")]; ds_pb = ds_p + [P.dsem("p2"), P.dsem("p3")]

            def loadx(s, i, k):
                rd = [b_h1[s][i]] if mode == "full" else []
                P.dma("sp", xtb[k][:], src[s, i * 128:(i + 1) * 128, :], ds_xb[k], reads=rd, writes=[b_xtb[k]])
                P.dma("sp", ptb3[k][:], p_d[1, s, i * 128:(i + 1) * 128, :], ds_pb[k], writes=[b_ptb3[k]])

            def loadog(s, i, k):
                P.dma("sp", ogl[k][:], og_d[s, i * 128:(i + 1) * 128, :], ds_gl[k], reads=[b_ogd[s][i]], writes=[b_ogl[k]])

            def frontB(s, i, par, sl):
                X = xtb[sl]; bX = b_xtb[sl]
                for g0 in range(0, 16, 4):
                    transposes(ogl[par][:, g0 * 128:(g0 + 4) * 128], 4, g0 * 128,
                               lambda a0, a1, g0=g0: ogT16[:, g0 + a0:g0 + a1, :], [b_ogl[par]], [b_ogT16])
                    yield 1.0

                for nb in range(2):
                    def mm_y1(e, nb=nb):
                        ins = None
                        for c in range(16):
                            ins = e.matmul(PS[:, nb * 512:(nb + 1) * 512], lhsT=ogT16[:, c, :],
                                           rhs=wout1[:, c, nb * 512:(nb + 1) * 512], start=(c == 0), stop=(c == 15))
                        return ins
                    P.op("pe", mm_y1, reads=[b_ogT16, b_wout1], writes=[PB[nb]])
                    yield 3.5
                P.op("dve", lambda e: e.tensor_tensor(out=X[:], in0=X[:], in1=PS[:, 0:1024], op=ALU.add),
                     reads=[bX] + PB[0:2], writes=[bX])
                yield 1.0

            def backB(s, i, par, sl):
                X = xtb[sl]; bX = b_xtb[sl]
                P.op("act", lambda e: e.activation(out=hn[:], in_=X[:], func=AF.Square, accum_out=st[:, 0:1]),
                     reads=[bX], writes=[b_hn, b_st])
                rstd_from_ss(0, 1, 1.0 / D)
                P.op("dve", lambda e: e.scalar_tensor_tensor(out=hn[:], in0=X[:], scalar=st[:, 0:1], in1=gple[:],
                                                             op0=ALU.mult, op1=ALU.mult),
                     reads=[bX, b_st, b_gple], writes=[b_hn])
                yield 5.0
                transposes(hn, 8, 2048, lambda g0, g1: hnT[:, g0:g1, :], [b_hn], [b_hnT])
                yield 2.0

                def mm_u(e):
                    ins = None
                    for nb in range(2):
                        for c in range(8):
                            ins = e.matmul(PS[:, 3072 + nb * 512:3072 + (nb + 1) * 512], lhsT=hnT[:, c, :],
                                           rhs=wpg[:, c, nb * 512:(nb + 1) * 512], start=(c == 0), stop=(c == 7))
                    return ins
                P.op("pe", mm_u, reads=[b_hnT, b_wpg], writes=PB[6:8])
                P.op("act", lambda e: e.activation(out=ptb[:], in_=ptb3[sl][:], func=AF.Copy),
                     reads=[b_ptb3[sl]], writes=[b_ptb])
                transposes(ptb, 2, 2048, lambda g0, g1: pT[:, g0:g1, :], [b_ptb], [b_pT])
                yield 4.0

                def mm_pw(e):
                    ins = None
                    for nb in range(2):
                        for c in range(2):
                            ins = e.matmul(PS[:, 2048 + nb * 512:2048 + (nb + 1) * 512], lhsT=pT[:, c, :],
                                           rhs=wp[:, c, nb * 512:(nb + 1) * 512], start=(c == 0), stop=(c == 1))
                    return ins
                P.op("pe", mm_pw, reads=[b_pT, b_wp], writes=PB[4:6])
                P.op("act", lambda e: e.activation(out=tg, in_=PS[:, 3072:4096], func=AF.Tanh, scale=0.5),
                     reads=PB[6:8], writes=b_tg)
                P.op("dve", lambda e: e.scalar_tensor_tensor(out=t2, in0=tg, scalar=1.0, in1=PS[:, 2048:3072],
                                                             op0=ALU.add, op1=ALU.mult),
                     reads=b_tg + PB[4:6], writes=b_t2)
                P.op("dve", lambda e: e.scalar_tensor_tensor(out=X[:], in0=t2, scalar=0.5, in1=X[:],
                                                             op0=ALU.mult, op1=ALU.add),
                     reads=b_t2 + [bX], writes=[bX])
                yield 5.0

            def backB2(s, i, par, sl):
                X = xtb[sl]; bX = b_xtb[sl]
                P.op("act", lambda e: e.activation(out=hn2[:], in_=X[:], func=AF.Square, accum_out=st2[:, 0:1]),
                     reads=[bX], writes=[b_hn2, b_st2])
                P.op("dve", lambda e: e.tensor_scalar(out=st2[:, 0:1], in0=st2[:, 0:1], scalar1=1.0 / D, scalar2=EPS,
                                                      op0=ALU.mult, op1=ALU.add), reads=[b_st2], writes=[b_st2])
                yield 2.0
                newton_rsqrt(P, st2[:, 0:1], st2[:, 4:7], [b_st2], 1)
                yield 3.0
                P.op("dve", lambda e: e.scalar_tensor_tensor(out=X[:], in0=X[:], scalar=st2[:, 0:1], in1=gfin[:],
                                                             op0=ALU.mult, op1=ALU.mult),
                     reads=[bX, b_st2, b_gfin], writes=[bX])
                P.dma("sp", out_d[s, i * 128:(i + 1) * 128, :], X[:], ds_xb[sl], reads=[bX])
                yield 2.0

            items = [(s, i) for s in range(NS) for i in range(NT)]

            def run_all(g):
                for _ in g:
                    pass

            def interleave(ga, gb, ta, tb):
                gens = [[ga, 0.0, ta], [gb, 0.0, tb]]
                while gens:
                    g = min(gens, key=lambda x: x[1] / x[2])
                    try:
                        c = next(g[0])
                        g[1] += (c if c else 1.0)
                    except StopIteration:
                        gens.remove(g)

            def interleave3(gl):
                gens = [[g, 0.0, t] for (g, t) in gl if g is not None]
                while gens:
                    g = min(gens, key=lambda x: x[1] / x[2])
                    try:
                        c = next(g[0])
                        g[1] += (c if c else 1.0)
                    except StopIteration:
                        gens.remove(g)

            NI = len(items)
            for n in range(min(3, NI)):
                loadx(items[n][0], items[n][1], n % 4)
            for n in range(min(2, NI)):
                loadog(items[n][0], items[n][1], n % 2)
            run_all(frontB(items[0][0], items[0][1], 0, 0))
            for m in range(NI + 1):
                if m + 2 < NI:
                    loadog(items[m + 2][0], items[m + 2][1], m % 2)
                gl = []
                if m - 1 >= 0:
                    gl.append((backB2(items[m - 1][0], items[m - 1][1], (m - 1) % 2, (m - 1) % 4), 7.0))
                if m < NI:
                    gl.append((backB(items[m][0], items[m][1], m % 2, m % 4), 16.0))
                if m + 1 < NI:
                    gl.append((frontB(items[m + 1][0], items[m + 1][1], (m + 1) % 2, (m + 1) % 4), 12.0))
                interleave3(gl)
                if m + 3 < NI:
                    loadx(items[m + 3][0], items[m + 3][1], (m + 3) % 4)

        if mode in ("full", "l0"):
            with ExitStack() as es0:
                layer0(es0)
            P.barrier()
        if mode in ("full", "l1", "l1a"):
            with ExitStack() as es1:
                layer1a(es1)
            P.barrier()
        if mode in ("full", "l1"):
            with ExitStack() as es2:
                layer1b(es2)
        P.emit(final_waits=P.dsems)
    return nc


def _chunked(w, c):
    n = w.shape[1]
    return np.ascontiguousarray(w.reshape(c, 128, n).transpose(1, 0, 2))


def host_consts(L, KBIS):
    NT = L // 128
    i = np.arange(128)
    ident = np.eye(128, dtype=np.float32)
    causT = (i[None, :] >= i[:, None]).astype(np.float32)
    cbias = np.where(i[None, :] <= i[:, None], 0.0, -1e9).astype(np.float32)
    pow2 = np.broadcast_to((2.0 ** -(np.arange(KBIS) + 1.0)).astype(np.float32), (128, KBIS)).copy()
    pos = np.arange(L, dtype=np.float32)
    angle = (1.0 / (10000.0 ** np.linspace(0.0, 1.0, 128, dtype=np.float32))).astype(np.float32)
    theta = pos[:, None] * angle[None, :]
    cos = np.cos(theta).astype(np.float32).reshape(NT, 128, 128)
    sin = np.sin(theta).astype(np.float32).reshape(NT, 128, 128)
    gam = np.array([1.0 - 2.0 ** (-5.0 - h) for h in range(4)], dtype=np.float64)
    xi = gam[None, :] ** (i[:, None] + 1.0)
    ks = (gam[None, :] ** (-(i[:, None] + 1.0))) * (256.0 ** -0.5)
    rot = np.empty((NT, 128, 4, 4, 128), dtype=np.float32)
    rot[:, :, 0] = cos[:, :, None, :] * xi[None, :, :, None]
    rot[:, :, 1] = sin[:, :, None, :] * xi[None, :, :, None]
    rot[:, :, 2] = cos[:, :, None, :] * ks[None, :, :, None]
    rot[:, :, 3] = sin[:, :, None, :] * ks[None, :, :, None]
    return dict(ident=ident, causT=causT, cbias=cbias, pow2=pow2, rot=rot)


def host_weights(g_pre, a_w_in, a_g_q, a_g_kv, a_w_q_up, a_w_idx_q, a_g_ik, a_b_ik, a_w_uk, a_w_uv, a_w_out,
                 r_w_in, r_w_out, w_ple_gate, g_ple, w_ple, g_final):
    f = np.float32
    rep = lambda v: np.broadcast_to(np.asarray(v, f)[None, :], (128, v.shape[0]))
    vecs = np.stack([rep(g_pre[0]), rep(g_ple[0]), rep(g_pre[1]), rep(g_ple[1]), rep(g_final)]).astype(f)
    vsm = np.concatenate([rep(a_g_q[0]), rep(a_g_kv[0]), rep(a_g_ik[0]), rep(a_b_ik[0])], axis=1).astype(f)
    w = np.asarray(a_w_in[0], f)
    wpad = np.zeros((1024, 1792), f)
    wpad[:, 0:384] = w[:, 0:384]
    wpad[:, 384:448] = w[:, 640:704]
    wpad[:, 448:456] = w[:, 704:712]
    wpad[:, 512:768] = w[:, 384:640]
    wpad[:, 768:1792] = w[:, 712:1736]
    wukT = np.ascontiguousarray(np.asarray(a_w_uk[0], f).transpose(2, 0, 1))
    wuv = np.asarray(a_w_uv[0], f).transpose(1, 0, 2).reshape(256, 1024)
    return dict(
        vecs=np.ascontiguousarray(vecs), vsm=np.ascontiguousarray(vsm),
        w0in=_chunked(wpad, 8), wq=_chunked(np.asarray(a_w_q_up[0], f), 3), wiq=_chunked(np.asarray(a_w_idx_q[0], f), 3),
        wukT=wukT, wuv=_chunked(wuv, 2), wout0=_chunked(np.asarray(a_w_out[0], f), 8),
        wpg=np.stack([_chunked(np.asarray(w_ple_gate[l], f), 8) for l in range(2)]),
        wp=np.stack([_chunked(np.asarray(w_ple[l], f), 2) for l in range(2)]),
        w1in=_chunked(np.asarray(r_w_in[0], f), 8), wout1=_chunked(np.asarray(r_w_out[0], f), 16),
    )


_NC_CACHE = {}


def run(x, p, wts, L, NS, ncores, KBIS=24, mode="full"):
    key = (L, NS, KBIS, mode)
    if key not in _NC_CACHE:
        _NC_CACHE[key] = build(L, NS, KBIS, mode)
    nc = _NC_CACHE[key]
    consts = host_consts(L, KBIS)
    in_maps = []
    for c in range(ncores):
        m = dict(wts)
        m.update(consts)
        m["x"] = np.ascontiguousarray(x[c * NS:(c + 1) * NS])
        m["p"] = np.ascontiguousarray(p[:, c * NS:(c + 1) * NS])
        in_maps.append(m)
    res = run_bass_kernel_spmd(nc, in_maps, core_ids=list(range(ncores)))
    return np.concatenate([r["out"] for r in res.results], axis=0)


def kernel(x, p, g_pre, a_w_in, a_g_q, a_g_kv, a_w_q_up, a_w_idx_q, a_g_ik, a_b_ik, a_w_uk, a_w_uv, a_w_out,
           r_w_in, r_w_out, w_ple_gate, g_ple, w_ple, g_final):
    x = np.asarray(x, np.float32)
    p = np.asarray(p, np.float32)
    wts = host_weights(g_pre, a_w_in, a_g_q, a_g_kv, a_w_q_up, a_w_idx_q, a_g_ik, a_b_ik, a_w_uk, a_w_uv, a_w_out,
                       r_w_in, r_w_out, w_ple_gate, g_ple, w_ple, g_final)
    Bn, L, _ = x.shape
    out = run(x, p, wts, L, Bn // 8, 8)
    return out.astype(np.float32)
```

```python
import numpy as np
import concourse.bass as bass
import concourse.mybir as mybir
from concourse.bass_utils import run_bass_kernel_spmd
from contextlib import ExitStack

F32 = mybir.dt.float32
BF16 = mybir.dt.bfloat16
AF = mybir.ActivationFunctionType
ALU = mybir.AluOpType
AX = mybir.AxisListType

D = 1024
EPS = 1e-6
COMPUTE = ("pe", "act", "dve", "pool")


class Buf:
    __slots__ = ("name", "writer", "readers")

    def __init__(self, name):
        self.name = name
        self.writer = None
        self.readers = []


class DmaSem:
    __slots__ = ("sem", "count")

    def __init__(self, sem):
        self.sem = sem
        self.count = 0


class Op:
    __slots__ = ("eng", "fn", "waits", "sig", "count", "dsem", "dval", "is_dma")

    def __init__(self, eng, fn):
        self.eng = eng
        self.fn = fn
        self.waits = []
        self.sig = False
        self.count = None
        self.dsem = None
        self.dval = None
        self.is_dma = False


class Prog:
    def __init__(self, nc, es, strict=False):
        self.nc = nc
        self.es = es
        self.strict = strict
        self.ops = {e: [] for e in ("pe", "act", "dve", "pool", "sp")}
        self.esem = {e: es.enter_context(nc.semaphore("s_" + e)) for e in COMPUTE}
        self.dsems = []
        self.pending = {e: [] for e in self.ops}

    def sb(self, name, shape, dt):
        return self.es.enter_context(self.nc.sbuf_tensor("sb_" + name, list(shape), dt))

    def dsem(self, name):
        d = DmaSem(self.es.enter_context(self.nc.semaphore("d_" + name)))
        self.dsems.append(d)
        return d

    def _dep(self, op, prod):
        if prod is None or prod is op:
            return
        if prod.is_dma:
            op.waits.append((prod.dsem, prod.dsem.count))
        else:
            prod.sig = True
            op.waits.append(prod)

    def barrier(self):
        lasts = []
        for e in COMPUTE:
            comp = [o for o in self.ops[e] if not o.is_dma]
            if comp:
                comp[-1].sig = True
                lasts.append(comp[-1])
        dm = [(d, d.count) for d in self.dsems if d.count > 0]
        for e in self.ops:
            self.pending[e] = list(lasts) + list(dm)

    def _track(self, op, reads, writes):
        for b in reads:
            w = b.writer
            if w is not None:
                if not (w.eng == op.eng and op.eng == "pe" and not w.is_dma and not op.is_dma):
                    self._dep(op, w)
        for b in writes:
            w = b.writer
            if w is not None and (w.is_dma or op.is_dma or w.eng != op.eng or (self.strict and op.eng != "pe")):
                self._dep(op, w)
            for r in b.readers:
                if r.is_dma or op.is_dma or r.eng != op.eng or (self.strict and op.eng != "pe"):
                    self._dep(op, r)
        for b in reads:
            b.readers.append(op)
        for b in writes:
            b.writer = op
            b.readers = []

    def op(self, eng, fn, reads=(), writes=()):
        o = Op(eng, fn)
        if self.pending[eng]:
            o.waits.extend(self.pending[eng])
            self.pending[eng] = []
        self._track(o, reads, writes)
        self.ops[eng].append(o)
        return o

    def dma(self, eng, out, in_, dsem, reads=(), writes=()):
        def fn(e):
            return e.dma_start(out=out, in_=in_)
        o = Op(eng, fn)
        o.is_dma = True
        o.dsem = dsem
        if self.pending[eng]:
            o.waits.extend(self.pending[eng])
            self.pending[eng] = []
        self._track(o, reads, writes)
        dsem.count += 16
        o.dval = dsem.count
        self.ops[eng].append(o)
        return o

    def emit(self, final_waits=()):
        nc = self.nc
        for e in COMPUTE:
            c = 0
            for o in self.ops[e]:
                if o.is_dma:
                    continue
                if o.sig:
                    c += 1
                o.count = c

        def run(engname, engobj):
            waited = {}
            for o in self.ops[engname]:
                for p in o.waits:
                    if isinstance(p, tuple):
                        sem, val = p[0].sem, p[1]
                    else:
                        if p.eng == engname and p.count is None:
                            continue
                        sem, val = self.esem[p.eng], p.count
                    key = id(sem)
                    if waited.get(key, 0) < val:
                        engobj.wait_ge(sem, val)
                        waited[key] = val
                ins = o.fn(engobj)
                if o.is_dma:
                    ins.then_inc(o.dsem.sem, 16)
                elif o.sig:
                    ins.then_inc(self.esem[engname], 1)
            if engname == "sp":
                for ds in final_waits:
                    engobj.wait_ge(ds.sem, ds.count)

        with nc.Block() as block:
            @block.tensor
            def _(e):
                run("pe", e)

            @block.scalar
            def _(e):
                run("act", e)

            @block.vector
            def _(e):
                run("dve", e)

            @block.gpsimd
            def _(e):
                run("pool", e)

            @block.sync
            def _(e):
                run("sp", e)


I32 = mybir.dt.int32


def newton_rsqrt(P, v, tmp, bufs, n):
    hv = tmp[:, 0:n]
    y = tmp[:, n:2 * n]
    t = tmp[:, 2 * n:3 * n]
    P.op("dve", lambda e: e.tensor_scalar(out=hv, in0=v, scalar1=-0.5, scalar2=None, op0=ALU.mult), reads=bufs, writes=bufs)
    P.op("dve", lambda e: e.tensor_copy(out=t, in_=v.bitcast(I32)), reads=bufs, writes=bufs)
    P.op("dve", lambda e: e.tensor_scalar(out=y.bitcast(I32), in0=t, scalar1=-0.5, scalar2=1597463007.0,
                                          op0=ALU.mult, op1=ALU.add), reads=bufs, writes=bufs)
    for it in range(3):
        dst = v if it == 2 else y
        if n == 1:
            P.op("dve", lambda e: e.scalar_tensor_tensor(out=t, in0=y, scalar=y, in1=hv, op0=ALU.mult, op1=ALU.mult),
                 reads=bufs, writes=bufs)
        else:
            P.op("dve", lambda e: e.tensor_tensor(out=t, in0=y, in1=y, op=ALU.mult), reads=bufs, writes=bufs)
            P.op("dve", lambda e: e.tensor_tensor(out=t, in0=t, in1=hv, op=ALU.mult), reads=bufs, writes=bufs)
        P.op("dve", lambda e, dst=dst: e.scalar_tensor_tensor(out=dst, in0=t, scalar=1.5, in1=y, op0=ALU.add, op1=ALU.mult),
             reads=bufs, writes=bufs)


def build(L=2048, NS=2, KBIS=24, mode="full", strict=False):
    NT = L // 128
    TOPK = min(256, L // 4)
    nc = bass.Bass("TRN2", target_bir_lowering=False)

    def din(name, shape):
        return nc.dram_tensor(name, list(shape), F32, kind="ExternalInput").ap()

    x_d = din("x", [NS, L, D])
    p_d = din("p", [2, NS, L, 256])
    vec_d = din("vecs", [5, 128, D])
    vsm_d = din("vsm", [128, 768])
    w0in_d = din("w0in", [128, 8, 1792])
    wq_d = din("wq", [128, 3, 1024])
    wiq_d = din("wiq", [128, 3, 512])
    wukT_d = din("wukT", [128, 8, 256])
    wuv_d = din("wuv", [128, 2, 1024])
    wout0_d = din("wout0", [128, 8, 1024])
    wpg_d = din("wpg", [2, 128, 8, 1024])
    wp_d = din("wp", [2, 128, 2, 1024])
    w1in_d = din("w1in", [128, 8, 6144])
    wout1_d = din("wout1", [128, 16, 1024])
    ident_d = din("ident", [128, 128])
    causT_d = din("causT", [128, 128])
    cbias_d = din("cbias", [128, 128])
    pow2_d = din("pow2", [128, KBIS])
    rot_d = din("rot", [NT, 128, 4, 4, 128])
    out_d = nc.dram_tensor("out", [NS, L, D], F32, kind="ExternalOutput").ap()
    h1_d = nc.dram_tensor("h1s", [NS, L, D], F32, kind="Internal").ap()
    og_d = nc.dram_tensor("ogs", [NS, L, 2048], BF16, kind="Internal").ap()

    gam = [1.0 - 2.0 ** (-5.0 - h) for h in range(4)]
    gch = [g ** 128 for g in gam]

    es = ExitStack()
    with es:
        P = Prog(nc, es, strict)

        def B(name):
            return Buf(name)

        PS = es.enter_context(nc.psum_tensor("PS", [128, 4096], F32))
        PB = [B("bank%d" % k) for k in range(8)]

        def pbk(lo, hi):
            return PB[lo // 512:(hi - 1) // 512 + 1]

        sb = P.sb
        ident = sb("ident", [128, 128], BF16); b_ident = B("ident")
        causT = sb("causT", [128, 128], BF16); b_causT = B("causT")
        cbias = sb("cbias", [128, 128], F32); b_cbias = B("cbias")
        pow2 = sb("pow2", [128, KBIS], F32); b_pow2 = B("pow2")
        ds_c = P.dsem("const")
        P.dma("pool", ident[:], ident_d, ds_c, writes=[b_ident])
        P.dma("pool", causT[:], causT_d, ds_c, writes=[b_causT])
        ds_c2 = P.dsem("const2")
        P.dma("sp", cbias[:], cbias_d, ds_c2, writes=[b_cbias])
        P.dma("sp", pow2[:], pow2_d, ds_c2, writes=[b_pow2])

        gpre = sb("gpre", [128, D], F32); b_gpre = B("gpre")
        gple = sb("gple", [128, D], F32); b_gple = B("gple")
        ds_w = P.dsem("w")
        ds_v = P.dsem("v")
        xt = [sb("xt%d" % k, [128, D], F32) for k in range(2)]
        b_xt = [B("xt%d" % k) for k in range(2)]
        ds_x = [P.dsem("x%d" % k) for k in range(2)]
        pt = [sb("pt%d" % k, [128, 256], F32) for k in range(2)]
        b_pt = [B("pt%d" % k) for k in range(2)]
        ds_p = [P.dsem("p%d" % k) for k in range(2)]
        st = sb("st", [128, 32], F32); b_st = B("st")
        hn = sb("hn", [128, D], BF16); b_hn = B("hn")
        hnT = sb("hnT", [128, 8, 128], BF16); b_hnT = B("hnT")
        ptb = sb("ptb", [128, 256], BF16); b_ptb = B("ptb")
        pT = sb("pT", [128, 2, 128], BF16); b_pT = B("pT")
        b_bq = [B("bq%d" % k) for k in range(4)]
        b_tg = b_bq[0:2]
        b_t2 = b_bq[2:4]
        b_og = B("og")
        th = sb("th", [128, 2048], BF16); b_th = B("th")
        b_h1 = [[B("h1_%d_%d" % (s, i)) for i in range(NT)] for s in range(NS)]
        b_ogd = [[B("ogd_%d_%d" % (s, i)) for i in range(NT)] for s in range(NS)]

        def transposes(src, nchunk, pcol0, dst_view_fn, rb, wb):
            for g0 in range(0, nchunk, 4):
                g1 = min(nchunk, g0 + 4)
                c0 = pcol0 + g0 * 128
                c1 = pcol0 + g1 * 128
                banks = pbk(c0, c1)

                def mm(e, g0=g0, g1=g1, c0=c0):
                    ins = None
                    for c in range(g0, g1):
                        ins = e.matmul(PS[:, c0 + (c - g0) * 128: c0 + (c - g0 + 1) * 128],
                                       lhsT=src[:, c * 128:(c + 1) * 128], rhs=ident[:], start=True, stop=True)
                    return ins
                P.op("pe", mm, reads=rb + [b_ident], writes=banks)
                P.op("act", lambda e, g0=g0, g1=g1, c0=c0, c1=c1: e.activation(
                    out=dst_view_fn(g0, g1), in_=PS[:, c0:c1].rearrange("p (c t) -> p c t", t=128), func=AF.Copy),
                    reads=banks, writes=wb)

        def rstd_from_ss(col, n, scale):
            P.op("dve", lambda e: e.tensor_scalar(out=st[:, col:col + n], in0=st[:, col:col + n],
                                                  scalar1=scale, scalar2=EPS, op0=ALU.mult, op1=ALU.add),
                 reads=[b_st], writes=[b_st])
            newton_rsqrt(P, st[:, col:col + n], st[:, 28:28 + 3 * n], [b_st], n)

        def rmsnorm_T(xs, bx, gvec, bg):
            P.op("act", lambda e: e.activation(out=hn[:], in_=xs[:], func=AF.Square, accum_out=st[:, 0:1]),
                 reads=[bx], writes=[b_hn, b_st])
            rstd_from_ss(0, 1, 1.0 / D)
            P.op("dve", lambda e: e.scalar_tensor_tensor(out=hn[:], in0=xs[:], scalar=st[:, 0:1], in1=gvec[:],
                                                         op0=ALU.mult, op1=ALU.mult),
                 reads=[bx, b_st, bg], writes=[b_hn])
            transposes(hn, 8, 0, lambda g0, g1: hnT[:, g0:g1, :], [b_hn], [b_hnT])

        def load_tile(s, i, k, layer, src=None, rd=()):
            if src is None:
                src = x_d
            P.dma("sp", xt[k][:], src[s, i * 128:(i + 1) * 128, :], ds_x[k], reads=list(rd), writes=[b_xt[k]])
            P.dma("sp", pt[k][:], p_d[layer, s, i * 128:(i + 1) * 128, :], ds_p[k], writes=[b_pt[k]])

        def ple_stage(k, wpg, b_wpg, wp, b_wp, tg, t2):
            X = xt[k]; bX = b_xt[k]
            rmsnorm_T(X, bX, gple, b_gple)

            def mm_u(e):
                ins = None
                for nb in range(2):
                    for c in range(8):
                        ins = e.matmul(PS[:, 1024 + nb * 512:1024 + (nb + 1) * 512], lhsT=hnT[:, c, :],
                                       rhs=wpg[:, c, nb * 512:(nb + 1) * 512], start=(c == 0), stop=(c == 7))
                return ins
            P.op("pe", mm_u, reads=[b_hnT, b_wpg], writes=PB[2:4])
            P.op("act", lambda e: e.activation(out=ptb[:], in_=pt[k][:], func=AF.Copy),
                 reads=[b_pt[k]], writes=[b_ptb])
            transposes(ptb, 2, 3072, lambda g0, g1: pT[:, g0:g1, :], [b_ptb], [b_pT])

            def mm_pw(e):
                ins = None
                for nb in range(2):
                    for c in range(2):
                        ins = e.matmul(PS[:, 2048 + nb * 512:2048 + (nb + 1) * 512], lhsT=pT[:, c, :],
                                       rhs=wp[:, c, nb * 512:(nb + 1) * 512], start=(c == 0), stop=(c == 1))
                return ins
            P.op("pe", mm_pw, reads=[b_pT, b_wp], writes=PB[4:6])
            P.op("act", lambda e: e.activation(out=tg, in_=PS[:, 1024:2048], func=AF.Tanh, scale=0.5),
                 reads=PB[2:4], writes=b_tg)
            P.op("dve", lambda e: e.scalar_tensor_tensor(out=t2, in0=tg, scalar=1.0, in1=PS[:, 2048:3072],
                                                         op0=ALU.add, op1=ALU.mult),
                 reads=b_tg + PB[4:6], writes=b_t2)
            P.op("dve", lambda e: e.scalar_tensor_tensor(out=X[:], in0=t2, scalar=0.5, in1=X[:],
                                                         op0=ALU.mult, op1=ALU.add),
                 reads=b_t2 + [bX], writes=[bX])

        def layer0(es0):
            sb = lambda name, shape, dt: es0.enter_context(nc.sbuf_tensor("sb_" + name, list(shape), dt))
            w0in = sb("w0in", [128, 8, 1792], BF16); b_w0in = B("w0in")
            wq = sb("wq", [128, 3, 1024], BF16); b_wq = B("wq")
            wiq = sb("wiq", [128, 3, 512], BF16); b_wiq = B("wiq")
            wukT = sb("wukT", [128, 8, 256], BF16); b_wukT = B("wukT")
            wuv = sb("wuv", [128, 2, 1024], BF16); b_wuv = B("wuv")
            wout0 = sb("wout0", [128, 8, 1024], BF16); b_wout0 = B("wout0")
            wpg = sb("wpg0", [128, 8, 1024], BF16); b_wpg = B("wpg0")
            wp = sb("wp0", [128, 2, 1024], BF16); b_wp = B("wp0")
            vsm = sb("vsm", [128, 768], F32); b_vsm = B("vsm")
            P.dma("pool", w0in[:], w0in_d, ds_w, writes=[b_w0in])
            P.dma("sp", gpre[:], vec_d[0], ds_v, writes=[b_gpre])
            P.dma("sp", gple[:], vec_d[1], ds_v, writes=[b_gple])
            P.dma("sp", vsm[:], vsm_d, ds_v, writes=[b_vsm])
            P.dma("pool", wq[:], wq_d, ds_w, writes=[b_wq])
            P.dma("pool", wiq[:], wiq_d, ds_w, writes=[b_wiq])
            P.dma("pool", wukT[:], wukT_d, ds_w, writes=[b_wukT])
            P.dma("pool", wuv[:], wuv_d, ds_w, writes=[b_wuv])
            P.dma("pool", wout0[:], wout0_d, ds_w, writes=[b_wout0])
            P.dma("pool", wpg[:], wpg_d[0], ds_w, writes=[b_wpg])
            P.dma("pool", wp[:], wp_d[0], ds_w, writes=[b_wp])

            ckvT = sb("ckvT", [128, 2, L], BF16); b_ckvT = [B("ckvT%d" % j) for j in range(NT)]
            Vaug = sb("Vaug", [128, NT, 8, 129], BF16); b_Vaug = [B("Vaug%d" % j) for j in range(NT)]
            b_Vones = B("Vones")
            kxT = sb("kxT", [128, L], BF16); b_kxT = [B("kxT%d" % j) for j in range(NT)]
            hnF = sb("hnF", [128, D], BF16); b_hnF = B("hnF")
            hnTF_1 = sb("hnTF", [128, 8, 128], BF16); b_hnTF_1 = B("hnTF")
            hnTF2 = [hnTF_1, hnTF_1]; b_hnTF2 = [b_hnTF_1, b_hnTF_1]
            stF = sb("stF", [128, 32], F32); b_stF = B("stF")
            cqn = sb("cqn", [128, 384], BF16); b_cqn = B("cqn")
            cqT = sb("cqT", [128, 3, 128], BF16); b_cqT = B("cqT")
            ckvn = sb("ckvn", [128, 256], BF16); b_ckvn = B("ckvn")
            kxt = sb("kxt", [128, 64], F32); b_kxt = B("kxt")
            kx2 = sb("kx2", [128, 128], BF16); b_kx2 = B("kx2")
            wi2 = [sb("wi%d" % k, [128, 24], F32) for k in range(2)]; b_wi2 = [B("wi%d" % k) for k in range(2)]
            xt3 = xt + [sb("xt2a", [128, D], F32)]; b_xt3 = b_xt + [B("xt2a")]; ds_x3 = ds_x + [P.dsem("x2a")]
            qT_1 = sb("qT", [128, 8, 128], BF16); b_qT_1 = B("qT")
            qT2 = [qT_1, qT_1]; b_qT2 = [b_qT_1, b_qT_1]
            qiT2 = [sb("qiT%d" % k, [128, 4, 128], BF16) for k in range(2)]; b_qiT2 = [B("qiT%d" % k) for k in range(2)]
            acc = sb("acc", [128, L], F32); b_acc = B("acc")
            rl = [sb("rl%d" % k, [128, 512], F32) for k in range(2)]; b_rl = [B("rl%d" % k) for k in range(2)]
            bs = sb("bs", [128, 8 + KBIS], F32); b_bs = B("bs")
            qaT = [sb("qaT%d" % k, [128, 2, 8, 128], BF16) for k in range(2)]; b_qaT = [B("qaT%d" % k) for k in range(2)]
            mq1 = sb("maskq", [128, L], BF16); bmq1 = B("maskq")
            maskq = [mq1, mq1]; b_maskq = [bmq1, bmq1]
            thg = [th[:, 0:1024], th[:, 1024:2048]]; b_thg = [B("thg0"), B("thg1")]
            maskT = sb("maskT", [128, NT, 128], BF16); b_maskT = B("maskT")
            NE = 4
            Eb = [sb("Eb%d" % k, [128, 4, 128], BF16) for k in range(NE)]; b_Eb = [B("Eb%d" % k) for k in range(NE)]
            rden = sb("rden", [128, 4, 1], F32); b_rden = B("rden")
            big0 = sb("big0", [128, 1024], F32)
            on = big0[:, :]; b_on = b_bq[0:2]
            tg = big0[:, :]
            ogB = hn[:, :]
            P.op("dve", lambda e: e.memset(Vaug[:, :, :, 128:129], 1.0), writes=[b_Vones])

            def rstdF(col, n, scale):
                P.op("dve", lambda e: e.tensor_scalar(out=stF[:, col:col + n], in0=stF[:, col:col + n],
                                                      scalar1=scale, scalar2=EPS, op0=ALU.mult, op1=ALU.add),
                     reads=[b_stF], writes=[b_stF])
                newton_rsqrt(P, stF[:, col:col + n], stF[:, 16:16 + 3 * n], [b_stF], n)

            def proj(s, i, par, sl):
                X = xt3[sl]; bX = b_xt3[sl]
                qT = qT2[par]; b_qT = b_qT2[par]
                hnTF = hnTF2[par]; b_hnTF = b_hnTF2[par]
                WI = wi2[par]; bWI = b_wi2[par]
                QI = qiT2[par]; bQI = b_qiT2[par]
                P.op("act", lambda e: e.activation(out=hnF[:], in_=X[:], func=AF.Square, accum_out=stF[:, 0:1]),
                     reads=[bX], writes=[b_hnF, b_stF])
                rstdF(0, 1, 1.0 / D)
                P.op("dve", lambda e: e.scalar_tensor_tensor(out=hnF[:], in0=X[:], scalar=stF[:, 0:1], in1=gpre[:],
                                                             op0=ALU.mult, op1=ALU.mult),
                     reads=[bX, b_stF, b_gpre], writes=[b_hnF])
                yield 2.5
                transposes(hnF, 8, 0, lambda g0, g1: hnTF[:, g0:g1, :], [b_hnF], [b_hnTF])
                yield 2.5
                def mm_za(e):
                    ins = None
                    for (pc, wc, n) in ((0, 0, 512), (512, 512, 256)):
                        for c in range(8):
                            ins = e.matmul(PS[:, pc:pc + n], lhsT=hnTF[:, c, :], rhs=w0in[:, c, wc:wc + n],
                                           start=(c == 0), stop=(c == 7))
                    return ins
                P.op("pe", mm_za, reads=[b_hnTF, b_w0in], writes=PB[0:2])
                ZQ, ZK, ZW, ZKV = 0, 384, 448, 512
                P.op("act", lambda e: e.activation(out=cqn[:], in_=PS[:, ZQ:ZQ + 384], func=AF.Square,
                                                   accum_out=stF[:, 4:5]),
                     reads=[PB[0]], writes=[b_cqn, b_stF])
                P.op("act", lambda e: e.activation(out=ckvn[:], in_=PS[:, ZKV:ZKV + 256], func=AF.Square,
                                                   accum_out=stF[:, 5:6]),
                     reads=[PB[1]], writes=[b_ckvn, b_stF])
                P.op("act", lambda e: e.activation(out=kx2[:, 0:64], in_=PS[:, ZK:ZK + 64], func=AF.Square,
                                                   accum_out=stF[:, 6:7]),
                     reads=[PB[0]], writes=[b_kx2, b_stF])
                P.op("act", lambda e: e.activation(out=kx2[:, 64:128], in_=PS[:, ZK:ZK + 64], func=AF.Copy,
                                                   accum_out=stF[:, 7:8]),
                     reads=[PB[0]], writes=[b_kx2, b_stF])
                P.op("dve", lambda e: e.tensor_scalar(out=stF[:, 4:5], in0=stF[:, 4:5], scalar1=1.0 / 384,
                                                      scalar2=None, op0=ALU.mult), reads=[b_stF], writes=[b_stF])
                P.op("dve", lambda e: e.tensor_scalar(out=stF[:, 5:6], in0=stF[:, 5:6], scalar1=1.0 / 256,
                                                      scalar2=None, op0=ALU.mult), reads=[b_stF], writes=[b_stF])
                P.op("dve", lambda e: e.tensor_scalar(out=stF[:, 7:8], in0=stF[:, 7:8], scalar1=1.0 / 64,
                                                      scalar2=None, op0=ALU.mult), reads=[b_stF], writes=[b_stF])
                P.op("dve", lambda e: e.tensor_tensor(out=stF[:, 8:9], in0=stF[:, 7:8], in1=stF[:, 7:8],
                                                      op=ALU.mult), reads=[b_stF], writes=[b_stF])
                P.op("dve", lambda e: e.scalar_tensor_tensor(out=stF[:, 6:7], in0=stF[:, 6:7], scalar=1.0 / 64,
                                                             in1=stF[:, 8:9], op0=ALU.mult, op1=ALU.subtract),
                     reads=[b_stF], writes=[b_stF])
                rstdF(4, 3, 1.0)
                P.op("dve", lambda e: e.scalar_tensor_tensor(out=cqn[:], in0=PS[:, ZQ:ZQ + 384], scalar=stF[:, 4:5],
                                                             in1=vsm[:, 0:384], op0=ALU.mult, op1=ALU.mult),
                     reads=[PB[0], b_stF, b_vsm], writes=[b_cqn])
                P.op("dve", lambda e: e.scalar_tensor_tensor(out=ckvn[:], in0=PS[:, ZKV:ZKV + 256], scalar=stF[:, 5:6],
                                                             in1=vsm[:, 384:640], op0=ALU.mult, op1=ALU.mult),
                     reads=[PB[1], b_stF, b_vsm], writes=[b_ckvn])
                P.op("dve", lambda e: e.tensor_scalar(out=kxt[:], in0=PS[:, ZK:ZK + 64], scalar1=stF[:, 7:8],
                                                      scalar2=stF[:, 6:7], op0=ALU.subtract, op1=ALU.mult),
                     reads=[PB[0], b_stF], writes=[b_kxt])
                P.op("dve", lambda e: e.tensor_tensor(out=kxt[:], in0=kxt[:], in1=vsm[:, 640:704], op=ALU.mult),
                     reads=[b_kxt, b_vsm], writes=[b_kxt])
                P.op("dve", lambda e: e.tensor_tensor(out=kx2[:, 0:64], in0=kxt[:], in1=vsm[:, 704:768], op=ALU.add),
                     reads=[b_kxt, b_vsm], writes=[b_kx2])
                P.op("dve", lambda e: e.tensor_tensor(out=kx2[:, 64:128], in0=kxt[:], in1=vsm[:, 704:768], op=ALU.add),
                     reads=[b_kxt, b_vsm], writes=[b_kx2])
                P.op("dve", lambda e: e.tensor_scalar(out=WI[:, 0:8], in0=PS[:, ZW:ZW + 8],
                                                      scalar1=float(8 ** -0.5 * 64 ** -0.5), scalar2=None, op0=ALU.mult),
                     reads=[PB[0]], writes=[bWI])
                P.op("act", lambda e: e.activation(out=WI[:, 8:16], in_=WI[:, 0:8], func=AF.Abs),
                     reads=[bWI], writes=[bWI])
                P.op("act", lambda e: e.activation(out=WI[:, 16:24], in_=WI[:, 0:8], func=AF.Sign),
                     reads=[bWI], writes=[bWI])
                yield 7.5
                transposes(cqn, 3, 0, lambda g0, g1: cqT[:, g0:g1, :], [b_cqn], [b_cqT])
                transposes(ckvn, 2, 512, lambda g0, g1: ckvT[:, g0:g1, i * 128:(i + 1) * 128], [b_ckvn], [b_ckvT[i]])
                transposes(kx2, 1, 768, lambda g0, g1: kxT[:, i * 128:(i + 1) * 128].unsqueeze(1), [b_kx2], [b_kxT[i]])
                yield 2.5
                def mm_v(e):
                    ins = None
                    for nb in range(2):
                        for c in range(2):
                            ins = e.matmul(PS[:, nb * 512:(nb + 1) * 512], lhsT=ckvT[:, c, i * 128:(i + 1) * 128],
                                           rhs=wuv[:, c, nb * 512:(nb + 1) * 512], start=(c == 0), stop=(c == 1))
                    return ins
                P.op("pe", mm_v, reads=[b_ckvT[i], b_wuv], writes=PB[0:2])
                P.op("dve", lambda e: e.tensor_copy(out=Vaug[:, i, :, 0:128],
                                                    in_=PS[:, 0:1024].rearrange("p (h v) -> p h v", h=8)),
                     reads=PB[0:2], writes=[b_Vaug[i]])
                yield 2.5
                def mm_q(e):
                    ins = None
                    for h in range(8):
                        for c in range(3):
                            ins = e.matmul(PS[:, h * 128:(h + 1) * 128], lhsT=wq[:, c, h * 128:(h + 1) * 128],
                                           rhs=cqT[:, c, :], start=(c == 0), stop=(c == 2))
                    return ins
                P.op("pe", mm_q, reads=[b_wq, b_cqT], writes=PB[0:2])
                P.op("dve", lambda e: e.tensor_copy(out=qT[:], in_=PS[:, 0:1024].rearrange("p (h t) -> p h t", h=8)),
                     reads=PB[0:2], writes=[b_qT])
                yield 2.5
                def mm_qi(e):
                    ins = None
                    for hp in range(4):
                        for c in range(3):
                            ins = e.matmul(PS[:, hp * 128:(hp + 1) * 128], lhsT=wiq[:, c, hp * 128:(hp + 1) * 128],
                                           rhs=cqT[:, c, :], start=(c == 0), stop=(c == 2))
                    return ins
                P.op("pe", mm_qi, reads=[b_wiq, b_cqT], writes=[PB[0]])
                P.op("act", lambda e: e.activation(out=QI[:], in_=PS[:, 0:512].rearrange("p (h t) -> p h t", h=4),
                                                   func=AF.Copy),
                     reads=[PB[0]], writes=[bQI])
                yield 2.5

            def projB(s, i, par):
                QA = qaT[par]; bQA = b_qaT[par]
                TH = thg[par]; bTH = b_thg[par]
                qT = qT2[par]; b_qT = b_qT2[par]
                hnTF = hnTF2[par]; b_hnTF = b_hnTF2[par]
                for cc in range(2):
                    bk = 1 - cc

                    def mm_qa(e, cc=cc, bk=bk):
                        ins = None
                        for hh in range(8):
                            hq, hr = divmod(hh, 4)
                            ins = e.matmul(PS[:, hq * 512 + hr * 128: hq * 512 + (hr + 1) * 128],
                                           lhsT=wukT[:, hh, cc * 128:(cc + 1) * 128], rhs=qT[:, hh, :], start=True, stop=True)
                        return ins
                    P.op("pe", mm_qa, reads=[b_wukT, b_qT], writes=PB[0:2])
                    P.op("dve", lambda e, cc=cc: e.tensor_scalar(out=QA[:, cc, :, :],
                                                                 in0=PS[:, 0:1024].rearrange("p (h t) -> p h t", h=8),
                                                                 scalar1=float(128 ** -0.5), scalar2=None, op0=ALU.mult),
                         reads=PB[0:2], writes=[bQA])
                    yield 2.5
                def mm_zb(e):
                    ins = None
                    for nb in range(2):
                        for c in range(8):
                            ins = e.matmul(PS[:, nb * 512:(nb + 1) * 512], lhsT=hnTF[:, c, :],
                                           rhs=w0in[:, c, 768 + nb * 512:768 + (nb + 1) * 512],
                                           start=(c == 0), stop=(c == 7))
                    return ins
                P.op("pe", mm_zb, reads=[b_hnTF, b_w0in], writes=PB[0:2])
                P.op("act", lambda e: e.activation(out=TH, in_=PS[:, 0:1024], func=AF.Tanh, scale=0.5),
                     reads=PB[0:2], writes=[bTH])
                P.op("dve", lambda e: e.scalar_tensor_tensor(out=TH, in0=TH, scalar=1.0, in1=PS[:, 0:1024],
                                                             op0=ALU.add, op1=ALU.mult),
                     reads=[bTH] + PB[0:2], writes=[bTH])
                yield 2.5

            def idxbis(s, i, par):
                nk = (i + 1) * 128
                MQ = maskq[par]; bMQ = b_maskq[par]
                WI = wi2[par]; bWI = b_wi2[par]
                QI = qiT2[par]; bQI = b_qiT2[par]
                nkb = (nk + 511) // 512
                for kb in range(nkb):
                    k0 = kb * 512
                    wd = min(512, nk - k0)
                    kbufs = b_kxT[k0 // 128:(k0 + wd) // 128]
                    for h in range(8):
                        bank = h % 2
                        r = h % 2
                        pr = (h % 2) * 64
                        P.op("pe", lambda e, h=h, bank=bank, pr=pr, k0=k0, wd=wd: e.matmul(
                            PS[:, bank * 512:bank * 512 + wd], lhsT=QI[pr:pr + 64, h // 2, :],
                            rhs=kxT[pr:pr + 64, k0:k0 + wd], start=True, stop=True),
                            reads=[bQI] + kbufs, writes=[PB[bank]])
                        P.op("act", lambda e, h=h, bank=bank, r=r, wd=wd: e.activation(
                            out=rl[r][:, 0:wd], in_=PS[:, bank * 512:bank * 512 + wd], func=AF.Relu,
                            scale=WI[:, 8 + h:9 + h]),
                            reads=[PB[bank], bWI], writes=[b_rl[r]])
                        if h == 0:
                            P.op("dve", lambda e, r=r, k0=k0, wd=wd: e.tensor_scalar(
                                out=acc[:, k0:k0 + wd], in0=rl[r][:, 0:wd], scalar1=WI[:, 16:17], scalar2=None,
                                op0=ALU.mult), reads=[b_rl[r], bWI], writes=[b_acc])
                        else:
                            P.op("dve", lambda e, h=h, r=r, k0=k0, wd=wd: e.scalar_tensor_tensor(
                                out=acc[:, k0:k0 + wd], in0=rl[r][:, 0:wd], scalar=WI[:, 16 + h:17 + h],
                                in1=acc[:, k0:k0 + wd], op0=ALU.mult, op1=ALU.add),
                                reads=[b_rl[r], bWI, b_acc], writes=[b_acc])
                        yield 0.7
                LO, HI, WW, THR, CAND, CNT, MM = 0, 1, 2, 3, 4, 5, 6
                P.op("dve", lambda e: e.tensor_reduce(out=bs[:, LO:LO + 1], in_=acc[:, 0:nk], axis=AX.X, op=ALU.min),
                     reads=[b_acc], writes=[b_bs])
                P.op("dve", lambda e: e.tensor_reduce(out=bs[:, HI:HI + 1], in_=acc[:, 0:nk], axis=AX.X, op=ALU.max),
                     reads=[b_acc], writes=[b_bs])
                P.op("dve", lambda e: e.tensor_tensor(out=acc[:, i * 128:(i + 1) * 128], in0=acc[:, i * 128:(i + 1) * 128],
                                                      in1=cbias[:], op=ALU.add),
                     reads=[b_acc, b_cbias], writes=[b_acc])
                P.op("dve", lambda e: e.tensor_copy(out=bs[:, THR:THR + 1], in_=bs[:, LO:LO + 1]),
                     reads=[b_bs], writes=[b_bs])
                yield 2.5
                if nk > TOPK:
                    NTHR, NCAND = WW, CAND
                    P.op("dve", lambda e: e.tensor_tensor(out=bs[:, 7:8], in0=bs[:, HI:HI + 1], in1=bs[:, LO:LO + 1],
                                                          op=ALU.subtract), reads=[b_bs], writes=[b_bs])
                    P.op("dve", lambda e: e.tensor_scalar(out=bs[:, 8:8 + KBIS], in0=pow2[:], scalar1=bs[:, 7:8],
                                                          scalar2=None, op0=ALU.mult),
                         reads=[b_bs, b_pow2], writes=[b_bs])
                    P.op("dve", lambda e: e.tensor_scalar(out=bs[:, NTHR:NTHR + 1], in0=bs[:, LO:LO + 1], scalar1=-1.0,
                                                          scalar2=None, op0=ALU.mult), reads=[b_bs], writes=[b_bs])
                    for kk in range(KBIS):
                        P.op("dve", lambda e, kk=kk: e.tensor_scalar(out=bs[:, NCAND:NCAND + 1], in0=bs[:, NTHR:NTHR + 1],
                                                                     scalar1=bs[:, 8 + kk:9 + kk], scalar2=None,
                                                                     op0=ALU.subtract),
                             reads=[b_bs], writes=[b_bs])
                        P.op("act", lambda e: e.activation(out=MQ[:, 0:nk], in_=acc[:, 0:nk], func=AF.Sign,
                                                           bias=bs[:, NCAND:NCAND + 1], accum_out=bs[:, CNT:CNT + 1]),
                             reads=[b_acc, b_bs], writes=[bMQ, b_bs])
                        P.op("dve", lambda e: e.tensor_scalar(out=bs[:, MM:MM + 1], in0=bs[:, CNT:CNT + 1],
                                                              scalar1=float(2 * TOPK - 1 - nk), scalar2=1e30,
                                                              op0=ALU.is_lt, op1=ALU.mult),
                             reads=[b_bs], writes=[b_bs])
                        P.op("dve", lambda e: e.scalar_tensor_tensor(out=bs[:, NTHR:NTHR + 1], in0=bs[:, NCAND:NCAND + 1],
                                                                     scalar=bs[:, MM:MM + 1], in1=bs[:, NTHR:NTHR + 1],
                                                                     op0=ALU.add, op1=ALU.min),
                             reads=[b_bs], writes=[b_bs])
                        yield 1.5 + (nk + 224) / 1200.0
                    P.op("dve", lambda e: e.tensor_scalar(out=bs[:, THR:THR + 1], in0=bs[:, NTHR:NTHR + 1], scalar1=-1.0,
                                                          scalar2=None, op0=ALU.mult), reads=[b_bs], writes=[b_bs])
                P.op("dve", lambda e: e.tensor_scalar(out=MQ[:, 0:nk], in0=acc[:, 0:nk], scalar1=bs[:, THR:THR + 1],
                                                      scalar2=None, op0=ALU.is_ge),
                     reads=[b_acc, b_bs], writes=[bMQ])
                yield 2.5

            SBK = [4, 5, 6, 7]

            def back(s, i, par, sl):
                X = xt3[sl]; bX = b_xt3[sl]
                QA = qaT[par]; bQA = b_qaT[par]
                MQ = maskq[par]; bMQ = b_maskq[par]
                TH = thg[par]; bTH = b_thg[par]
                for gi, g0 in enumerate(range(0, i + 1, 4)):
                    g1 = min(i + 1, g0 + 4)
                    bank = SBK[gi % 4]

                    def mm_mt(e, g0=g0, g1=g1, bank=bank):
                        ins = None
                        for j in range(g0, g1):
                            ins = e.matmul(PS[:, bank * 512 + (j - g0) * 128: bank * 512 + (j - g0 + 1) * 128],
                                           lhsT=MQ[:, j * 128:(j + 1) * 128], rhs=ident[:], start=True, stop=True)
                        return ins
                    P.op("pe", mm_mt, reads=[bMQ, b_ident], writes=[PB[bank]])
                    P.op("act", lambda e, g0=g0, g1=g1, bank=bank: e.activation(
                        out=maskT[:, g0:g1, :],
                        in_=PS[:, bank * 512: bank * 512 + (g1 - g0) * 128].rearrange("p (j t) -> p j t", t=128),
                        func=AF.Copy), reads=[PB[bank]], writes=[b_maskT])
                    yield 1.0
                OV = PS[:, 1024:2048].rearrange("p (h w) -> p h w", h=4)
                for hf in range(2):
                    N = i + 1

                    def score_step(n, hf=hf):
                        u = n % NE
                        bank = SBK[u]

                        def mm_s(e, j=n, hf=hf, bank=bank):
                            ins = None
                            for cc in range(2):
                                ins = e.matmul(PS[:, bank * 512:(bank + 1) * 512], lhsT=ckvT[:, cc, j * 128:(j + 1) * 128],
                                               rhs=QA[:, cc, 4 * hf:4 * hf + 4, :], start=(cc == 0), stop=(cc == 1))
                            return ins
                        P.op("pe", mm_s, reads=[b_ckvT[n], bQA], writes=[PB[bank]])
                        P.op("act", lambda e, u=u, bank=bank: e.activation(
                            out=Eb[u][:], in_=PS[:, bank * 512:(bank + 1) * 512].rearrange("p (h t) -> p h t", h=4),
                            func=AF.Exp), reads=[PB[bank]], writes=[b_Eb[u]])
                        P.op("dve", lambda e, u=u, j=n: e.tensor_tensor(
                            out=Eb[u][:], in0=Eb[u][:], in1=maskT[:, j:j + 1, :].to_broadcast([128, 4, 128]), op=ALU.mult),
                            reads=[b_Eb[u], b_maskT], writes=[b_Eb[u]])

                    def pv_step(n, hf=hf):
                        u = n % NE

                        def mm_pv(e, j=n, hf=hf, u=u):
                            ins = None
                            for hl in range(4):
                                h = 4 * hf + hl
                                ins = e.matmul(PS[:, 1024 + hl * 256:1024 + hl * 256 + 129], lhsT=Eb[u][:, hl, :],
                                               rhs=Vaug[:, j, h, :], start=(j == 0 and hl % 2 == 0), stop=(j == i),
                                               skip_group_check=True)
                            return ins
                        P.op("pe", mm_pv, reads=[b_Eb[u], b_Vaug[n], b_Vones], writes=PB[2:4])

                    for n in range(min(NE - 1, N)):
                        score_step(n)
                    yield 2.5
                    for n in range(N):
                        if n + NE - 1 < N:
                            score_step(n + NE - 1)
                        pv_step(n)
                        yield 0.85
                    P.op("dve", lambda e: e.reciprocal(out=rden[:], in_=OV[:, :, 128:129]), reads=PB[2:4], writes=[b_rden])
                    P.op("dve", lambda e, hf=hf: e.tensor_tensor(
                        out=on[:, hf * 512:(hf + 1) * 512].rearrange("p (h v) -> p h v", h=4), in0=OV[:, :, 0:128],
                        in1=rden[:].to_broadcast([128, 4, 128]), op=ALU.mult),
                        reads=PB[2:4] + [b_rden], writes=[b_on[hf]])
                    yield 0.85
                P.op("dve", lambda e: e.scalar_tensor_tensor(out=ogB, in0=on, scalar=0.5, in1=TH,
                                                             op0=ALU.mult, op1=ALU.mult),
                     reads=b_on + [bTH], writes=[b_hn])
                transposes(hn, 8, 2048, lambda g0, g1: hnT[:, g0:g1, :], [b_hn], [b_hnT])
                yield 2.5

                def mm_y(e):
                    ins = None
                    for nb in range(2):
                        for c in range(8):
                            ins = e.matmul(PS[:, 3072 + nb * 512:3072 + (nb + 1) * 512], lhsT=hnT[:, c, :],
                                           rhs=wout0[:, c, nb * 512:(nb + 1) * 512], start=(c == 0), stop=(c == 7))
                    return ins
                P.op("pe", mm_y, reads=[b_hnT, b_wout0], writes=PB[6:8])
                P.op("dve", lambda e: e.tensor_tensor(out=X[:], in0=X[:], in1=PS[:, 3072:4096], op=ALU.add),
                     reads=[bX] + PB[6:8], writes=[bX])
                yield 2.5
                P.op("act", lambda e: e.activation(out=hn[:], in_=X[:], func=AF.Square, accum_out=st[:, 0:1]),
                     reads=[bX], writes=[b_hn, b_st])
                P.op("dve", lambda e: e.tensor_scalar(out=st[:, 0:1], in0=st[:, 0:1], scalar1=1.0 / D, scalar2=EPS,
                                                      op0=ALU.mult, op1=ALU.add), reads=[b_st], writes=[b_st])
                newton_rsqrt(P, st[:, 0:1], st[:, 28:31], [b_st], 1)
                P.op("dve", lambda e: e.scalar_tensor_tensor(out=hn[:], in0=X[:], scalar=st[:, 0:1], in1=gple[:],
                                                             op0=ALU.mult, op1=ALU.mult),
                     reads=[bX, b_st, b_gple], writes=[b_hn])
                transposes(hn, 8, 2048, lambda g0, g1: hnT[:, g0:g1, :], [b_hn], [b_hnT])
                yield 2.5

                def mm_u(e):
                    ins = None
                    for nb in range(2):
                        for c in range(8):
                            ins = e.matmul(PS[:, 1024 + nb * 512:1024 + (nb + 1) * 512], lhsT=hnT[:, c, :],
                                           rhs=wpg[:, c, nb * 512:(nb + 1) * 512], start=(c == 0), stop=(c == 7))
                    return ins
                P.op("pe", mm_u, reads=[b_hnT, b_wpg], writes=PB[2:4])
                P.op("act", lambda e: e.activation(out=ptb[:], in_=pt[par][:], func=AF.Copy),
                     reads=[b_pt[par]], writes=[b_ptb])
                transposes(ptb, 2, 3072, lambda g0, g1: pT[:, g0:g1, :], [b_ptb], [b_pT])
                yield 2.5

                def mm_pw(e):
                    ins = None
                    for nb in range(2):
                        for c in range(2):
                            ins = e.matmul(PS[:, 3072 + nb * 512:3072 + (nb + 1) * 512], lhsT=pT[:, c, :],
                                           rhs=wp[:, c, nb * 512:(nb + 1) * 512], start=(c == 0), stop=(c == 1))
                    return ins
                P.op("pe", mm_pw, reads=[b_pT, b_wp], writes=PB[6:8])
                P.op("act", lambda e: e.activation(out=tg, in_=PS[:, 1024:2048], func=AF.Tanh, scale=0.5),
                     reads=PB[2:4], writes=b_tg)
                P.op("dve", lambda e: e.scalar_tensor_tensor(out=tg, in0=tg, scalar=1.0, in1=PS[:, 3072:4096],
                                                             op0=ALU.add, op1=ALU.mult),
                     reads=b_tg + PB[6:8], writes=b_tg)
                P.op("dve", lambda e: e.scalar_tensor_tensor(out=X[:], in0=tg, scalar=0.5, in1=X[:],
                                                             op0=ALU.mult, op1=ALU.add),
                     reads=b_tg + [bX], writes=[bX])
                if mode == "full":
                    P.dma("sp", h1_d[s, i * 128:(i + 1) * 128, :], X[:], ds_x3[sl], reads=[bX], writes=[b_h1[s][i]])
                else:
                    P.dma("sp", out_d[s, i * 128:(i + 1) * 128, :], X[:], ds_x3[sl], reads=[bX])
                yield 2.5

            items = [(s, i) for s in range(NS) for i in range(NT)]

            def run_all(g):
                for _ in g:
                    pass

            def interleave3(gl):
                gens = [[g, 0.0, t] for (g, t) in gl]
                while gens:
                    g = min(gens, key=lambda x: x[1] / x[2])
                    try:
                        c = next(g[0])
                        g[1] += (c if c else 1.0)
                    except StopIteration:
                        gens.remove(g)

            def idxbis_total(i):
                nk = (i + 1) * 128
                t = 5.0 + 0.7 * 8 * ((nk + 511) // 512)
                if nk > TOPK:
                    t += KBIS * (1.5 + (nk + 224) / 1200.0)
                return t

            def back_total(i):
                return 1.0 * ((i + 4) // 4) + 2 * (0.85 * (i + 2) + 2.5) + 2.5 * 5

            PROJ_T = 32.0

            def loadx(g):
                s_, i_ = items[g]
                P.dma("sp", xt3[g % 3][:], x_d[s_, i_ * 128:(i_ + 1) * 128, :], ds_x3[g % 3], writes=[b_xt3[g % 3]])

            def loadp(g):
                s_, i_ = items[g]
                P.dma("sp", pt[g % 2][:], p_d[0, s_, i_ * 128:(i_ + 1) * 128, :], ds_p[g % 2], writes=[b_pt[g % 2]])

            NI = len(items)
            for g in range(min(3, NI)):
                loadx(g)
            for g in range(min(2, NI)):
                loadp(g)
            for s_ in range(NS):
                g0 = s_ * NT
                run_all(proj(s_, 0, g0 % 2, g0 % 3))
                run_all(projB(s_, 0, g0 % 2))
                gl = [(idxbis(s_, 0, g0 % 2), idxbis_total(0))]
                if NT > 1:
                    gl.append((proj(s_, 1, (g0 + 1) % 2, (g0 + 1) % 3), PROJ_T))
                interleave3(gl)
                for m in range(NT):
                    g = g0 + m
                    if m + 1 < NT:
                        run_all(projB(s_, m + 1, (g + 1) % 2))
                    gl = [(back(s_, m, g % 2, g % 3), back_total(m))]
                    if m + 1 < NT:
                        gl.append((idxbis(s_, m + 1, (g + 1) % 2), idxbis_total(m + 1)))
                    if m + 2 < NT:
                        gl.append((proj(s_, m + 2, (g + 2) % 2, (g + 2) % 3), PROJ_T))
                    interleave3(gl)
                    if g + 3 < NI:
                        loadx(g + 3)
                    if g + 2 < NI:
                        loadp(g + 2)

        def layer1a(es1):
            sb = lambda name, shape, dt: es1.enter_context(nc.sbuf_tensor("sb_" + name, list(shape), dt))
            w1in = sb("w1in", [128, 8, 6144], BF16); b_w1in = [B("w1in%d" % c) for c in range(3)]
            for c in range(3):
                P.dma("pool", w1in[:, :, c * 2048:(c + 1) * 2048], w1in_d[:, :, c * 2048:(c + 1) * 2048], ds_w,
                      writes=[b_w1in[c]])
            P.dma("sp", gpre[:], vec_d[2], ds_v, writes=[b_gpre])
            rot = [sb("rot%d" % k, [128, 4, 4, 128], F32) for k in range(2)]
            b_rot = [B("rot%d" % k) for k in range(2)]
            ds_r = [P.dsem("r%d" % k) for k in range(2)]
            ds_g = [P.dsem("og%d" % k) for k in range(2)]
            Uh = sb("Uh", [128, 2, 4, 512], F32); b_Uh = B("Uh")
            Ubf = sb("Ubf", [128, 2, 4, 512], BF16); b_Ubf = B("Ubf")
            qr = [sb("qr%d" % k, [128, 1024], BF16) for k in range(2)]; b_qr = [B("qr%d" % k) for k in range(2)]
            kr = [sb("kr%d" % k, [128, 1024], BF16) for k in range(2)]; b_kr = [B("kr%d" % k) for k in range(2)]
            vb = [sb("vb%d" % k, [128, 2048], BF16) for k in range(2)]; b_vb = [B("vb%d" % k) for k in range(2)]
            thb = [th, sb("thb1", [128, 2048], BF16)]; b_thb = [b_th, B("thb1")]
            ogb = [sb("ogb%d" % k, [128, 2048], BF16) for k in range(2)]; b_ogb = [B("ogb%d" % k) for k in range(2)]
            qkT = sb("qkT", [128, 16, 128], BF16); b_qkT = B("qkT")
            rtm = sb("rtm", [128, 2048], F32); b_rt = [B("rtm%d" % k) for k in range(4)]
            rt_ = [rtm[:, q * 512:(q + 1) * 512] for q in range(4)]
            stB = sb("stB", [128, 48], F32); b_stB = B("stB")
            sTm = sb("sTm", [128, 4, 128], BF16); b_sTm = B("sTm")

            def front(s, i, par):
                X = xt[par]; bX = b_xt[par]
                R = rot[par]; bR = b_rot[par]
                P.op("act", lambda e: e.activation(out=hn[:], in_=X[:], func=AF.Square, accum_out=st[:, 0:1]),
                     reads=[bX], writes=[b_hn, b_st])
                P.op("dve", lambda e: e.tensor_scalar(out=st[:, 0:1], in0=st[:, 0:1], scalar1=1.0 / D, scalar2=EPS,
                                                      op0=ALU.mult, op1=ALU.add), reads=[b_st], writes=[b_st])
                newton_rsqrt(P, st[:, 0:1], st[:, 28:31], [b_st], 1)
                P.op("dve", lambda e: e.scalar_tensor_tensor(out=hn[:], in0=X[:], scalar=st[:, 0:1], in1=gpre[:],
                                                             op0=ALU.mult, op1=ALU.mult),
                     reads=[bX, b_st, b_gpre], writes=[b_hn])
                yield 4.0
                transposes(hn, 8, 0, lambda g0, g1: hnT[:, g0:g1, :], [b_hn], [b_hnT])
                yield 2.0

                def mm_in(e, col0, nb):
                    ins = None
                    for c in range(8):
                        ins = e.matmul(PS[:, nb * 512:(nb + 1) * 512], lhsT=hnT[:, c, :],
                                       rhs=w1in[:, c, col0 + nb * 512:col0 + (nb + 1) * 512],
                                       start=(c == 0), stop=(c == 7))
                    return ins
                for nb in range(4):
                    P.op("pe", lambda e, nb=nb: mm_in(e, 0, nb), reads=[b_hnT, b_w1in[0]], writes=[PB[nb]])

                def v4(t):
                    return t.rearrange("p (h m) -> p h m", h=4)
                for (zc0, ti, dstt, bd) in ((0, 0, qr[par], b_qr[par]), (1024, 2, kr[par], b_kr[par])):
                    Z = PS[:, zc0:zc0 + 1024].rearrange("p (h m two) -> p h m two", h=4, two=2)
                    Dv = dstt[:].rearrange("p (h m two) -> p h m two", h=4, two=2)
                    C = R[:, ti, :, :]
                    S = R[:, ti + 1, :, :]
                    zb = pbk(zc0, zc0 + 1024)
                    P.op("dve", lambda e, Z=Z, C=C: e.tensor_tensor(out=v4(rt_[0]), in0=Z[:, :, :, 0], in1=C, op=ALU.mult),
                         reads=zb + [bR], writes=[b_rt[0]])
                    P.op("dve", lambda e, Z=Z, S=S: e.tensor_tensor(out=v4(rt_[1]), in0=Z[:, :, :, 1], in1=S, op=ALU.mult),
                         reads=zb + [bR], writes=[b_rt[1]])
                    P.op("dve", lambda e, Dv=Dv: e.tensor_tensor(out=Dv[:, :, :, 0], in0=v4(rt_[0]), in1=v4(rt_[1]),
                                                                 op=ALU.subtract),
                         reads=[b_rt[0], b_rt[1]], writes=[bd])
                    P.op("dve", lambda e, Z=Z, C=C: e.tensor_tensor(out=v4(rt_[2]), in0=Z[:, :, :, 1], in1=C, op=ALU.mult),
                         reads=zb + [bR], writes=[b_rt[2]])
                    P.op("dve", lambda e, Z=Z, S=S: e.tensor_tensor(out=v4(rt_[3]), in0=Z[:, :, :, 0], in1=S, op=ALU.mult),
                         reads=zb + [bR], writes=[b_rt[3]])
                    P.op("dve", lambda e, Dv=Dv: e.tensor_tensor(out=Dv[:, :, :, 1], in0=v4(rt_[2]), in1=v4(rt_[3]),
                                                                 op=ALU.add),
                         reads=[b_rt[2], b_rt[3]], writes=[bd])
                yield 14.0
                for nb in range(4):
                    P.op("pe", lambda e, nb=nb: mm_in(e, 2048, nb), reads=[b_hnT, b_w1in[1]], writes=[PB[nb]])
                    P.op("act", lambda e, nb=nb: e.activation(out=vb[par][:, nb * 512:(nb + 1) * 512],
                                                              in_=PS[:, nb * 512:(nb + 1) * 512], func=AF.Copy),
                         reads=[PB[nb]], writes=[b_vb[par]])
                    yield 1.7
                for nb in range(4):
                    P.op("pe", lambda e, nb=nb: mm_in(e, 4096, nb), reads=[b_hnT, b_w1in[2]], writes=[PB[nb]])
                    P.op("act", lambda e, nb=nb: e.activation(out=thb[par][:, nb * 512:(nb + 1) * 512],
                                                              in_=PS[:, nb * 512:(nb + 1) * 512], func=AF.Tanh, scale=0.5),
                         reads=[PB[nb]], writes=[b_thb[par]])
                    P.op("dve", lambda e, nb=nb: e.scalar_tensor_tensor(
                        out=thb[par][:, nb * 512:(nb + 1) * 512], in0=thb[par][:, nb * 512:(nb + 1) * 512], scalar=1.0,
                        in1=PS[:, nb * 512:(nb + 1) * 512], op0=ALU.add, op1=ALU.mult),
                        reads=[b_thb[par], PB[nb]], writes=[b_thb[par]])
                    yield 1.7

            def back(s, i, par):
                QR = qr[par]; KR = kr[par]; VB = vb[par]; TH = thb[par]; OG = ogb[par]
                bQR = b_qr[par]; bKR = b_kr[par]; bVB = b_vb[par]; bTH = b_thb[par]; bOG = b_ogb[par]
                transposes(QR, 8, 2048, lambda g0, g1: qkT[:, g0:g1, :], [bQR], [b_qkT])
                yield 2.0
                transposes(KR, 8, 3072, lambda g0, g1: qkT[:, 8 + g0:8 + g1, :], [bKR], [b_qkT])
                yield 2.0
                def mm_st(e):
                    ins = None
                    for h in range(4):
                        for dc in range(2):
                            ins = e.matmul(PS[:, 2048 + h * 128:2048 + (h + 1) * 128], lhsT=qkT[:, 8 + 2 * h + dc, :],
                                           rhs=qkT[:, 2 * h + dc, :], start=(dc == 0), stop=(dc == 1))
                    return ins
                P.op("pe", mm_st, reads=[b_qkT], writes=[PB[4]])
                P.op("dve", lambda e: e.tensor_tensor(out=sTm[:], in0=PS[:, 2048:2560].rearrange("p (h t) -> p h t", h=4),
                                                      in1=causT[:].unsqueeze(1).to_broadcast([128, 4, 128]), op=ALU.mult),
                     reads=[PB[4], b_causT], writes=[b_sTm])
                yield 1.5
                for h in range(4):
                    def mm_o(e, h=h):
                        ins = e.matmul(PS[:, 2048 + h * 512:2048 + (h + 1) * 512], lhsT=sTm[:, h, :],
                                       rhs=VB[:, h * 512:(h + 1) * 512], start=True, stop=(i == 0))
                        if i > 0:
                            for dc in range(2):
                                ins = e.matmul(PS[:, 2048 + h * 512:2048 + (h + 1) * 512], lhsT=qkT[:, 2 * h + dc, :],
                                               rhs=Ubf[:, dc, h, :], start=False, stop=(dc == 1))
                        return ins
                    P.op("pe", mm_o, reads=[b_sTm, bVB, b_qkT] + ([b_Ubf] if i > 0 else []), writes=[PB[4 + h]])
                    oc = 2048 + h * 512
                    P.op("act", lambda e, h=h, oc=oc: e.activation(out=OG[:, h * 512:(h + 1) * 512], in_=PS[:, oc:oc + 512],
                                                                   func=AF.Copy, accum_out=stB[:, 12 + h:13 + h]),
                         reads=[PB[4 + h]], writes=[bOG, b_stB])
                    P.op("act", lambda e, h=h, oc=oc: e.activation(out=OG[:, h * 512:(h + 1) * 512], in_=PS[:, oc:oc + 512],
                                                                   func=AF.Square, accum_out=stB[:, 16 + h:17 + h]),
                         reads=[PB[4 + h]], writes=[bOG, b_stB])
                    yield 1.5
                if i + 1 < NT:
                    for h in range(4):
                        pc = (h % 2) * 1024

                        def mm_d(e, h=h, pc=pc):
                            ins = None
                            for dc in range(2):
                                ins = e.matmul(PS[:, pc + dc * 512:pc + (dc + 1) * 512],
                                               lhsT=KR[:, h * 256 + dc * 128:h * 256 + (dc + 1) * 128],
                                               rhs=VB[:, h * 512:(h + 1) * 512], start=True, stop=True)
                            return ins
                        P.op("pe", mm_d, reads=[bKR, bVB], writes=pbk(pc, pc + 1024))
                        Dl = PS[:, pc:pc + 1024].rearrange("p (c v) -> p c v", c=2)
                        if i == 0:
                            P.op("dve", lambda e, h=h, Dl=Dl: e.tensor_copy(out=Uh[:, :, h, :], in_=Dl),
                                 reads=pbk(pc, pc + 1024), writes=[b_Uh])
                        else:
                            P.op("dve", lambda e, h=h, Dl=Dl: e.scalar_tensor_tensor(
                                out=Uh[:, :, h, :], in0=Uh[:, :, h, :], scalar=float(gch[h]), in1=Dl,
                                op0=ALU.mult, op1=ALU.add), reads=pbk(pc, pc + 1024) + [b_Uh], writes=[b_Uh])
                        P.op("act", lambda e, h=h: e.activation(out=Ubf[:, :, h, :], in_=Uh[:, :, h, :], func=AF.Copy,
                                                                scale=float(gch[h])),
                             reads=[b_Uh], writes=[b_Ubf])
                        yield 2.5

                P.op("dve", lambda e: e.tensor_scalar(out=stB[:, 12:16], in0=stB[:, 12:16], scalar1=1.0 / 512, scalar2=None,
                                                      op0=ALU.mult), reads=[b_stB], writes=[b_stB])
                P.op("dve", lambda e: e.tensor_tensor(out=stB[:, 20:24], in0=stB[:, 12:16], in1=stB[:, 12:16], op=ALU.mult),
                     reads=[b_stB], writes=[b_stB])
                P.op("dve", lambda e: e.scalar_tensor_tensor(out=stB[:, 16:20], in0=stB[:, 16:20], scalar=1.0 / 512,
                                                             in1=stB[:, 20:24], op0=ALU.mult, op1=ALU.subtract),
                     reads=[b_stB], writes=[b_stB])
                P.op("dve", lambda e: e.tensor_scalar(out=stB[:, 16:20], in0=stB[:, 16:20], scalar1=1.0, scalar2=EPS,
                                                      op0=ALU.mult, op1=ALU.add), reads=[b_stB], writes=[b_stB])
                newton_rsqrt(P, stB[:, 16:20], stB[:, 32:44], [b_stB], 4)
                P.op("dve", lambda e: e.scalar_tensor_tensor(out=stB[:, 24:28], in0=stB[:, 12:16], scalar=-1.0, in1=stB[:, 16:20],
                                                             op0=ALU.mult, op1=ALU.mult), reads=[b_stB], writes=[b_stB])
                yield 5.0
                for h in range(4):
                    oc = 2048 + h * 512
                    P.op("act", lambda e, h=h, oc=oc: e.activation(out=PS[:, oc:oc + 512], in_=PS[:, oc:oc + 512],
                                                                   func=AF.Identity, scale=stB[:, 16 + h:17 + h],
                                                                   bias=stB[:, 24 + h:25 + h]),
                         reads=[PB[4 + h], b_stB], writes=[PB[4 + h]])
                    P.op("dve", lambda e, h=h, oc=oc: e.scalar_tensor_tensor(
                        out=OG[:, h * 512:(h + 1) * 512], in0=PS[:, oc:oc + 512], scalar=0.5,
                        in1=TH[:, h * 512:(h + 1) * 512], op0=ALU.mult, op1=ALU.mult),
                        reads=[PB[4 + h], bTH], writes=[bOG])
                    yield 1.2
                if mode == "l1a":
                    P.op("act", lambda e: e.activation(out=xt[par][:], in_=OG[:, 0:1024], func=AF.Copy), reads=[bOG], writes=[b_xt[par]])
                    P.dma("sp", out_d[s, i * 128:(i + 1) * 128, :], xt[par][:], ds_x[par], reads=[b_xt[par]])
                else:
                    P.dma("sp", og_d[s, i * 128:(i + 1) * 128, :], OG[:], ds_g[par], reads=[bOG], writes=[b_ogd[s][i]])
            src = h1_d if mode == "full" else x_d
            items = [(s, i) for s in range(NS) for i in range(NT)]

            def load1(n, k):
                s, i = items[n]
                load_tile(s, i, k, 1, src, [b_h1[s][i]] if mode == "full" else [])
                P.dma("sp", rot[k][:], rot_d[i], ds_r[k], writes=[b_rot[k]])

            def run_all(g):
                for _ in g:
                    pass

            def interleave(ga, gb):
                gens = [[ga, 0.0, 34.0], [gb, 0.0, 37.0]]
                while gens:
                    g = min(gens, key=lambda x: x[1] / x[2])
                    try:
                        c = next(g[0])
                        g[1] += (c if c else 1.0)
                    except StopIteration:
                        gens.remove(g)

            load1(0, 0)
            if len(items) > 1:
                load1(1, 1)
            run_all(front(items[0][0], items[0][1], 0))
            for n in range(len(items)):
                s, i = items[n]
                if n + 2 < len(items):
                    load1(n + 2, n % 2)
                gb = back(s, i, n % 2)
                if n + 1 < len(items):
                    interleave(gb, front(items[n + 1][0], items[n + 1][1], (n + 1) % 2))
                else:
                    run_all(gb)

        def layer1b(es2):
            sb = lambda name, shape, dt: es2.enter_context(nc.sbuf_tensor("sb_" + name, list(shape), dt))
            wout1 = sb("wout1", [128, 16, 1024], BF16); b_wout1 = B("wout1")
            wpg = sb("wpg1", [128, 8, 1024], BF16); b_wpg = B("wpg1")
            wp = sb("wp1", [128, 2, 1024], BF16); b_wp = B("wp1")
            gfin = sb("gfin", [128, D], F32); b_gfin = B("gfin")
            ogT16 = sb("ogT16", [128, 16, 128], BF16); b_ogT16 = B("ogT16")
            hn2 = sb("hn2", [128, D], BF16); b_hn2 = B("hn2")
            st2 = sb("st2", [128, 8], F32); b_st2 = B("st2")
            big = sb("big2", [128, 2048], F32)
            tg = big[:, 0:1024]
            t2 = big[:, 1024:2048]
            P.dma("pool", wout1[:], wout1_d, ds_w, writes=[b_wout1])
            P.dma("pool", wpg[:], wpg_d[1], ds_w, writes=[b_wpg])
            P.dma("pool", wp[:], wp_d[1], ds_w, writes=[b_wp])
            P.dma("sp", gple[:], vec_d[3], ds_v, writes=[b_gple])
            P.dma("sp", gfin[:], vec_d[4], ds_v, writes=[b_gfin])
            ogl = [sb("ogl%d" % k, [128, 2048], BF16) for k in range(2)]; b_ogl = [B("ogl%d" % k) for k in range(2)]
            ds_gl = [P.dsem("gl%d" % k) for k in range(2)]
            src = h1_d if mode == "full" else x_d
            xtb = xt + [sb("xt%d" % k, [128, D], F32) for k in (2, 3)]
            b_xtb = b_xt + [B("xt2"), B("xt3")]; ds_xb = ds_x + [P.dsem("x2"), P.dsem("x3")]
            ptb3 = pt + [sb("pt%d" % k, [128, 256], F32) for k in (2, 3)]
            b_ptb3 = b_pt + [B("pt2"), B("pt3")]; ds_pb = ds_p + [P.dsem("p2"), P.dsem("p3")]

            def loadx(s, i, k):
                rd = [b_h1[s][i]] if mode == "full" else []
                P.dma("sp", xtb[k][:], src[s, i * 128:(i + 1) * 128, :], ds_xb[k], reads=rd, writes=[b_xtb[k]])
                P.dma("sp", ptb3[k][:], p_d[1, s, i * 128:(i + 1) * 128, :], ds_pb[k], writes=[b_ptb3[k]])

            def loadog(s, i, k):
                P.dma("sp", ogl[k][:], og_d[s, i * 128:(i + 1) * 128, :], ds_gl[k], reads=[b_ogd[s][i]], writes=[b_ogl[k]])

            def frontB(s, i, par, sl):
                X = xtb[sl]; bX = b_xtb[sl]
                for g0 in range(0, 16, 4):
                    transposes(ogl[par][:, g0 * 128:(g0 + 4) * 128], 4, g0 * 128,
                               lambda a0, a1, g0=g0: ogT16[:, g0 + a0:g0 + a1, :], [b_ogl[par]], [b_ogT16])
                    yield 1.0

                for nb in range(2):
                    def mm_y1(e, nb=nb):
                        ins = None
                        for c in range(16):
                            ins = e.matmul(PS[:, nb * 512:(nb + 1) * 512], lhsT=ogT16[:, c, :],
                                           rhs=wout1[:, c, nb * 512:(nb + 1) * 512], start=(c == 0), stop=(c == 15))
                        return ins
                    P.op("pe", mm_y1, reads=[b_ogT16, b_wout1], writes=[PB[nb]])
                    yield 3.5
                P.op("dve", lambda e: e.tensor_tensor(out=X[:], in0=X[:], in1=PS[:, 0:1024], op=ALU.add),
                     reads=[bX] + PB[0:2], writes=[bX])
                yield 1.0

            def backB(s, i, par, sl):
                X = xtb[sl]; bX = b_xtb[sl]
                P.op("act", lambda e: e.activation(out=hn[:], in_=X[:], func=AF.Square, accum_out=st[:, 0:1]),
                     reads=[bX], writes=[b_hn, b_st])
                rstd_from_ss(0, 1, 1.0 / D)
                P.op("dve", lambda e: e.scalar_tensor_tensor(out=hn[:], in0=X[:], scalar=st[:, 0:1], in1=gple[:],
                                                             op0=ALU.mult, op1=ALU.mult),
                     reads=[bX, b_st, b_gple], writes=[b_hn])
                yield 5.0
                transposes(hn, 8, 2048, lambda g0, g1: hnT[:, g0:g1, :], [b_hn], [b_hnT])
                yield 2.0

                def mm_u(e):
                    ins = None
                    for nb in range(2):
                        for c in range(8):
                            ins = e.matmul(PS[:, 3072 + nb * 512:3072 + (nb + 1) * 512], lhsT=hnT[:, c, :],
                                           rhs=wpg[:, c, nb * 512:(nb + 1) * 512], start=(c == 0), stop=(c == 7))
                    return ins
                P.op("pe", mm_u, reads=[b_hnT, b_wpg], writes=PB[6:8])
                P.op("act", lambda e: e.activation(out=ptb[:], in_=ptb3[sl][:], func=AF.Copy),
                     reads=[b_ptb3[sl]], writes=[b_ptb])
                transposes(ptb, 2, 2048, lambda g0, g1: pT[:, g0:g1, :], [b_ptb], [b_pT])
                yield 4.0

                def mm_pw(e):
                    ins = None
                    for nb in range(2):
                        for c in range(2):
                            ins = e.matmul(PS[:, 2048 + nb * 512:2048 + (nb + 1) * 512], lhsT=pT[:, c, :],
                                           rhs=wp[:, c, nb * 512:(nb + 1) * 512], start=(c == 0), stop=(c == 1))
                    return ins
                P.op("pe", mm_pw, reads=[b_pT, b_wp], writes=PB[4:6])
                P.op("act", lambda e: e.activation(out=tg, in_=PS[:, 3072:4096], func=AF.Tanh, scale=0.5),
                     reads=PB[6:8], writes=b_tg)
                P.op("dve", lambda e: e.scalar_tensor_tensor(out=t2, in0=tg, scalar=1.0, in1=PS[:, 2048:3072],
                                                             op0=ALU.add, op1=ALU.mult),
                     reads=b_tg + PB[4:6], writes=b_t2)
                P.op("dve", lambda e: e.scalar_tensor_tensor(out=X[:], in0=t2, scalar=0.5, in1=X[:],
                                                             op0=ALU.mult, op1=ALU.add),
                     reads=b_t2 + [bX], writes=[bX])
                yield 5.0

            def backB2(s, i, par, sl):
                X = xtb[sl]; bX = b_xtb[sl]
                P.op("act", lambda e: e.activation(out=hn2[:], in_=X[:], func=AF.Square, accum_out=st2[:, 0:1]),
                     reads=[bX], writes=[b_hn2, b_st2])
                P.op("dve", lambda e: e.tensor_scalar(out=st2[:, 0:1], in0=st2[:, 0:1], scalar1=1.0 / D, scalar2=EPS,
                                                      op0=ALU.mult, op1=ALU.add), reads=[b_st2], writes=[b_st2])
                yield 2.0
                newton_rsqrt(P, st2[:, 0:1], st2[:, 4:7], [b_st2], 1)
                yield 3.0
                P.op("dve", lambda e: e.scalar_tensor_tensor(out=X[:], in0=X[:], scalar=st2[:, 0:1], in1=gfin[:],
                                                             op0=ALU.mult, op1=ALU.mult),
                     reads=[bX, b_st2, b_gfin], writes=[bX])
                P.dma("sp", out_d[s, i * 128:(i + 1) * 128, :], X[:], ds_xb[sl], reads=[bX])
                yield 2.0

            items = [(s, i) for s in range(NS) for i in range(NT)]

            def run_all(g):
                for _ in g:
                    pass

            def interleave(ga, gb, ta, tb):
                gens = [[ga, 0.0, ta], [gb, 0.0, tb]]
                while gens:
                    g = min(gens, key=lambda x: x[1] / x[2])
                    try:
                        c = next(g[0])
                        g[1] += (c if c else 1.0)
                    except StopIteration:
                        gens.remove(g)

            def interleave3(gl):
                gens = [[g, 0.0, t] for (g, t) in gl if g is not None]
                while gens:
                    g = min(gens, key=lambda x: x[1] / x[2])
                    try:
                        c = next(g[0])
                        g[1] += (c if c else 1.0)
                    except StopIteration:
                        gens.remove(g)

            NI = len(items)
            for n in range(min(3, NI)):
                loadx(items[n][0], items[n][1], n % 4)
            for n in range(min(2, NI)):
                loadog(items[n][0], items[n][1], n % 2)
            run_all(frontB(items[0][0], items[0][1], 0, 0))
            for m in range(NI + 1):
                if m + 2 < NI:
                    loadog(items[m + 2][0], items[m + 2][1], m % 2)
                gl = []
                if m - 1 >= 0:
                    gl.append((backB2(items[m - 1][0], items[m - 1][1], (m - 1) % 2, (m - 1) % 4), 7.0))
                if m < NI:
                    gl.append((backB(items[m][0], items[m][1], m % 2, m % 4), 16.0))
                if m + 1 < NI:
                    gl.append((frontB(items[m + 1][0], items[m + 1][1], (m + 1) % 2, (m + 1) % 4), 12.0))
                interleave3(gl)
                if m + 3 < NI:
                    loadx(items[m + 3][0], items[m + 3][1], (m + 3) % 4)

        if mode in ("full", "l0"):
            with ExitStack() as es0:
                layer0(es0)
            P.barrier()
        if mode in ("full", "l1", "l1a"):
            with ExitStack() as es1:
                layer1a(es1)
            P.barrier()
        if mode in ("full", "l1"):
            with ExitStack() as es2:
                layer1b(es2)
        P.emit(final_waits=P.dsems)
    return nc


def _chunked(w, c):
    n = w.shape[1]
    return np.ascontiguousarray(w.reshape(c, 128, n).transpose(1, 0, 2))


def host_consts(L, KBIS):
    NT = L // 128
    i = np.arange(128)
    ident = np.eye(128, dtype=np.float32)
    causT = (i[None, :] >= i[:, None]).astype(np.float32)
    cbias = np.where(i[None, :] <= i[:, None], 0.0, -1e9).astype(np.float32)
    pow2 = np.broadcast_to((2.0 ** -(np.arange(KBIS) + 1.0)).astype(np.float32), (128, KBIS)).copy()
    pos = np.arange(L, dtype=np.float32)
    angle = (1.0 / (10000.0 ** np.linspace(0.0, 1.0, 128, dtype=np.float32))).astype(np.float32)
    theta = pos[:, None] * angle[None, :]
    cos = np.cos(theta).astype(np.float32).reshape(NT, 128, 128)
    sin = np.sin(theta).astype(np.float32).reshape(NT, 128, 128)
    gam = np.array([1.0 - 2.0 ** (-5.0 - h) for h in range(4)], dtype=np.float64)
    xi = gam[None, :] ** (i[:, None] + 1.0)
    ks = (gam[None, :] ** (-(i[:, None] + 1.0))) * (256.0 ** -0.5)
    rot = np.empty((NT, 128, 4, 4, 128), dtype=np.float32)
    rot[:, :, 0] = cos[:, :, None, :] * xi[None, :, :, None]
    rot[:, :, 1] = sin[:, :, None, :] * xi[None, :, :, None]
    rot[:, :, 2] = cos[:, :, None, :] * ks[None, :, :, None]
    rot[:, :, 3] = sin[:, :, None, :] * ks[None, :, :, None]
    return dict(ident=ident, causT=causT, cbias=cbias, pow2=pow2, rot=rot)


def host_weights(g_pre, a_w_in, a_g_q, a_g_kv, a_w_q_up, a_w_idx_q, a_g_ik, a_b_ik, a_w_uk, a_w_uv, a_w_out,
                 r_w_in, r_w_out, w_ple_gate, g_ple, w_ple, g_final):
    f = np.float32
    rep = lambda v: np.broadcast_to(np.asarray(v, f)[None, :], (128, v.shape[0]))
    vecs = np.stack([rep(g_pre[0]), rep(g_ple[0]), rep(g_pre[1]), rep(g_ple[1]), rep(g_final)]).astype(f)
    vsm = np.concatenate([rep(a_g_q[0]), rep(a_g_kv[0]), rep(a_g_ik[0]), rep(a_b_ik[0])], axis=1).astype(f)
    w = np.asarray(a_w_in[0], f)
    wpad = np.zeros((1024, 1792), f)
    wpad[:, 0:384] = w[:, 0:384]
    wpad[:, 384:448] = w[:, 640:704]
    wpad[:, 448:456] = w[:, 704:712]
    wpad[:, 512:768] = w[:, 384:640]
    wpad[:, 768:1792] = w[:, 712:1736]
    wukT = np.ascontiguousarray(np.asarray(a_w_uk[0], f).transpose(2, 0, 1))
    wuv = np.asarray(a_w_uv[0], f).transpose(1, 0, 2).reshape(256, 1024)
    return dict(
        vecs=np.ascontiguousarray(vecs), vsm=np.ascontiguousarray(vsm),
        w0in=_chunked(wpad, 8), wq=_chunked(np.asarray(a_w_q_up[0], f), 3), wiq=_chunked(np.asarray(a_w_idx_q[0], f), 3),
        wukT=wukT, wuv=_chunked(wuv, 2), wout0=_chunked(np.asarray(a_w_out[0], f), 8),
        wpg=np.stack([_chunked(np.asarray(w_ple_gate[l], f), 8) for l in range(2)]),
        wp=np.stack([_chunked(np.asarray(w_ple[l], f), 2) for l in range(2)]),
        w1in=_chunked(np.asarray(r_w_in[0], f), 8), wout1=_chunked(np.asarray(r_w_out[0], f), 16),
    )


_NC_CACHE = {}


def run(x, p, wts, L, NS, ncores, KBIS=24, mode="full"):
    key = (L, NS, KBIS, mode)
    if key not in _NC_CACHE:
        _NC_CACHE[key] = build(L, NS, KBIS, mode)
    nc = _NC_CACHE[key]
    consts = host_consts(L, KBIS)
    in_maps = []
    for c in range(ncores):
        m = dict(wts)
        m.update(consts)
        m["x"] = np.ascontiguousarray(x[c * NS:(c + 1) * NS])
        m["p"] = np.ascontiguousarray(p[:, c * NS:(c + 1) * NS])
        in_maps.append(m)
    res = run_bass_kernel_spmd(nc, in_maps, core_ids=list(range(ncores)))
    return np.concatenate([r["out"] for r in res.results], axis=0)


def kernel(x, p, g_pre, a_w_in, a_g_q, a_g_kv, a_w_q_up, a_w_idx_q, a_g_ik, a_b_ik, a_w_uk, a_w_uv, a_w_out,
           r_w_in, r_w_out, w_ple_gate, g_ple, w_ple, g_final):
    x = np.asarray(x, np.float32)
    p = np.asarray(p, np.float32)
    wts = host_weights(g_pre, a_w_in, a_g_q, a_g_kv, a_w_q_up, a_w_idx_q, a_g_ik, a_b_ik, a_w_uk, a_w_uv, a_w_out,
                       r_w_in, r_w_out, w_ple_gate, g_ple, w_ple, g_final)
    Bn, L, _ = x.shape
    out = run(x, p, wts, L, Bn // 8, 8)
    return out.astype(np.float32)
```

```python
import numpy as np
import concourse.bass as bass
import concourse.mybir as mybir
from concourse.bass_utils import run_bass_kernel_spmd
from contextlib import ExitStack

F32 = mybir.dt.float32
BF16 = mybir.dt.bfloat16
AF = mybir.ActivationFunctionType
ALU = mybir.AluOpType
AX = mybir.AxisListType

D = 1024
EPS = 1e-6
COMPUTE = ("pe", "act", "dve", "pool")


class Buf:
    __slots__ = ("name", "writer", "readers")

    def __init__(self, name):
        self.name = name
        self.writer = None
        self.readers = []


class DmaSem:
    __slots__ = ("sem", "count")

    def __init__(self, sem):
        self.sem = sem
        self.count = 0


class Op:
    __slots__ = ("eng", "fn", "waits", "sig", "count", "dsem", "dval", "is_dma")

    def __init__(self, eng, fn):
        self.eng = eng
        self.fn = fn
        self.waits = []
        self.sig = False
        self.count = None
        self.dsem = None
        self.dval = None
        self.is_dma = False


class Prog:
    def __init__(self, nc, es, strict=False):
        self.nc = nc
        self.es = es
        self.strict = strict
        self.ops = {e: [] for e in ("pe", "act", "dve", "pool", "sp")}
        self.esem = {e: es.enter_context(nc.semaphore("s_" + e)) for e in COMPUTE}
        self.dsems = []
        self.pending = {e: [] for e in self.ops}

    def sb(self, name, shape, dt):
        return self.es.enter_context(self.nc.sbuf_tensor("sb_" + name, list(shape), dt))

    def dsem(self, name):
        d = DmaSem(self.es.enter_context(self.nc.semaphore("d_" + name)))
        self.dsems.append(d)
        return d

    def _dep(self, op, prod):
        if prod is None or prod is op:
            return
        if prod.is_dma:
            op.waits.append((prod.dsem, prod.dsem.count))
        else:
            prod.sig = True
            op.waits.append(prod)

    def barrier(self):
        lasts = []
        for e in COMPUTE:
            comp = [o for o in self.ops[e] if not o.is_dma]
            if comp:
                comp[-1].sig = True
                lasts.append(comp[-1])
        dm = [(d, d.count) for d in self.dsems if d.count > 0]
        for e in self.ops:
            self.pending[e] = list(lasts) + list(dm)

    def _track(self, op, reads, writes):
        for b in reads:
            w = b.writer
            if w is not None:
                if not (w.eng == op.eng and op.eng == "pe" and not w.is_dma and not op.is_dma):
                    self._dep(op, w)
        for b in writes:
            w = b.writer
            if w is not None and (w.is_dma or op.is_dma or w.eng != op.eng or (self.strict and op.eng != "pe")):
                self._dep(op, w)
            for r in b.readers:
                if r.is_dma or op.is_dma or r.eng != op.eng or (self.strict and op.eng != "pe"):
                    self._dep(op, r)
        for b in reads:
            b.readers.append(op)
        for b in writes:
            b.writer = op
            b.readers = []

    def op(self, eng, fn, reads=(), writes=()):
        o = Op(eng, fn)
        if self.pending[eng]:
            o.waits.extend(self.pending[eng])
            self.pending[eng] = []
        self._track(o, reads, writes)
        self.ops[eng].append(o)
        return o

    def dma(self, eng, out, in_, dsem, reads=(), writes=()):
        def fn(e):
            return e.dma_start(out=out, in_=in_)
        o = Op(eng, fn)
        o.is_dma = True
        o.dsem = dsem
        if self.pending[eng]:
            o.waits.extend(self.pending[eng])
            self.pending[eng] = []
        self._track(o, reads, writes)
        dsem.count += 16
        o.dval = dsem.count
        self.ops[eng].append(o)
        return o

    def emit(self, final_waits=()):
        nc = self.nc
        for e in COMPUTE:
            c = 0
            for o in self.ops[e]:
                if o.is_dma:
                    continue
                if o.sig:
                    c += 1
                o.count = c

        def run(engname, engobj):
            waited = {}
            for o in self.ops[engname]:
                for p in o.waits:
                    if isinstance(p, tuple):
                        sem, val = p[0].sem, p[1]
                    else:
                        if p.eng == engname and p.count is None:
                            continue
                        sem, val = self.esem[p.eng], p.count
                    key = id(sem)
                    if waited.get(key, 0) < val:
                        engobj.wait_ge(sem, val)
                        waited[key] = val
                ins = o.fn(engobj)
                if o.is_dma:
                    ins.then_inc(o.dsem.sem, 16)
                elif o.sig:
                    ins.then_inc(self.esem[engname], 1)
            if engname == "sp":
                for ds in final_waits:
                    engobj.wait_ge(ds.sem, ds.count)

        with nc.Block() as block:
            @block.tensor
            def _(e):
                run("pe", e)

            @block.scalar
            def _(e):
                run("act", e)

            @block.vector
            def _(e):
                run("dve", e)

            @block.gpsimd
            def _(e):
                run("pool", e)

            @block.sync
            def _(e):
                run("sp", e)


I32 = mybir.dt.int32


def newton_rsqrt(P, v, tmp, bufs, n):
    hv = tmp[:, 0:n]
    y = tmp[:, n:2 * n]
    t = tmp[:, 2 * n:3 * n]
    P.op("dve", lambda e: e.tensor_scalar(out=hv, in0=v, scalar1=-0.5, scalar2=None, op0=ALU.mult), reads=bufs, writes=bufs)
    P.op("dve", lambda e: e.tensor_scalar(out=y.bitcast(I32), in0=v.bitcast(I32), scalar1=-0.5, scalar2=1597463007.0,
                                          op0=ALU.mult, op1=ALU.add), reads=bufs, writes=bufs)
    for it in range(3):
        dst = v if it == 2 else y
        if n == 1:
            P.op("dve", lambda e: e.scalar_tensor_tensor(out=t, in0=y, scalar=y, in1=hv, op0=ALU.mult, op1=ALU.mult),
                 reads=bufs, writes=bufs)
        else:
            P.op("dve", lambda e: e.tensor_tensor(out=t, in0=y, in1=y, op=ALU.mult), reads=bufs, writes=bufs)
            P.op("dve", lambda e: e.tensor_tensor(out=t, in0=t, in1=hv, op=ALU.mult), reads=bufs, writes=bufs)
        P.op("dve", lambda e, dst=dst: e.scalar_tensor_tensor(out=dst, in0=t, scalar=1.5, in1=y, op0=ALU.add, op1=ALU.mult),
             reads=bufs, writes=bufs)


def build(L=2048, NS=2, KBIS=24, mode="full", strict=False):
    NT = L // 128
    TOPK = min(256, L // 4)
    nc = bass.Bass("TRN2", target_bir_lowering=False)

    def din(name, shape):
        return nc.dram_tensor(name, list(shape), F32, kind="ExternalInput").ap()

    x_d = din("x", [NS, L, D])
    p_d = din("p", [2, NS, L, 256])
    vec_d = din("vecs", [5, 128, D])
    vsm_d = din("vsm", [128, 768])
    w0in_d = din("w0in", [128, 8, 1792])
    wq_d = din("wq", [128, 3, 1024])
    wiq_d = din("wiq", [128, 3, 512])
    wukT_d = din("wukT", [128, 8, 256])
    wuv_d = din("wuv", [128, 2, 1024])
    wout0_d = din("wout0", [128, 8, 1024])
    wpg_d = din("wpg", [2, 128, 8, 1024])
    wp_d = din("wp", [2, 128, 2, 1024])
    w1in_d = din("w1in", [128, 8, 6144])
    wout1_d = din("wout1", [128, 16, 1024])
    ident_d = din("ident", [128, 128])
    causT_d = din("causT", [128, 128])
    cbias_d = din("cbias", [128, 128])
    pow2_d = din("pow2", [128, KBIS])
    rot_d = din("rot", [NT, 128, 4, 4, 128])
    out_d = nc.dram_tensor("out", [NS, L, D], F32, kind="ExternalOutput").ap()
    h1_d = nc.dram_tensor("h1s", [NS, L, D], F32, kind="Internal").ap()
    og_d = nc.dram_tensor("ogs", [NS, L, 2048], BF16, kind="Internal").ap()

    gam = [1.0 - 2.0 ** (-5.0 - h) for h in range(4)]
    gch = [g ** 128 for g in gam]

    es = ExitStack()
    with es:
        P = Prog(nc, es, strict)

        def B(name):
            return Buf(name)

        PS = es.enter_context(nc.psum_tensor("PS", [128, 4096], F32))
        PB = [B("bank%d" % k) for k in range(8)]

        def pbk(lo, hi):
            return PB[lo // 512:(hi - 1) // 512 + 1]

        sb = P.sb
        ident = sb("ident", [128, 128], BF16); b_ident = B("ident")
        causT = sb("causT", [128, 128], BF16); b_causT = B("causT")
        cbias = sb("cbias", [128, 128], F32); b_cbias = B("cbias")
        pow2 = sb("pow2", [128, KBIS], F32); b_pow2 = B("pow2")
        ds_c = P.dsem("const")
        P.dma("pool", ident[:], ident_d, ds_c, writes=[b_ident])
        P.dma("pool", causT[:], causT_d, ds_c, writes=[b_causT])
        ds_c2 = P.dsem("const2")
        P.dma("sp", cbias[:], cbias_d, ds_c2, writes=[b_cbias])
        P.dma("sp", pow2[:], pow2_d, ds_c2, writes=[b_pow2])

        gpre = sb("gpre", [128, D], F32); b_gpre = B("gpre")
        gple = sb("gple", [128, D], F32); b_gple = B("gple")
        ds_w = P.dsem("w")
        ds_v = P.dsem("v")
        xt = [sb("xt%d" % k, [128, D], F32) for k in range(2)]
        b_xt = [B("xt%d" % k) for k in range(2)]
        ds_x = [P.dsem("x%d" % k) for k in range(2)]
        pt = [sb("pt%d" % k, [128, 256], F32) for k in range(2)]
        b_pt = [B("pt%d" % k) for k in range(2)]
        ds_p = [P.dsem("p%d" % k) for k in range(2)]
        st = sb("st", [128, 32], F32); b_st = B("st")
        hn = sb("hn", [128, D], BF16); b_hn = B("hn")
        hnT = sb("hnT", [128, 8, 128], BF16); b_hnT = B("hnT")
        ptb = sb("ptb", [128, 256], BF16); b_ptb = B("ptb")
        pT = sb("pT", [128, 2, 128], BF16); b_pT = B("pT")
        b_bq = [B("bq%d" % k) for k in range(4)]
        b_tg = b_bq[0:2]
        b_t2 = b_bq[2:4]
        b_og = B("og")
        th = sb("th", [128, 2048], BF16); b_th = B("th")
        b_h1 = [[B("h1_%d_%d" % (s, i)) for i in range(NT)] for s in range(NS)]
        b_ogd = [[B("ogd_%d_%d" % (s, i)) for i in range(NT)] for s in range(NS)]

        def transposes(src, nchunk, pcol0, dst_view_fn, rb, wb):
            for g0 in range(0, nchunk, 4):
                g1 = min(nchunk, g0 + 4)
                c0 = pcol0 + g0 * 128
                c1 = pcol0 + g1 * 128
                banks = pbk(c0, c1)

                def mm(e, g0=g0, g1=g1, c0=c0):
                    ins = None
                    for c in range(g0, g1):
                        ins = e.matmul(PS[:, c0 + (c - g0) * 128: c0 + (c - g0 + 1) * 128],
                                       lhsT=src[:, c * 128:(c + 1) * 128], rhs=ident[:], start=True, stop=True)
                    return ins
                P.op("pe", mm, reads=rb + [b_ident], writes=banks)
                P.op("act", lambda e, g0=g0, g1=g1, c0=c0, c1=c1: e.activation(
                    out=dst_view_fn(g0, g1), in_=PS[:, c0:c1].rearrange("p (c t) -> p c t", t=128), func=AF.Copy),
                    reads=banks, writes=wb)

        def rstd_from_ss(col, n, scale):
            P.op("dve", lambda e: e.tensor_scalar(out=st[:, col:col + n], in0=st[:, col:col + n],
                                                  scalar1=scale, scalar2=EPS, op0=ALU.mult, op1=ALU.add),
                 reads=[b_st], writes=[b_st])
            newton_rsqrt(P, st[:, col:col + n], st[:, 28:28 + 3 * n], [b_st], n)

        def rmsnorm_T(xs, bx, gvec, bg):
            P.op("act", lambda e: e.activation(out=hn[:], in_=xs[:], func=AF.Square, accum_out=st[:, 0:1]),
                 reads=[bx], writes=[b_hn, b_st])
            rstd_from_ss(0, 1, 1.0 / D)
            P.op("dve", lambda e: e.scalar_tensor_tensor(out=hn[:], in0=xs[:], scalar=st[:, 0:1], in1=gvec[:],
                                                         op0=ALU.mult, op1=ALU.mult),
                 reads=[bx, b_st, bg], writes=[b_hn])
            transposes(hn, 8, 0, lambda g0, g1: hnT[:, g0:g1, :], [b_hn], [b_hnT])

        def load_tile(s, i, k, layer, src=None, rd=()):
            if src is None:
                src = x_d
            P.dma("sp", xt[k][:], src[s, i * 128:(i + 1) * 128, :], ds_x[k], reads=list(rd), writes=[b_xt[k]])
            P.dma("sp", pt[k][:], p_d[layer, s, i * 128:(i + 1) * 128, :], ds_p[k], writes=[b_pt[k]])

        def ple_stage(k, wpg, b_wpg, wp, b_wp, tg, t2):
            X = xt[k]; bX = b_xt[k]
            rmsnorm_T(X, bX, gple, b_gple)

            def mm_u(e):
                ins = None
                for nb in range(2):
                    for c in range(8):
                        ins = e.matmul(PS[:, 1024 + nb * 512:1024 + (nb + 1) * 512], lhsT=hnT[:, c, :],
                                       rhs=wpg[:, c, nb * 512:(nb + 1) * 512], start=(c == 0), stop=(c == 7))
                return ins
            P.op("pe", mm_u, reads=[b_hnT, b_wpg], writes=PB[2:4])
            P.op("act", lambda e: e.activation(out=ptb[:], in_=pt[k][:], func=AF.Copy),
                 reads=[b_pt[k]], writes=[b_ptb])
            transposes(ptb, 2, 3072, lambda g0, g1: pT[:, g0:g1, :], [b_ptb], [b_pT])

            def mm_pw(e):
                ins = None
                for nb in range(2):
                    for c in range(2):
                        ins = e.matmul(PS[:, 2048 + nb * 512:2048 + (nb + 1) * 512], lhsT=pT[:, c, :],
                                       rhs=wp[:, c, nb * 512:(nb + 1) * 512], start=(c == 0), stop=(c == 1))
                return ins
            P.op("pe", mm_pw, reads=[b_pT, b_wp], writes=PB[4:6])
            P.op("act", lambda e: e.activation(out=tg, in_=PS[:, 1024:2048], func=AF.Tanh, scale=0.5),
                 reads=PB[2:4], writes=b_tg)
            P.op("dve", lambda e: e.scalar_tensor_tensor(out=t2, in0=tg, scalar=1.0, in1=PS[:, 2048:3072],
                                                         op0=ALU.add, op1=ALU.mult),
                 reads=b_tg + PB[4:6], writes=b_t2)
            P.op("dve", lambda e: e.scalar_tensor_tensor(out=X[:], in0=t2, scalar=0.5, in1=X[:],
                                                         op0=ALU.mult, op1=ALU.add),
                 reads=b_t2 + [bX], writes=[bX])

        def layer0(es0):
            sb = lambda name, shape, dt: es0.enter_context(nc.sbuf_tensor("sb_" + name, list(shape), dt))
            w0in = sb("w0in", [128, 8, 1792], BF16); b_w0in = B("w0in")
            wq = sb("wq", [128, 3, 1024], BF16); b_wq = B("wq")
            wiq = sb("wiq", [128, 3, 512], BF16); b_wiq = B("wiq")
            wukT = sb("wukT", [128, 8, 256], BF16); b_wukT = B("wukT")
            wuv = sb("wuv", [128, 2, 1024], BF16); b_wuv = B("wuv")
            wout0 = sb("wout0", [128, 8, 1024], BF16); b_wout0 = B("wout0")
            wpg = sb("wpg0", [128, 8, 1024], BF16); b_wpg = B("wpg0")
            wp = sb("wp0", [128, 2, 1024], BF16); b_wp = B("wp0")
            vsm = sb("vsm", [128, 768], F32); b_vsm = B("vsm")
            P.dma("pool", w0in[:], w0in_d, ds_w, writes=[b_w0in])
            P.dma("sp", gpre[:], vec_d[0], ds_v, writes=[b_gpre])
            P.dma("sp", gple[:], vec_d[1], ds_v, writes=[b_gple])
            P.dma("sp", vsm[:], vsm_d, ds_v, writes=[b_vsm])
            P.dma("pool", wq[:], wq_d, ds_w, writes=[b_wq])
            P.dma("pool", wiq[:], wiq_d, ds_w, writes=[b_wiq])
            P.dma("pool", wukT[:], wukT_d, ds_w, writes=[b_wukT])
            P.dma("pool", wuv[:], wuv_d, ds_w, writes=[b_wuv])
            P.dma("pool", wout0[:], wout0_d, ds_w, writes=[b_wout0])
            P.dma("pool", wpg[:], wpg_d[0], ds_w, writes=[b_wpg])
            P.dma("pool", wp[:], wp_d[0], ds_w, writes=[b_wp])

            ckvT = sb("ckvT", [128, 2, L], BF16); b_ckvT = [B("ckvT%d" % j) for j in range(NT)]
            Vaug = sb("Vaug", [128, NT, 8, 129], BF16); b_Vaug = [B("Vaug%d" % j) for j in range(NT)]
            b_Vones = B("Vones")
            kxT = sb("kxT", [128, L], BF16); b_kxT = [B("kxT%d" % j) for j in range(NT)]
            hnF = sb("hnF", [128, D], BF16); b_hnF = B("hnF")
            hnTF_1 = sb("hnTF", [128, 8, 128], BF16); b_hnTF_1 = B("hnTF")
            hnTF2 = [hnTF_1, hnTF_1]; b_hnTF2 = [b_hnTF_1, b_hnTF_1]
            stF = sb("stF", [128, 32], F32); b_stF = B("stF")
            cqn = sb("cqn", [128, 384], BF16); b_cqn = B("cqn")
            cqT = sb("cqT", [128, 3, 128], BF16); b_cqT = B("cqT")
            ckvn = sb("ckvn", [128, 256], BF16); b_ckvn = B("ckvn")
            kxt = sb("kxt", [128, 64], F32); b_kxt = B("kxt")
            kx2 = sb("kx2", [128, 128], BF16); b_kx2 = B("kx2")
            wi2 = [sb("wi%d" % k, [128, 24], F32) for k in range(2)]; b_wi2 = [B("wi%d" % k) for k in range(2)]
            xt3 = xt + [sb("xt2a", [128, D], F32)]; b_xt3 = b_xt + [B("xt2a")]; ds_x3 = ds_x + [P.dsem("x2a")]
            qT_1 = sb("qT", [128, 8, 128], BF16); b_qT_1 = B("qT")
            qT2 = [qT_1, qT_1]; b_qT2 = [b_qT_1, b_qT_1]
            qiT2 = [sb("qiT%d" % k, [128, 4, 128], BF16) for k in range(2)]; b_qiT2 = [B("qiT%d" % k) for k in range(2)]
            acc = sb("acc", [128, L], F32); b_acc = B("acc")
            rl = [sb("rl%d" % k, [128, 512], F32) for k in range(2)]; b_rl = [B("rl%d" % k) for k in range(2)]
            bs = sb("bs", [128, 8 + KBIS], F32); b_bs = B("bs")
            qaT = [sb("qaT%d" % k, [128, 2, 8, 128], BF16) for k in range(2)]; b_qaT = [B("qaT%d" % k) for k in range(2)]
            mq1 = sb("maskq", [128, L], BF16); bmq1 = B("maskq")
            maskq = [mq1, mq1]; b_maskq = [bmq1, bmq1]
            thg = [th[:, 0:1024], th[:, 1024:2048]]; b_thg = [B("thg0"), B("thg1")]
            maskT = sb("maskT", [128, NT, 128], BF16); b_maskT = B("maskT")
            NE = 4
            Eb = [sb("Eb%d" % k, [128, 4, 128], BF16) for k in range(NE)]; b_Eb = [B("Eb%d" % k) for k in range(NE)]
            rden = sb("rden", [128, 4, 1], F32); b_rden = B("rden")
            big0 = sb("big0", [128, 1024], F32)
            on = big0[:, :]; b_on = b_bq[0:2]
            tg = big0[:, :]
            ogB = hn[:, :]
            P.op("dve", lambda e: e.memset(Vaug[:, :, :, 128:129], 1.0), writes=[b_Vones])

            def rstdF(col, n, scale):
                P.op("dve", lambda e: e.tensor_scalar(out=stF[:, col:col + n], in0=stF[:, col:col + n],
                                                      scalar1=scale, scalar2=EPS, op0=ALU.mult, op1=ALU.add),
                     reads=[b_stF], writes=[b_stF])
                newton_rsqrt(P, stF[:, col:col + n], stF[:, 16:16 + 3 * n], [b_stF], n)

            def proj(s, i, par, sl):
                X = xt3[sl]; bX = b_xt3[sl]
                qT = qT2[par]; b_qT = b_qT2[par]
                hnTF = hnTF2[par]; b_hnTF = b_hnTF2[par]
                WI = wi2[par]; bWI = b_wi2[par]
                QI = qiT2[par]; bQI = b_qiT2[par]
                P.op("act", lambda e: e.activation(out=hnF[:], in_=X[:], func=AF.Square, accum_out=stF[:, 0:1]),
                     reads=[bX], writes=[b_hnF, b_stF])
                rstdF(0, 1, 1.0 / D)
                P.op("dve", lambda e: e.scalar_tensor_tensor(out=hnF[:], in0=X[:], scalar=stF[:, 0:1], in1=gpre[:],
                                                             op0=ALU.mult, op1=ALU.mult),
                     reads=[bX, b_stF, b_gpre], writes=[b_hnF])
                yield 2.5
                transposes(hnF, 8, 0, lambda g0, g1: hnTF[:, g0:g1, :], [b_hnF], [b_hnTF])
                yield 2.5
                def mm_za(e):
                    ins = None
                    for (pc, wc, n) in ((0, 0, 512), (512, 512, 256)):
                        for c in range(8):
                            ins = e.matmul(PS[:, pc:pc + n], lhsT=hnTF[:, c, :], rhs=w0in[:, c, wc:wc + n],
                                           start=(c == 0), stop=(c == 7))
                    return ins
                P.op("pe", mm_za, reads=[b_hnTF, b_w0in], writes=PB[0:2])
                ZQ, ZK, ZW, ZKV = 0, 384, 448, 512
                P.op("act", lambda e: e.activation(out=cqn[:], in_=PS[:, ZQ:ZQ + 384], func=AF.Square,
                                                   accum_out=stF[:, 4:5]),
                     reads=[PB[0]], writes=[b_cqn, b_stF])
                P.op("act", lambda e: e.activation(out=ckvn[:], in_=PS[:, ZKV:ZKV + 256], func=AF.Square,
                                                   accum_out=stF[:, 5:6]),
                     reads=[PB[1]], writes=[b_ckvn, b_stF])
                P.op("act", lambda e: e.activation(out=kx2[:, 0:64], in_=PS[:, ZK:ZK + 64], func=AF.Square,
                                                   accum_out=stF[:, 6:7]),
                     reads=[PB[0]], writes=[b_kx2, b_stF])
                P.op("act", lambda e: e.activation(out=kx2[:, 64:128], in_=PS[:, ZK:ZK + 64], func=AF.Copy,
                                                   accum_out=stF[:, 7:8]),
                     reads=[PB[0]], writes=[b_kx2, b_stF])
                P.op("dve", lambda e: e.tensor_scalar(out=stF[:, 4:5], in0=stF[:, 4:5], scalar1=1.0 / 384,
                                                      scalar2=None, op0=ALU.mult), reads=[b_stF], writes=[b_stF])
                P.op("dve", lambda e: e.tensor_scalar(out=stF[:, 5:6], in0=stF[:, 5:6], scalar1=1.0 / 256,
                                                      scalar2=None, op0=ALU.mult), reads=[b_stF], writes=[b_stF])
                P.op("dve", lambda e: e.tensor_scalar(out=stF[:, 7:8], in0=stF[:, 7:8], scalar1=1.0 / 64,
                                                      scalar2=None, op0=ALU.mult), reads=[b_stF], writes=[b_stF])
                P.op("dve", lambda e: e.tensor_tensor(out=stF[:, 8:9], in0=stF[:, 7:8], in1=stF[:, 7:8],
                                                      op=ALU.mult), reads=[b_stF], writes=[b_stF])
                P.op("dve", lambda e: e.scalar_tensor_tensor(out=stF[:, 6:7], in0=stF[:, 6:7], scalar=1.0 / 64,
                                                             in1=stF[:, 8:9], op0=ALU.mult, op1=ALU.subtract),
                     reads=[b_stF], writes=[b_stF])
                rstdF(4, 3, 1.0)
                P.op("dve", lambda e: e.scalar_tensor_tensor(out=cqn[:], in0=PS[:, ZQ:ZQ + 384], scalar=stF[:, 4:5],
                                                             in1=vsm[:, 0:384], op0=ALU.mult, op1=ALU.mult),
                     reads=[PB[0], b_stF, b_vsm], writes=[b_cqn])
                P.op("dve", lambda e: e.scalar_tensor_tensor(out=ckvn[:], in0=PS[:, ZKV:ZKV + 256], scalar=stF[:, 5:6],
                                                             in1=vsm[:, 384:640], op0=ALU.mult, op1=ALU.mult),
                     reads=[PB[1], b_stF, b_vsm], writes=[b_ckvn])
                P.op("dve", lambda e: e.tensor_scalar(out=kxt[:], in0=PS[:, ZK:ZK + 64], scalar1=stF[:, 7:8],
                                                      scalar2=stF[:, 6:7], op0=ALU.subtract, op1=ALU.mult),
                     reads=[PB[0], b_stF], writes=[b_kxt])
                P.op("dve", lambda e: e.tensor_tensor(out=kxt[:], in0=kxt[:], in1=vsm[:, 640:704], op=ALU.mult),
                     reads=[b_kxt, b_vsm], writes=[b_kxt])
                P.op("dve", lambda e: e.tensor_tensor(out=kx2[:, 0:64], in0=kxt[:], in1=vsm[:, 704:768], op=ALU.add),
                     reads=[b_kxt, b_vsm], writes=[b_kx2])
                P.op("dve", lambda e: e.tensor_tensor(out=kx2[:, 64:128], in0=kxt[:], in1=vsm[:, 704:768], op=ALU.add),
                     reads=[b_kxt, b_vsm], writes=[b_kx2])
                P.op("dve", lambda e: e.tensor_scalar(out=WI[:, 0:8], in0=PS[:, ZW:ZW + 8],
                                                      scalar1=float(8 ** -0.5 * 64 ** -0.5), scalar2=None, op0=ALU.mult),
                     reads=[PB[0]], writes=[bWI])
                P.op("act", lambda e: e.activation(out=WI[:, 8:16], in_=WI[:, 0:8], func=AF.Abs),
                     reads=[bWI], writes=[bWI])
                P.op("act", lambda e: e.activation(out=WI[:, 16:24], in_=WI[:, 0:8], func=AF.Sign),
                     reads=[bWI], writes=[bWI])
                yield 7.5
                transposes(cqn, 3, 0, lambda g0, g1: cqT[:, g0:g1, :], [b_cqn], [b_cqT])
                transposes(ckvn, 2, 512, lambda g0, g1: ckvT[:, g0:g1, i * 128:(i + 1) * 128], [b_ckvn], [b_ckvT[i]])
                transposes(kx2, 1, 768, lambda g0, g1: kxT[:, i * 128:(i + 1) * 128].unsqueeze(1), [b_kx2], [b_kxT[i]])
                yield 2.5
                def mm_v(e):
                    ins = None
                    for nb in range(2):
                        for c in range(2):
                            ins = e.matmul(PS[:, nb * 512:(nb + 1) * 512], lhsT=ckvT[:, c, i * 128:(i + 1) * 128],
                                           rhs=wuv[:, c, nb * 512:(nb + 1) * 512], start=(c == 0), stop=(c == 1))
                    return ins
                P.op("pe", mm_v, reads=[b_ckvT[i], b_wuv], writes=PB[0:2])
                P.op("dve", lambda e: e.tensor_copy(out=Vaug[:, i, :, 0:128],
                                                    in_=PS[:, 0:1024].rearrange("p (h v) -> p h v", h=8)),
                     reads=PB[0:2], writes=[b_Vaug[i]])
                yield 2.5
                def mm_q(e):
                    ins = None
                    for h in range(8):
                        for c in range(3):
                            ins = e.matmul(PS[:, h * 128:(h + 1) * 128], lhsT=wq[:, c, h * 128:(h + 1) * 128],
                                           rhs=cqT[:, c, :], start=(c == 0), stop=(c == 2))
                    return ins
                P.op("pe", mm_q, reads=[b_wq, b_cqT], writes=PB[0:2])
                P.op("dve", lambda e: e.tensor_copy(out=qT[:], in_=PS[:, 0:1024].rearrange("p (h t) -> p h t", h=8)),
                     reads=PB[0:2], writes=[b_qT])
                yield 2.5
                def mm_qi(e):
                    ins = None
                    for hp in range(4):
                        for c in range(3):
                            ins = e.matmul(PS[:, hp * 128:(hp + 1) * 128], lhsT=wiq[:, c, hp * 128:(hp + 1) * 128],
                                           rhs=cqT[:, c, :], start=(c == 0), stop=(c == 2))
                    return ins
                P.op("pe", mm_qi, reads=[b_wiq, b_cqT], writes=[PB[0]])
                P.op("act", lambda e: e.activation(out=QI[:], in_=PS[:, 0:512].rearrange("p (h t) -> p h t", h=4),
                                                   func=AF.Copy),
                     reads=[PB[0]], writes=[bQI])
                yield 2.5

            def projB(s, i, par):
                QA = qaT[par]; bQA = b_qaT[par]
                TH = thg[par]; bTH = b_thg[par]
                qT = qT2[par]; b_qT = b_qT2[par]
                hnTF = hnTF2[par]; b_hnTF = b_hnTF2[par]
                for cc in range(2):
                    bk = 1 - cc

                    def mm_qa(e, cc=cc, bk=bk):
                        ins = None
                        for hh in range(8):
                            hq, hr = divmod(hh, 4)
                            ins = e.matmul(PS[:, hq * 512 + hr * 128: hq * 512 + (hr + 1) * 128],
                                           lhsT=wukT[:, hh, cc * 128:(cc + 1) * 128], rhs=qT[:, hh, :], start=True, stop=True)
                        return ins
                    P.op("pe", mm_qa, reads=[b_wukT, b_qT], writes=PB[0:2])
                    P.op("dve", lambda e, cc=cc: e.tensor_scalar(out=QA[:, cc, :, :],
                                                                 in0=PS[:, 0:1024].rearrange("p (h t) -> p h t", h=8),
                                                                 scalar1=float(128 ** -0.5), scalar2=None, op0=ALU.mult),
                         reads=PB[0:2], writes=[bQA])
                    yield 2.5
                def mm_zb(e):
                    ins = None
                    for nb in range(2):
                        for c in range(8):
                            ins = e.matmul(PS[:, nb * 512:(nb + 1) * 512], lhsT=hnTF[:, c, :],
                                           rhs=w0in[:, c, 768 + nb * 512:768 + (nb + 1) * 512],
                                           start=(c == 0), stop=(c == 7))
                    return ins
                P.op("pe", mm_zb, reads=[b_hnTF, b_w0in], writes=PB[0:2])
                P.op("act", lambda e: e.activation(out=TH, in_=PS[:, 0:1024], func=AF.Tanh, scale=0.5),
                     reads=PB[0:2], writes=[bTH])
                P.op("dve", lambda e: e.scalar_tensor_tensor(out=TH, in0=TH, scalar=1.0, in1=PS[:, 0:1024],
                                                             op0=ALU.add, op1=ALU.mult),
                     reads=[bTH] + PB[0:2], writes=[bTH])
                yield 2.5

            def idxbis(s, i, par):
                nk = (i + 1) * 128
                MQ = maskq[par]; bMQ = b_maskq[par]
                WI = wi2[par]; bWI = b_wi2[par]
                QI = qiT2[par]; bQI = b_qiT2[par]
                nkb = (nk + 511) // 512
                for kb in range(nkb):
                    k0 = kb * 512
                    wd = min(512, nk - k0)
                    kbufs = b_kxT[k0 // 128:(k0 + wd) // 128]
                    for h in range(8):
                        bank = h % 2
                        r = h % 2
                        pr = (h % 2) * 64
                        P.op("pe", lambda e, h=h, bank=bank, pr=pr, k0=k0, wd=wd: e.matmul(
                            PS[:, bank * 512:bank * 512 + wd], lhsT=QI[pr:pr + 64, h // 2, :],
                            rhs=kxT[pr:pr + 64, k0:k0 + wd], start=True, stop=True),
                            reads=[bQI] + kbufs, writes=[PB[bank]])
                        P.op("act", lambda e, h=h, bank=bank, r=r, wd=wd: e.activation(
                            out=rl[r][:, 0:wd], in_=PS[:, bank * 512:bank * 512 + wd], func=AF.Relu,
                            scale=WI[:, 8 + h:9 + h]),
                            reads=[PB[bank], bWI], writes=[b_rl[r]])
                        if h == 0:
                            P.op("dve", lambda e, r=r, k0=k0, wd=wd: e.tensor_scalar(
                                out=acc[:, k0:k0 + wd], in0=rl[r][:, 0:wd], scalar1=WI[:, 16:17], scalar2=None,
                                op0=ALU.mult), reads=[b_rl[r], bWI], writes=[b_acc])
                        else:
                            P.op("dve", lambda e, h=h, r=r, k0=k0, wd=wd: e.scalar_tensor_tensor(
                                out=acc[:, k0:k0 + wd], in0=rl[r][:, 0:wd], scalar=WI[:, 16 + h:17 + h],
                                in1=acc[:, k0:k0 + wd], op0=ALU.mult, op1=ALU.add),
                                reads=[b_rl[r], bWI, b_acc], writes=[b_acc])
                        yield 0.7
                LO, HI, WW, THR, CAND, CNT, MM = 0, 1, 2, 3, 4, 5, 6
                P.op("dve", lambda e: e.tensor_reduce(out=bs[:, LO:LO + 1], in_=acc[:, 0:nk], axis=AX.X, op=ALU.min),
                     reads=[b_acc], writes=[b_bs])
                P.op("dve", lambda e: e.tensor_reduce(out=bs[:, HI:HI + 1], in_=acc[:, 0:nk], axis=AX.X, op=ALU.max),
                     reads=[b_acc], writes=[b_bs])
                P.op("dve", lambda e: e.tensor_tensor(out=acc[:, i * 128:(i + 1) * 128], in0=acc[:, i * 128:(i + 1) * 128],
                                                      in1=cbias[:], op=ALU.add),
                     reads=[b_acc, b_cbias], writes=[b_acc])
                P.op("dve", lambda e: e.tensor_copy(out=bs[:, THR:THR + 1], in_=bs[:, LO:LO + 1]),
                     reads=[b_bs], writes=[b_bs])
                yield 2.5
                if nk > TOPK:
                    NTHR, NCAND = WW, CAND
                    P.op("dve", lambda e: e.tensor_tensor(out=bs[:, 7:8], in0=bs[:, HI:HI + 1], in1=bs[:, LO:LO + 1],
                                                          op=ALU.subtract), reads=[b_bs], writes=[b_bs])
                    P.op("dve", lambda e: e.tensor_scalar(out=bs[:, 8:8 + KBIS], in0=pow2[:], scalar1=bs[:, 7:8],
                                                          scalar2=None, op0=ALU.mult),
                         reads=[b_bs, b_pow2], writes=[b_bs])
                    P.op("dve", lambda e: e.tensor_scalar(out=bs[:, NTHR:NTHR + 1], in0=bs[:, LO:LO + 1], scalar1=-1.0,
                                                          scalar2=None, op0=ALU.mult), reads=[b_bs], writes=[b_bs])
                    for kk in range(KBIS):
                        P.op("dve", lambda e, kk=kk: e.tensor_scalar(out=bs[:, NCAND:NCAND + 1], in0=bs[:, NTHR:NTHR + 1],
                                                                     scalar1=bs[:, 8 + kk:9 + kk], scalar2=None,
                                                                     op0=ALU.subtract),
                             reads=[b_bs], writes=[b_bs])
                        P.op("act", lambda e: e.activation(out=MQ[:, 0:nk], in_=acc[:, 0:nk], func=AF.Sign,
                                                           bias=bs[:, NCAND:NCAND + 1], accum_out=bs[:, CNT:CNT + 1]),
                             reads=[b_acc, b_bs], writes=[bMQ, b_bs])
                        P.op("dve", lambda e: e.tensor_scalar(out=bs[:, MM:MM + 1], in0=bs[:, CNT:CNT + 1],
                                                              scalar1=float(2 * TOPK - 1 - nk), scalar2=1e30,
                                                              op0=ALU.is_lt, op1=ALU.mult),
                             reads=[b_bs], writes=[b_bs])
                        P.op("dve", lambda e: e.scalar_tensor_tensor(out=bs[:, NTHR:NTHR + 1], in0=bs[:, NCAND:NCAND + 1],
                                                                     scalar=bs[:, MM:MM + 1], in1=bs[:, NTHR:NTHR + 1],
                                                                     op0=ALU.add, op1=ALU.min),
                             reads=[b_bs], writes=[b_bs])
                        yield 1.5 + (nk + 224) / 1200.0
                    P.op("dve", lambda e: e.tensor_scalar(out=bs[:, THR:THR + 1], in0=bs[:, NTHR:NTHR + 1], scalar1=-1.0,
                                                          scalar2=None, op0=ALU.mult), reads=[b_bs], writes=[b_bs])
                P.op("dve", lambda e: e.tensor_scalar(out=MQ[:, 0:nk], in0=acc[:, 0:nk], scalar1=bs[:, THR:THR + 1],
                                                      scalar2=None, op0=ALU.is_ge),
                     reads=[b_acc, b_bs], writes=[bMQ])
                yield 2.5

            SBK = [4, 5, 6, 7]

            def back(s, i, par, sl):
                X = xt3[sl]; bX = b_xt3[sl]
                QA = qaT[par]; bQA = b_qaT[par]
                MQ = maskq[par]; bMQ = b_maskq[par]
                TH = thg[par]; bTH = b_thg[par]
                for gi, g0 in enumerate(range(0, i + 1, 4)):
                    g1 = min(i + 1, g0 + 4)
                    bank = SBK[gi % 4]

                    def mm_mt(e, g0=g0, g1=g1, bank=bank):
                        ins = None
                        for j in range(g0, g1):
                            ins = e.matmul(PS[:, bank * 512 + (j - g0) * 128: bank * 512 + (j - g0 + 1) * 128],
                                           lhsT=MQ[:, j * 128:(j + 1) * 128], rhs=ident[:], start=True, stop=True)
                        return ins
                    P.op("pe", mm_mt, reads=[bMQ, b_ident], writes=[PB[bank]])
                    P.op("act", lambda e, g0=g0, g1=g1, bank=bank: e.activation(
                        out=maskT[:, g0:g1, :],
                        in_=PS[:, bank * 512: bank * 512 + (g1 - g0) * 128].rearrange("p (j t) -> p j t", t=128),
                        func=AF.Copy), reads=[PB[bank]], writes=[b_maskT])
                    yield 1.0
                OV = PS[:, 1024:2048].rearrange("p (h w) -> p h w", h=4)
                for hf in range(2):
                    N = i + 1

                    def score_step(n, hf=hf):
                        u = n % NE
                        bank = SBK[u]

                        def mm_s(e, j=n, hf=hf, bank=bank):
                            ins = None
                            for cc in range(2):
                                ins = e.matmul(PS[:, bank * 512:(bank + 1) * 512], lhsT=ckvT[:, cc, j * 128:(j + 1) * 128],
                                               rhs=QA[:, cc, 4 * hf:4 * hf + 4, :], start=(cc == 0), stop=(cc == 1))
                            return ins
                        P.op("pe", mm_s, reads=[b_ckvT[n], bQA], writes=[PB[bank]])
                        P.op("act", lambda e, u=u, bank=bank: e.activation(
                            out=Eb[u][:], in_=PS[:, bank * 512:(bank + 1) * 512].rearrange("p (h t) -> p h t", h=4),
                            func=AF.Exp), reads=[PB[bank]], writes=[b_Eb[u]])
                        P.op("dve", lambda e, u=u, j=n: e.tensor_tensor(
                            out=Eb[u][:], in0=Eb[u][:], in1=maskT[:, j:j + 1, :].to_broadcast([128, 4, 128]), op=ALU.mult),
                            reads=[b_Eb[u], b_maskT], writes=[b_Eb[u]])

                    def pv_step(n, hf=hf):
                        u = n % NE

                        def mm_pv(e, j=n, hf=hf, u=u):
                            ins = None
                            for hl in range(4):
                                h = 4 * hf + hl
                                ins = e.matmul(PS[:, 1024 + hl * 256:1024 + hl * 256 + 129], lhsT=Eb[u][:, hl, :],
                                               rhs=Vaug[:, j, h, :], start=(j == 0 and hl % 2 == 0), stop=(j == i),
                                               skip_group_check=True)
                            return ins
                        P.op("pe", mm_pv, reads=[b_Eb[u], b_Vaug[n], b_Vones], writes=PB[2:4])

                    for n in range(min(NE - 1, N)):
                        score_step(n)
                    yield 2.5
                    for n in range(N):
                        if n + NE - 1 < N:
                            score_step(n + NE - 1)
                        pv_step(n)
                        yield 0.85
                    P.op("dve", lambda e: e.reciprocal(out=rden[:], in_=OV[:, :, 128:129]), reads=PB[2:4], writes=[b_rden])
                    P.op("dve", lambda e, hf=hf: e.tensor_tensor(
                        out=on[:, hf * 512:(hf + 1) * 512].rearrange("p (h v) -> p h v", h=4), in0=OV[:, :, 0:128],
                        in1=rden[:].to_broadcast([128, 4, 128]), op=ALU.mult),
                        reads=PB[2:4] + [b_rden], writes=[b_on[hf]])
                    yield 0.85
                P.op("dve", lambda e: e.scalar_tensor_tensor(out=ogB, in0=on, scalar=0.5, in1=TH,
                                                             op0=ALU.mult, op1=ALU.mult),
                     reads=b_on + [bTH], writes=[b_hn])
                transposes(hn, 8, 2048, lambda g0, g1: hnT[:, g0:g1, :], [b_hn], [b_hnT])
                yield 2.5

                def mm_y(e):
                    ins = None
                    for nb in range(2):
                        for c in range(8):
                            ins = e.matmul(PS[:, 3072 + nb * 512:3072 + (nb + 1) * 512], lhsT=hnT[:, c, :],
                                           rhs=wout0[:, c, nb * 512:(nb + 1) * 512], start=(c == 0), stop=(c == 7))
                    return ins
                P.op("pe", mm_y, reads=[b_hnT, b_wout0], writes=PB[6:8])
                P.op("dve", lambda e: e.tensor_tensor(out=X[:], in0=X[:], in1=PS[:, 3072:4096], op=ALU.add),
                     reads=[bX] + PB[6:8], writes=[bX])
                yield 2.5
                P.op("act", lambda e: e.activation(out=hn[:], in_=X[:], func=AF.Square, accum_out=st[:, 0:1]),
                     reads=[bX], writes=[b_hn, b_st])
                P.op("dve", lambda e: e.tensor_scalar(out=st[:, 0:1], in0=st[:, 0:1], scalar1=1.0 / D, scalar2=EPS,
                                                      op0=ALU.mult, op1=ALU.add), reads=[b_st], writes=[b_st])
                newton_rsqrt(P, st[:, 0:1], st[:, 28:31], [b_st], 1)
                P.op("dve", lambda e: e.scalar_tensor_tensor(out=hn[:], in0=X[:], scalar=st[:, 0:1], in1=gple[:],
                                                             op0=ALU.mult, op1=ALU.mult),
                     reads=[bX, b_st, b_gple], writes=[b_hn])
                transposes(hn, 8, 2048, lambda g0, g1: hnT[:, g0:g1, :], [b_hn], [b_hnT])
                yield 2.5

                def mm_u(e):
                    ins = None
                    for nb in range(2):
                        for c in range(8):
                            ins = e.matmul(PS[:, 1024 + nb * 512:1024 + (nb + 1) * 512], lhsT=hnT[:, c, :],
                                           rhs=wpg[:, c, nb * 512:(nb + 1) * 512], start=(c == 0), stop=(c == 7))
                    return ins
                P.op("pe", mm_u, reads=[b_hnT, b_wpg], writes=PB[2:4])
                P.op("act", lambda e: e.activation(out=ptb[:], in_=pt[par][:], func=AF.Copy),
                     reads=[b_pt[par]], writes=[b_ptb])
                transposes(ptb, 2, 3072, lambda g0, g1: pT[:, g0:g1, :], [b_ptb], [b_pT])
                yield 2.5

                def mm_pw(e):
                    ins = None
                    for nb in range(2):
                        for c in range(2):
                            ins = e.matmul(PS[:, 3072 + nb * 512:3072 + (nb + 1) * 512], lhsT=pT[:, c, :],
                                           rhs=wp[:, c, nb * 512:(nb + 1) * 512], start=(c == 0), stop=(c == 1))
                    return ins
                P.op("pe", mm_pw, reads=[b_pT, b_wp], writes=PB[6:8])
                P.op("act", lambda e: e.activation(out=tg, in_=PS[:, 1024:2048], func=AF.Tanh, scale=0.5),
                     reads=PB[2:4], writes=b_tg)
                P.op("dve", lambda e: e.scalar_tensor_tensor(out=tg, in0=tg, scalar=1.0, in1=PS[:, 3072:4096],
                                                             op0=ALU.add, op1=ALU.mult),
                     reads=b_tg + PB[6:8], writes=b_tg)
                P.op("dve", lambda e: e.scalar_tensor_tensor(out=X[:], in0=tg, scalar=0.5, in1=X[:],
                                                             op0=ALU.mult, op1=ALU.add),
                     reads=b_tg + [bX], writes=[bX])
                if mode == "full":
                    P.dma("sp", h1_d[s, i * 128:(i + 1) * 128, :], X[:], ds_x3[sl], reads=[bX], writes=[b_h1[s][i]])
                else:
                    P.dma("sp", out_d[s, i * 128:(i + 1) * 128, :], X[:], ds_x3[sl], reads=[bX])
                yield 2.5

            items = [(s, i) for s in range(NS) for i in range(NT)]

            def run_all(g):
                for _ in g:
                    pass

            def interleave3(gl):
                gens = [[g, 0.0, t] for (g, t) in gl]
                while gens:
                    g = min(gens, key=lambda x: x[1] / x[2])
                    try:
                        c = next(g[0])
                        g[1] += (c if c else 1.0)
                    except StopIteration:
                        gens.remove(g)

            def idxbis_total(i):
                nk = (i + 1) * 128
                t = 5.0 + 0.7 * 8 * ((nk + 511) // 512)
                if nk > TOPK:
                    t += KBIS * (1.5 + (nk + 224) / 1200.0)
                return t

            def back_total(i):
                return 1.0 * ((i + 4) // 4) + 2 * (0.85 * (i + 2) + 2.5) + 2.5 * 5

            PROJ_T = 32.0

            def loadx(g):
                s_, i_ = items[g]
                P.dma("sp", xt3[g % 3][:], x_d[s_, i_ * 128:(i_ + 1) * 128, :], ds_x3[g % 3], writes=[b_xt3[g % 3]])

            def loadp(g):
                s_, i_ = items[g]
                P.dma("sp", pt[g % 2][:], p_d[0, s_, i_ * 128:(i_ + 1) * 128, :], ds_p[g % 2], writes=[b_pt[g % 2]])

            NI = len(items)
            for g in range(min(3, NI)):
                loadx(g)
            for g in range(min(2, NI)):
                loadp(g)
            for s_ in range(NS):
                g0 = s_ * NT
                run_all(proj(s_, 0, g0 % 2, g0 % 3))
                run_all(projB(s_, 0, g0 % 2))
                gl = [(idxbis(s_, 0, g0 % 2), idxbis_total(0))]
                if NT > 1:
                    gl.append((proj(s_, 1, (g0 + 1) % 2, (g0 + 1) % 3), PROJ_T))
                interleave3(gl)
                for m in range(NT):
                    g = g0 + m
                    if m + 1 < NT:
                        run_all(projB(s_, m + 1, (g + 1) % 2))
                    gl = [(back(s_, m, g % 2, g % 3), back_total(m))]
                    if m + 1 < NT:
                        gl.append((idxbis(s_, m + 1, (g + 1) % 2), idxbis_total(m + 1)))
                    if m + 2 < NT:
                        gl.append((proj(s_, m + 2, (g + 2) % 2, (g + 2) % 3), PROJ_T))
                    interleave3(gl)
                    if g + 3 < NI:
                        loadx(g + 3)
                    if g + 2 < NI:
                        loadp(g + 2)

        def layer1a(es1):
            sb = lambda name, shape, dt: es1.enter_context(nc.sbuf_tensor("sb_" + name, list(shape), dt))
            w1in = sb("w1in", [128, 8, 6144], BF16); b_w1in = [B("w1in%d" % c) for c in range(3)]
            for c in range(3):
                P.dma("pool", w1in[:, :, c * 2048:(c + 1) * 2048], w1in_d[:, :, c * 2048:(c + 1) * 2048], ds_w,
                      writes=[b_w1in[c]])
            P.dma("sp", gpre[:], vec_d[2], ds_v, writes=[b_gpre])
            rot = [sb("rot%d" % k, [128, 4, 4, 128], F32) for k in range(2)]
            b_rot = [B("rot%d" % k) for k in range(2)]
            ds_r = [P.dsem("r%d" % k) for k in range(2)]
            ds_g = [P.dsem("og%d" % k) for k in range(2)]
            Uh = sb("Uh", [128, 2, 4, 512], F32); b_Uh = B("Uh")
            Ubf = sb("Ubf", [128, 2, 4, 512], BF16); b_Ubf = B("Ubf")
            qr = [sb("qr%d" % k, [128, 1024], BF16) for k in range(2)]; b_qr = [B("qr%d" % k) for k in range(2)]
            kr = [sb("kr%d" % k, [128, 1024], BF16) for k in range(2)]; b_kr = [B("kr%d" % k) for k in range(2)]
            vb = [sb("vb%d" % k, [128, 2048], BF16) for k in range(2)]; b_vb = [B("vb%d" % k) for k in range(2)]
            thb = [th, sb("thb1", [128, 2048], BF16)]; b_thb = [b_th, B("thb1")]
            ogb = [sb("ogb%d" % k, [128, 2048], BF16) for k in range(2)]; b_ogb = [B("ogb%d" % k) for k in range(2)]
            qkT = sb("qkT", [128, 16, 128], BF16); b_qkT = B("qkT")
            rtm = sb("rtm", [128, 2048], F32); b_rt = [B("rtm%d" % k) for k in range(4)]
            rt_ = [rtm[:, q * 512:(q + 1) * 512] for q in range(4)]
            stB = sb("stB", [128, 48], F32); b_stB = B("stB")
            sTm = sb("sTm", [128, 4, 128], BF16); b_sTm = B("sTm")

            def front(s, i, par):
                X = xt[par]; bX = b_xt[par]
                R = rot[par]; bR = b_rot[par]
                P.op("act", lambda e: e.activation(out=hn[:], in_=X[:], func=AF.Square, accum_out=st[:, 0:1]),
                     reads=[bX], writes=[b_hn, b_st])
                P.op("dve", lambda e: e.tensor_scalar(out=st[:, 0:1], in0=st[:, 0:1], scalar1=1.0 / D, scalar2=EPS,
                                                      op0=ALU.mult, op1=ALU.add), reads=[b_st], writes=[b_st])
                newton_rsqrt(P, st[:, 0:1], st[:, 28:31], [b_st], 1)
                P.op("dve", lambda e: e.scalar_tensor_tensor(out=hn[:], in0=X[:], scalar=st[:, 0:1], in1=gpre[:],
                                                             op0=ALU.mult, op1=ALU.mult),
                     reads=[bX, b_st, b_gpre], writes=[b_hn])
                yield 4.0
                transposes(hn, 8, 0, lambda g0, g1: hnT[:, g0:g1, :], [b_hn], [b_hnT])
                yield 2.0

                def mm_in(e, col0, nb):
                    ins = None
                    for c in range(8):
                        ins = e.matmul(PS[:, nb * 512:(nb + 1) * 512], lhsT=hnT[:, c, :],
                                       rhs=w1in[:, c, col0 + nb * 512:col0 + (nb + 1) * 512],
                                       start=(c == 0), stop=(c == 7))
                    return ins
                for nb in range(4):
                    P.op("pe", lambda e, nb=nb: mm_in(e, 0, nb), reads=[b_hnT, b_w1in[0]], writes=[PB[nb]])

                def v4(t):
                    return t.rearrange("p (h m) -> p h m", h=4)
                for (zc0, ti, dstt, bd) in ((0, 0, qr[par], b_qr[par]), (1024, 2, kr[par], b_kr[par])):
                    Z = PS[:, zc0:zc0 + 1024].rearrange("p (h m two) -> p h m two", h=4, two=2)
                    Dv = dstt[:].rearrange("p (h m two) -> p h m two", h=4, two=2)
                    C = R[:, ti, :, :]
                    S = R[:, ti + 1, :, :]
                    zb = pbk(zc0, zc0 + 1024)
                    P.op("dve", lambda e, Z=Z, C=C: e.tensor_tensor(out=v4(rt_[0]), in0=Z[:, :, :, 0], in1=C, op=ALU.mult),
                         reads=zb + [bR], writes=[b_rt[0]])
                    P.op("dve", lambda e, Z=Z, S=S: e.tensor_tensor(out=v4(rt_[1]), in0=Z[:, :, :, 1], in1=S, op=ALU.mult),
                         reads=zb + [bR], writes=[b_rt[1]])
                    P.op("dve", lambda e, Dv=Dv: e.tensor_tensor(out=Dv[:, :, :, 0], in0=v4(rt_[0]), in1=v4(rt_[1]),
                                                                 op=ALU.subtract),
                         reads=[b_rt[0], b_rt[1]], writes=[bd])
                    P.op("dve", lambda e, Z=Z, C=C: e.tensor_tensor(out=v4(rt_[2]), in0=Z[:, :, :, 1], in1=C, op=ALU.mult),
                         reads=zb + [bR], writes=[b_rt[2]])
                    P.op("dve", lambda e, Z=Z, S=S: e.tensor_tensor(out=v4(rt_[3]), in0=Z[:, :, :, 0], in1=S, op=ALU.mult),
                         reads=zb + [bR], writes=[b_rt[3]])
                    P.op("dve", lambda e, Dv=Dv: e.tensor_tensor(out=Dv[:, :, :, 1], in0=v4(rt_[2]), in1=v4(rt_[3]),
                                                                 op=ALU.add),
                         reads=[b_rt[2], b_rt[3]], writes=[bd])
                yield 14.0
                for nb in range(4):
                    P.op("pe", lambda e, nb=nb: mm_in(e, 2048, nb), reads=[b_hnT, b_w1in[1]], writes=[PB[nb]])
                    P.op("act", lambda e, nb=nb: e.activation(out=vb[par][:, nb * 512:(nb + 1) * 512],
                                                              in_=PS[:, nb * 512:(nb + 1) * 512], func=AF.Copy),
                         reads=[PB[nb]], writes=[b_vb[par]])
                    yield 1.7
                for nb in range(4):
                    P.op("pe", lambda e, nb=nb: mm_in(e, 4096, nb), reads=[b_hnT, b_w1in[2]], writes=[PB[nb]])
                    P.op("act", lambda e, nb=nb: e.activation(out=thb[par][:, nb * 512:(nb + 1) * 512],
                                                              in_=PS[:, nb * 512:(nb + 1) * 512], func=AF.Tanh, scale=0.5),
                         reads=[PB[nb]], writes=[b_thb[par]])
                    P.op("dve", lambda e, nb=nb: e.scalar_tensor_tensor(
                        out=thb[par][:, nb * 512:(nb + 1) * 512], in0=thb[par][:, nb * 512:(nb + 1) * 512], scalar=1.0,
                        in1=PS[:, nb * 512:(nb + 1) * 512], op0=ALU.add, op1=ALU.mult),
                        reads=[b_thb[par], PB[nb]], writes=[b_thb[par]])
                    yield 1.7

            def back(s, i, par):
                QR = qr[par]; KR = kr[par]; VB = vb[par]; TH = thb[par]; OG = ogb[par]
                bQR = b_qr[par]; bKR = b_kr[par]; bVB = b_vb[par]; bTH = b_thb[par]; bOG = b_ogb[par]
                transposes(QR, 8, 2048, lambda g0, g1: qkT[:, g0:g1, :], [bQR], [b_qkT])
                yield 2.0
                transposes(KR, 8, 3072, lambda g0, g1: qkT[:, 8 + g0:8 + g1, :], [bKR], [b_qkT])
                yield 2.0
                def mm_st(e):
                    ins = None
                    for h in range(4):
                        for dc in range(2):
                            ins = e.matmul(PS[:, 2048 + h * 128:2048 + (h + 1) * 128], lhsT=qkT[:, 8 + 2 * h + dc, :],
                                           rhs=qkT[:, 2 * h + dc, :], start=(dc == 0), stop=(dc == 1))
                    return ins
                P.op("pe", mm_st, reads=[b_qkT], writes=[PB[4]])
                P.op("dve", lambda e: e.tensor_tensor(out=sTm[:], in0=PS[:, 2048:2560].rearrange("p (h t) -> p h t", h=4),
                                                      in1=causT[:].unsqueeze(1).to_broadcast([128, 4, 128]), op=ALU.mult),
                     reads=[PB[4], b_causT], writes=[b_sTm])
                yield 1.5
                for h in range(4):
                    def mm_o(e, h=h):
                        ins = e.matmul(PS[:, 2048 + h * 512:2048 + (h + 1) * 512], lhsT=sTm[:, h, :],
                                       rhs=VB[:, h * 512:(h + 1) * 512], start=True, stop=(i == 0))
                        if i > 0:
                            for dc in range(2):
                                ins = e.matmul(PS[:, 2048 + h * 512:2048 + (h + 1) * 512], lhsT=qkT[:, 2 * h + dc, :],
                                               rhs=Ubf[:, dc, h, :], start=False, stop=(dc == 1))
                        return ins
                    P.op("pe", mm_o, reads=[b_sTm, bVB, b_qkT] + ([b_Ubf] if i > 0 else []), writes=[PB[4 + h]])
                    oc = 2048 + h * 512
                    P.op("act", lambda e, h=h, oc=oc: e.activation(out=OG[:, h * 512:(h + 1) * 512], in_=PS[:, oc:oc + 512],
                                                                   func=AF.Copy, accum_out=stB[:, 12 + h:13 + h]),
                         reads=[PB[4 + h]], writes=[bOG, b_stB])
                    P.op("act", lambda e, h=h, oc=oc: e.activation(out=OG[:, h * 512:(h + 1) * 512], in_=PS[:, oc:oc + 512],
                                                                   func=AF.Square, accum_out=stB[:, 16 + h:17 + h]),
                         reads=[PB[4 + h]], writes=[bOG, b_stB])
                    yield 1.5
                if i + 1 < NT:
                    for h in range(4):
                        pc = (h % 2) * 1024

                        def mm_d(e, h=h, pc=pc):
                            ins = None
                            for dc in range(2):
                                ins = e.matmul(PS[:, pc + dc * 512:pc + (dc + 1) * 512],
                                               lhsT=KR[:, h * 256 + dc * 128:h * 256 + (dc + 1) * 128],
                                               rhs=VB[:, h * 512:(h + 1) * 512], start=True, stop=True)
                            return ins
                        P.op("pe", mm_d, reads=[bKR, bVB], writes=pbk(pc, pc + 1024))
                        Dl = PS[:, pc:pc + 1024].rearrange("p (c v) -> p c v", c=2)
                        if i == 0:
                            P.op("dve", lambda e, h=h, Dl=Dl: e.tensor_copy(out=Uh[:, :, h, :], in_=Dl),
                                 reads=pbk(pc, pc + 1024), writes=[b_Uh])
                        else:
                            P.op("dve", lambda e, h=h, Dl=Dl: e.scalar_tensor_tensor(
                                out=Uh[:, :, h, :], in0=Uh[:, :, h, :], scalar=float(gch[h]), in1=Dl,
                                op0=ALU.mult, op1=ALU.add), reads=pbk(pc, pc + 1024) + [b_Uh], writes=[b_Uh])
                        P.op("act", lambda e, h=h: e.activation(out=Ubf[:, :, h, :], in_=Uh[:, :, h, :], func=AF.Copy,
                                                                scale=float(gch[h])),
                             reads=[b_Uh], writes=[b_Ubf])
                        yield 2.5

                P.op("dve", lambda e: e.tensor_scalar(out=stB[:, 12:16], in0=stB[:, 12:16], scalar1=1.0 / 512, scalar2=None,
                                                      op0=ALU.mult), reads=[b_stB], writes=[b_stB])
                P.op("dve", lambda e: e.tensor_tensor(out=stB[:, 20:24], in0=stB[:, 12:16], in1=stB[:, 12:16], op=ALU.mult),
                     reads=[b_stB], writes=[b_stB])
                P.op("dve", lambda e: e.scalar_tensor_tensor(out=stB[:, 16:20], in0=stB[:, 16:20], scalar=1.0 / 512,
                                                             in1=stB[:, 20:24], op0=ALU.mult, op1=ALU.subtract),
                     reads=[b_stB], writes=[b_stB])
                P.op("dve", lambda e: e.tensor_scalar(out=stB[:, 16:20], in0=stB[:, 16:20], scalar1=1.0, scalar2=EPS,
                                                      op0=ALU.mult, op1=ALU.add), reads=[b_stB], writes=[b_stB])
                newton_rsqrt(P, stB[:, 16:20], stB[:, 32:44], [b_stB], 4)
                P.op("dve", lambda e: e.scalar_tensor_tensor(out=stB[:, 24:28], in0=stB[:, 12:16], scalar=-1.0, in1=stB[:, 16:20],
                                                             op0=ALU.mult, op1=ALU.mult), reads=[b_stB], writes=[b_stB])
                yield 5.0
                for h in range(4):
                    oc = 2048 + h * 512
                    P.op("act", lambda e, h=h, oc=oc: e.activation(out=PS[:, oc:oc + 512], in_=PS[:, oc:oc + 512],
                                                                   func=AF.Identity, scale=stB[:, 16 + h:17 + h],
                                                                   bias=stB[:, 24 + h:25 + h]),
                         reads=[PB[4 + h], b_stB], writes=[PB[4 + h]])
                    P.op("dve", lambda e, h=h, oc=oc: e.scalar_tensor_tensor(
                        out=OG[:, h * 512:(h + 1) * 512], in0=PS[:, oc:oc + 512], scalar=0.5,
                        in1=TH[:, h * 512:(h + 1) * 512], op0=ALU.mult, op1=ALU.mult),
                        reads=[PB[4 + h], bTH], writes=[bOG])
                    yield 1.2
                if mode == "l1a":
                    P.op("act", lambda e: e.activation(out=xt[par][:], in_=OG[:, 0:1024], func=AF.Copy), reads=[bOG], writes=[b_xt[par]])
                    P.dma("sp", out_d[s, i * 128:(i + 1) * 128, :], xt[par][:], ds_x[par], reads=[b_xt[par]])
                else:
                    P.dma("sp", og_d[s, i * 128:(i + 1) * 128, :], OG[:], ds_g[par], reads=[bOG], writes=[b_ogd[s][i]])
            src = h1_d if mode == "full" else x_d
            items = [(s, i) for s in range(NS) for i in range(NT)]

            def load1(n, k):
                s, i = items[n]
                load_tile(s, i, k, 1, src, [b_h1[s][i]] if mode == "full" else [])
                P.dma("sp", rot[k][:], rot_d[i], ds_r[k], writes=[b_rot[k]])

            def run_all(g):
                for _ in g:
                    pass

            def interleave(ga, gb):
                gens = [[ga, 0.0, 34.0], [gb, 0.0, 37.0]]
                while gens:
                    g = min(gens, key=lambda x: x[1] / x[2])
                    try:
                        c = next(g[0])
                        g[1] += (c if c else 1.0)
                    except StopIteration:
                        gens.remove(g)

            load1(0, 0)
            if len(items) > 1:
                load1(1, 1)
            run_all(front(items[0][0], items[0][1], 0))
            for n in range(len(items)):
                s, i = items[n]
                if n + 2 < len(items):
                    load1(n + 2, n % 2)
                gb = back(s, i, n % 2)
                if n + 1 < len(items):
                    interleave(gb, front(items[n + 1][0], items[n + 1][1], (n + 1) % 2))
                else:
                    run_all(gb)

        def layer1b(es2):
            sb = lambda name, shape, dt: es2.enter_context(nc.sbuf_tensor("sb_" + name, list(shape), dt))
            wout1 = sb("wout1", [128, 16, 1024], BF16); b_wout1 = B("wout1")
            wpg = sb("wpg1", [128, 8, 1024], BF16); b_wpg = B("wpg1")
            wp = sb("wp1", [128, 2, 1024], BF16); b_wp = B("wp1")
            gfin = sb("gfin", [128, D], F32); b_gfin = B("gfin")
            ogT16 = sb("ogT16", [128, 16, 128], BF16); b_ogT16 = B("ogT16")
            hn2 = sb("hn2", [128, D], BF16); b_hn2 = B("hn2")
            st2 = sb("st2", [128, 8], F32); b_st2 = B("st2")
            big = sb("big2", [128, 2048], F32)
            tg = big[:, 0:1024]
            t2 = big[:, 1024:2048]
            P.dma("pool", wout1[:], wout1_d, ds_w, writes=[b_wout1])
            P.dma("pool", wpg[:], wpg_d[1], ds_w, writes=[b_wpg])
            P.dma("pool", wp[:], wp_d[1], ds_w, writes=[b_wp])
            P.dma("sp", gple[:], vec_d[3], ds_v, writes=[b_gple])
            P.dma("sp", gfin[:], vec_d[4], ds_v, writes=[b_gfin])
            ogl = [sb("ogl%d" % k, [128, 2048], BF16) for k in range(2)]; b_ogl = [B("ogl%d" % k) for k in range(2)]
            ds_gl = [P.dsem("gl%d" % k) for k in range(2)]
            src = h1_d if mode == "full" else x_d
            xtb = xt + [sb("xt%d" % k, [128, D], F32) for k in (2, 3)]
            b_xtb = b_xt + [B("xt2"), B("xt3")]; ds_xb = ds_x + [P.dsem("x2"), P.dsem("x3")]
            ptb3 = pt + [sb("pt%d" % k, [128, 256], F32) for k in (2, 3)]
            b_ptb3 = b_pt + [B("pt2"), B("pt3")]; ds_pb = ds_p + [P.dsem("p2"), P.dsem("p3")]

            def loadx(s, i, k):
                rd = [b_h1[s][i]] if mode == "full" else []
                P.dma("sp", xtb[k][:], src[s, i * 128:(i + 1) * 128, :], ds_xb[k], reads=rd, writes=[b_xtb[k]])
                P.dma("sp", ptb3[k][:], p_d[1, s, i * 128:(i + 1) * 128, :], ds_pb[k], writes=[b_ptb3[k]])

            def loadog(s, i, k):
                P.dma("sp", ogl[k][:], og_d[s, i * 128:(i + 1) * 128, :], ds_gl[k], reads=[b_ogd[s][i]], writes=[b_ogl[k]])

            def frontB(s, i, par, sl):
                X = xtb[sl]; bX = b_xtb[sl]
                for g0 in range(0, 16, 4):
                    transposes(ogl[par][:, g0 * 128:(g0 + 4) * 128], 4, g0 * 128,
                               lambda a0, a1, g0=g0: ogT16[:, g0 + a0:g0 + a1, :], [b_ogl[par]], [b_ogT16])
                    yield 1.0

                for nb in range(2):
                    def mm_y1(e, nb=nb):
                        ins = None
                        for c in range(16):
                            ins = e.matmul(PS[:, nb * 512:(nb + 1) * 512], lhsT=ogT16[:, c, :],
                                           rhs=wout1[:, c, nb * 512:(nb + 1) * 512], start=(c == 0), stop=(c == 15))
                        return ins
                    P.op("pe", mm_y1, reads=[b_ogT16, b_wout1], writes=[PB[nb]])
                    yield 3.5
                P.op("dve", lambda e: e.tensor_tensor(out=X[:], in0=X[:], in1=PS[:, 0:1024], op=ALU.add),
                     reads=[bX] + PB[0:2], writes=[bX])
                yield 1.0

            def backB(s, i, par, sl):
                X = xtb[sl]; bX = b_xtb[sl]
                P.op("act", lambda e: e.activation(out=hn[:], in_=X[:], func=AF.Square, accum_out=st[:, 0:1]),
                     reads=[bX], writes=[b_hn, b_st])
                rstd_from_ss(0, 1, 1.0 / D)
                P.op("dve", lambda e: e.scalar_tensor_tensor(out=hn[:], in0=X[:], scalar=st[:, 0:1], in1=gple[:],
                                                             op0=ALU.mult, op1=ALU.mult),
                     reads=[bX, b_st, b_gple], writes=[b_hn])
                yield 5.0
                transposes(hn, 8, 2048, lambda g0, g1: hnT[:, g0:g1, :], [b_hn], [b_hnT])
                yield 2.0

                def mm_u(e):
                    ins = None
                    for nb in range(2):
                        for c in range(8):
                            ins = e.matmul(PS[:, 3072 + nb * 512:3072 + (nb + 1) * 512], lhsT=hnT[:, c, :],
                                           rhs=wpg[:, c, nb * 512:(nb + 1) * 512], start=(c == 0), stop=(c == 7))
                    return ins
                P.op("pe", mm_u, reads=[b_hnT, b_wpg], writes=PB[6:8])
                P.op("act", lambda e: e.activation(out=ptb[:], in_=ptb3[sl][:], func=AF.Copy),
                     reads=[b_ptb3[sl]], writes=[b_ptb])
                transposes(ptb, 2, 2048, lambda g0, g1: pT[:, g0:g1, :], [b_ptb], [b_pT])
                yield 4.0

                def mm_pw(e):
                    ins = None
                    for nb in range(2):
                        for c in range(2):
                            ins = e.matmul(PS[:, 2048 + nb * 512:2048 + (nb + 1) * 512], lhsT=pT[:, c, :],
                                           rhs=wp[:, c, nb * 512:(nb + 1) * 512], start=(c == 0), stop=(c == 1))
                    return ins
                P.op("pe", mm_pw, reads=[b_pT, b_wp], writes=PB[4:6])
                P.op("act", lambda e: e.activation(out=tg, in_=PS[:, 3072:4096], func=AF.Tanh, scale=0.5),
                     reads=PB[6:8], writes=b_tg)
                P.op("dve", lambda e: e.scalar_tensor_tensor(out=t2, in0=tg, scalar=1.0, in1=PS[:, 2048:3072],
                                                             op0=ALU.add, op1=ALU.mult),
                     reads=b_tg + PB[4:6], writes=b_t2)
                P.op("dve", lambda e: e.scalar_tensor_tensor(out=X[:], in0=t2, scalar=0.5, in1=X[:],
                                                             op0=ALU.mult, op1=ALU.add),
                     reads=b_t2 + [bX], writes=[bX])
                yield 5.0

            def backB2(s, i, par, sl):
                X = xtb[sl]; bX = b_xtb[sl]
                P.op("act", lambda e: e.activation(out=hn2[:], in_=X[:], func=AF.Square, accum_out=st2[:, 0:1]),
                     reads=[bX], writes=[b_hn2, b_st2])
                P.op("dve", lambda e: e.tensor_scalar(out=st2[:, 0:1], in0=st2[:, 0:1], scalar1=1.0 / D, scalar2=EPS,
                                                      op0=ALU.mult, op1=ALU.add), reads=[b_st2], writes=[b_st2])
                yield 2.0
                newton_rsqrt(P, st2[:, 0:1], st2[:, 4:7], [b_st2], 1)
                yield 3.0
                P.op("dve", lambda e: e.scalar_tensor_tensor(out=X[:], in0=X[:], scalar=st2[:, 0:1], in1=gfin[:],
                                                             op0=ALU.mult, op1=ALU.mult),
                     reads=[bX, b_st2, b_gfin], writes=[bX])
                P.dma("sp", out_d[s, i * 128:(i + 1) * 128, :], X[:], ds_xb[sl], reads=[bX])
                yield 2.0

            items = [(s, i) for s in range(NS) for i in range(NT)]

            def run_all(g):
                for _ in g:
                    pass

            def interleave(ga, gb, ta, tb):
                gens = [[ga, 0.0, ta], [gb, 0.0, tb]]
                while gens:
                    g = min(gens, key=lambda x: x[1] / x[2])
                    try:
                        c = next(g[0])
                        g[1] += (c if c else 1.0)
                    except StopIteration:
                        gens.remove(g)

            def interleave3(gl):
                gens = [[g, 0.0, t] for (g, t) in gl if g is not None]
                while gens:
                    g = min(gens, key=lambda x: x[1] / x[2])
                    try:
                        c = next(g[0])
                        g[1] += (c if c else 1.0)
                    except StopIteration:
                        gens.remove(g)

            NI = len(items)
            for n in range(min(3, NI)):
                loadx(items[n][0], items[n][1], n % 4)
            for n in range(min(2, NI)):
                loadog(items[n][0], items[n][1], n % 2)
            run_all(frontB(items[0][0], items[0][1], 0, 0))
            for m in range(NI + 1):
                if m + 2 < NI:
                    loadog(items[m + 2][0], items[m + 2][1], m % 2)
                gl = []
                if m - 1 >= 0:
                    gl.append((backB2(items[m - 1][0], items[m - 1][1], (m - 1) % 2, (m - 1) % 4), 7.0))
                if m < NI:
                    gl.append((backB(items[m][0], items[m][1], m % 2, m % 4), 16.0))
                if m + 1 < NI:
                    gl.append((frontB(items[m + 1][0], items[m + 1][1], (m + 1) % 2, (m + 1) % 4), 12.0))
                interleave3(gl)
                if m + 3 < NI:
                    loadx(items[m + 3][0], items[m + 3][1], (m + 3) % 4)

        if mode in ("full", "l0"):
            with ExitStack() as es0:
                layer0(es0)
            P.barrier()
        if mode in ("full", "l1", "l1a"):
            with ExitStack() as es1:
                layer1a(es1)
            P.barrier()
        if mode in ("full", "l1"):
            with ExitStack() as es2:
                layer1b(es2)
        P.emit(final_waits=P.dsems)
    return nc


def _chunked(w, c):
    n = w.shape[1]
    return np.ascontiguousarray(w.reshape(c, 128, n).transpose(1, 0, 2))


def host_consts(L, KBIS):
    NT = L // 128
    i = np.arange(128)
    ident = np.eye(128, dtype=np.float32)
    causT = (i[None, :] >= i[:, None]).astype(np.float32)
    cbias = np.where(i[None, :] <= i[:, None], 0.0, -1e9).astype(np.float32)
    pow2 = np.broadcast_to((2.0 ** -(np.arange(KBIS) + 1.0)).astype(np.float32), (128, KBIS)).copy()
    pos = np.arange(L, dtype=np.float32)
    angle = (1.0 / (10000.0 ** np.linspace(0.0, 1.0, 128, dtype=np.float32))).astype(np.float32)
    theta = pos[:, None] * angle[None, :]
    cos = np.cos(theta).astype(np.float32).reshape(NT, 128, 128)
    sin = np.sin(theta).astype(np.float32).reshape(NT, 128, 128)
    gam = np.array([1.0 - 2.0 ** (-5.0 - h) for h in range(4)], dtype=np.float64)
    xi = gam[None, :] ** (i[:, None] + 1.0)
    ks = (gam[None, :] ** (-(i[:, None] + 1.0))) * (256.0 ** -0.5)
    rot = np.empty((NT, 128, 4, 4, 128), dtype=np.float32)
    rot[:, :, 0] = cos[:, :, None, :] * xi[None, :, :, None]
    rot[:, :, 1] = sin[:, :, None, :] * xi[None, :, :, None]
    rot[:, :, 2] = cos[:, :, None, :] * ks[None, :, :, None]
    rot[:, :, 3] = sin[:, :, None, :] * ks[None, :, :, None]
    return dict(ident=ident, causT=causT, cbias=cbias, pow2=pow2, rot=rot)


def host_weights(g_pre, a_w_in, a_g_q, a_g_kv, a_w_q_up, a_w_idx_q, a_g_ik, a_b_ik, a_w_uk, a_w_uv, a_w_out,
                 r_w_in, r_w_out, w_ple_gate, g_ple, w_ple, g_final):
    f = np.float32
    rep = lambda v: np.broadcast_to(np.asarray(v, f)[None, :], (128, v.shape[0]))
    vecs = np.stack([rep(g_pre[0]), rep(g_ple[0]), rep(g_pre[1]), rep(g_ple[1]), rep(g_final)]).astype(f)
    vsm = np.concatenate([rep(a_g_q[0]), rep(a_g_kv[0]), rep(a_g_ik[0]), rep(a_b_ik[0])], axis=1).astype(f)
    w = np.asarray(a_w_in[0], f)
    wpad = np.zeros((1024, 1792), f)
    wpad[:, 0:384] = w[:, 0:384]
    wpad[:, 384:448] = w[:, 640:704]
    wpad[:, 448:456] = w[:, 704:712]
    wpad[:, 512:768] = w[:, 384:640]
    wpad[:, 768:1792] = w[:, 712:1736]
    wukT = np.ascontiguousarray(np.asarray(a_w_uk[0], f).transpose(2, 0, 1))
    wuv = np.asarray(a_w_uv[0], f).transpose(1, 0, 2).reshape(256, 1024)
    return dict(
        vecs=np.ascontiguousarray(vecs), vsm=np.ascontiguousarray(vsm),
        w0in=_chunked(wpad, 8), wq=_chunked(np.asarray(a_w_q_up[0], f), 3), wiq=_chunked(np.asarray(a_w_idx_q[0], f), 3),
        wukT=wukT, wuv=_chunked(wuv, 2), wout0=_chunked(np.asarray(a_w_out[0], f), 8),
        wpg=np.stack([_chunked(np.asarray(w_ple_gate[l], f), 8) for l in range(2)]),
        wp=np.stack([_chunked(np.asarray(w_ple[l], f), 2) for l in range(2)]),
        w1in=_chunked(np.asarray(r_w_in[0], f), 8), wout1=_chunked(np.asarray(r_w_out[0], f), 16),
    )


_NC_CACHE = {}


def run(x, p, wts, L, NS, ncores, KBIS=24, mode="full"):
    key = (L, NS, KBIS, mode)
    if key not in _NC_CACHE:
        _NC_CACHE[key] = build(L, NS, KBIS, mode)
    nc = _NC_CACHE[key]
    consts = host_consts(L, KBIS)
    in_maps = []
    for c in range(ncores):
        m = dict(wts)
        m.update(consts)
        m["x"] = np.ascontiguousarray(x[c * NS:(c + 1) * NS])
        m["p"] = np.ascontiguousarray(p[:, c * NS:(c + 1) * NS])
        in_maps.append(m)
    res = run_bass_kernel_spmd(nc, in_maps, core_ids=list(range(ncores)))
    return np.concatenate([r["out"] for r in res.results], axis=0)


def kernel(x, p, g_pre, a_w_in, a_g_q, a_g_kv, a_w_q_up, a_w_idx_q, a_g_ik, a_b_ik, a_w_uk, a_w_uv, a_w_out,
           r_w_in, r_w_out, w_ple_gate, g_ple, w_ple, g_final):
    x = np.asarray(x, np.float32)
    p = np.asarray(p, np.float32)
    wts = host_weights(g_pre, a_w_in, a_g_q, a_g_kv, a_w_q_up, a_w_idx_q, a_g_ik, a_b_ik, a_w_uk, a_w_uv, a_w_out,
                       r_w_in, r_w_out, w_ple_gate, g_ple, w_ple, g_final)
    Bn, L, _ = x.shape
    out = run(x, p, wts, L, Bn // 8, 8)
    return out.astype(np.float32)
```
